# Optimizing a Trainium2 kernel written in Bass

```python
import math
import jax
import jax.numpy as jnp
from jax import lax
import numpy as np


D_MODEL = 1024
BATCH = 16
SEQ = 4096
DEPTH = 1

MLA_HEADS = 8
MLA_NOPE_DIM = 64
MLA_ROPE_DIM = 32
MLA_V_DIM = 64
MLA_Q_LORA = 256
MLA_KV_LORA = 128
ROPE_THETA = 10000.0

NSA_HEADS = 8
NSA_KV_HEADS = 2
NSA_GROUP = NSA_HEADS // NSA_KV_HEADS
NSA_HEAD_DIM = 64
CMP_LEN = 32
CMP_STRIDE = 16
CMP_HIDDEN = 128
SLC_LEN = 64
SLC_TOPN = 16
WINDOW = 512
N_BRANCH = 3

T5_BUCKETS = 32
T5_MAX_DIST = 128

MLA_OUT = MLA_HEADS * MLA_V_DIM
NSA_OUT = NSA_HEADS * NSA_HEAD_DIM
D_MIX = MLA_OUT + NSA_OUT
D_FF = -(-8 * D_MODEL // (3 * 256)) * 256

IN_SPLITS = (
    MLA_Q_LORA,
    MLA_KV_LORA,
    MLA_ROPE_DIM,
    NSA_HEADS * NSA_HEAD_DIM,
    2 * NSA_KV_HEADS * NSA_HEAD_DIM,
    2 * NSA_KV_HEADS * NSA_HEAD_DIM,
    2 * NSA_KV_HEADS * NSA_HEAD_DIM,
    NSA_HEADS * N_BRANCH,
)
D_IN = sum(IN_SPLITS)

Q_BLOCK = 128
NSA_Q_BLOCK = 64
EPS = 1e-6
NEG_INF = -1e30
FORCE_SCORE = 1e30

kernel_name = 'hybrid_mla_nsa_block'


def rmsnorm(x, g):
    xf = x.astype(jnp.float32)
    y = xf * lax.rsqrt(jnp.mean(xf * xf, axis=-1, keepdims=True) + EPS)
    return (y * g.astype(jnp.float32)).astype(x.dtype)


def rope_tables(seq):
    pos = jnp.arange(seq, dtype=jnp.float32)
    inv = ROPE_THETA ** (-jnp.arange(0, MLA_ROPE_DIM, 2, dtype=jnp.float32) / MLA_ROPE_DIM)
    ang = pos[:, None] * inv[None, :]
    return jnp.cos(ang), jnp.sin(ang)


def apply_rope(x, cos, sin):
    half = x.shape[-1] // 2
    x1 = x[..., :half].astype(jnp.float32)
    x2 = x[..., half:].astype(jnp.float32)
    return jnp.concatenate([x1 * cos - x2 * sin, x1 * sin + x2 * cos], axis=-1).astype(x.dtype)


def t5_bucket(dist):
    n = jnp.maximum(dist, 0)
    max_exact = T5_BUCKETS // 2
    nf = jnp.maximum(n, 1).astype(jnp.float32)
    large = max_exact + (jnp.log(nf / max_exact) / math.log(T5_MAX_DIST / max_exact)
                         * (T5_BUCKETS - max_exact)).astype(jnp.int32)
    large = jnp.minimum(large, T5_BUCKETS - 1)
    return jnp.where(n < max_exact, n, large)


def mla_mixer(c_q, c_kv, k_rope, q_norm_g, w_uq, kv_norm_g, w_ukv):
    B, S, _ = c_q.shape
    cos, sin = rope_tables(S)
    q = (rmsnorm(c_q, q_norm_g) @ w_uq).reshape(B, S, MLA_HEADS, MLA_NOPE_DIM + MLA_ROPE_DIM)
    q_nope, q_pe = q[..., :MLA_NOPE_DIM], q[..., MLA_NOPE_DIM:]
    q_pe = apply_rope(q_pe, cos[None, :, None], sin[None, :, None])
    kv = (rmsnorm(c_kv, kv_norm_g) @ w_ukv).reshape(B, S, MLA_HEADS, MLA_NOPE_DIM + MLA_V_DIM)
    k_nope, v = kv[..., :MLA_NOPE_DIM], kv[..., MLA_NOPE_DIM:]
    k_pe = apply_rope(k_rope, cos[None], sin[None])
    k_pe = jnp.broadcast_to(k_pe[:, :, None, :], (B, S, MLA_HEADS, MLA_ROPE_DIM))
    q = jnp.concatenate([q_nope, q_pe], axis=-1).transpose(0, 2, 1, 3)
    k = jnp.concatenate([k_nope, k_pe], axis=-1).transpose(0, 2, 1, 3)
    v = v.transpose(0, 2, 1, 3)
    scale = (MLA_NOPE_DIM + MLA_ROPE_DIM) ** -0.5
    key_pos = jnp.arange(S)

    def attend_block(i):
        start = i * Q_BLOCK
        qb = lax.dynamic_slice_in_dim(q, start, Q_BLOCK, axis=2)
        s = jnp.einsum('bhqd,bhkd->bhqk', qb, k, preferred_element_type=jnp.float32) * scale
        q_pos = start + jnp.arange(Q_BLOCK)
        s = jnp.where(key_pos[None, :] <= q_pos[:, None], s, NEG_INF)
        p = jax.nn.softmax(s, axis=-1).astype(v.dtype)
        return jnp.einsum('bhqk,bhkd->bhqd', p, v)

    o = lax.map(attend_block, jnp.arange(S // Q_BLOCK))
    return o.transpose(1, 0, 3, 2, 4).reshape(B, S, MLA_OUT)


def nsa_compress(raw, pos_emb, w1, w2, cmp_idx):
    B = raw.shape[0]
    n_cmp = cmp_idx.shape[0]
    blk = raw[:, cmp_idx] + pos_emb[None, None, :, None, :]
    blk = blk.transpose(0, 3, 1, 2, 4).reshape(B, NSA_KV_HEADS, n_cmp, CMP_LEN * NSA_HEAD_DIM)
    return jax.nn.gelu(blk @ w1) @ w2


def nsa_mixer(q, kv_cmp, kv_slc, kv_win, gate_logits, pos_k, w1_k, w2_k, pos_v, w1_v, w2_v, t5_table):
    B, S, _ = q.shape
    Hk, G, dk = NSA_KV_HEADS, NSA_GROUP, NSA_HEAD_DIM
    q = q.reshape(B, S, Hk, G, dk).transpose(0, 2, 3, 1, 4)

    def split_kv(kv):
        kv = kv.reshape(B, S, 2, Hk, dk)
        return kv[:, :, 0], kv[:, :, 1]

    n_cmp = (S - CMP_LEN) // CMP_STRIDE + 1
    cmp_start = np.arange(n_cmp) * CMP_STRIDE
    cmp_idx = cmp_start[:, None] + np.arange(CMP_LEN)[None, :]
    cmp_end = jnp.asarray(cmp_start + CMP_LEN - 1, dtype=jnp.int32)
    k_c_raw, v_c_raw = split_kv(kv_cmp)
    k_cmp = nsa_compress(k_c_raw, pos_k, w1_k, w2_k, cmp_idx)
    v_cmp = nsa_compress(v_c_raw, pos_v, w1_v, w2_v, cmp_idx)

    n_slc = S // SLC_LEN
    n_sel = min(SLC_TOPN, n_slc)
    slc_start = np.arange(n_slc) * SLC_LEN
    overlap = np.maximum(0, np.minimum(cmp_start[:, None] + CMP_LEN, slc_start[None, :] + SLC_LEN)
                         - np.maximum(cmp_start[:, None], slc_start[None, :])).astype(np.float32) / CMP_STRIDE
    overlap = jnp.asarray(overlap)
    k_s, v_s = split_kv(kv_slc)
    k_slc = k_s.transpose(0, 2, 1, 3).reshape(B, Hk, n_slc, SLC_LEN, dk)
    v_slc = v_s.transpose(0, 2, 1, 3).reshape(B, Hk, n_slc, SLC_LEN, dk)

    k_w, v_w = split_kv(kv_win)
    pad = ((0, 0), (0, 0), (WINDOW, 0), (0, 0))
    k_win = jnp.pad(k_w.transpose(0, 2, 1, 3), pad)
    v_win = jnp.pad(v_w.transpose(0, 2, 1, 3), pad)

    gates = jax.nn.sigmoid(gate_logits.astype(jnp.float32)).reshape(B, S, Hk, G, N_BRANCH)
    gates = gates.transpose(0, 2, 3, 1, 4).astype(q.dtype)
    tbl = t5_table.T.reshape(Hk, G, T5_BUCKETS)
    scale = dk ** -0.5
    blk_ids = jnp.arange(n_slc)
    b_ix = jnp.arange(B)[:, None, None, None]
    h_ix = jnp.arange(Hk)[None, :, None, None]
    tk_ix = jnp.arange(Hk)[None, :, None, None, None]
    tg_ix = jnp.arange(G)[None, None, :, None, None]

    def nsa_block(i):
        start = i * NSA_Q_BLOCK
        q_pos = start + jnp.arange(NSA_Q_BLOCK)
        qb = lax.dynamic_slice_in_dim(q, start, NSA_Q_BLOCK, axis=3)

        dist_c = q_pos[:, None] - cmp_end[None, :]
        valid_c = dist_c >= 0
        s_c = jnp.einsum('bkgqd,bknd->bkgqn', qb, k_cmp, preferred_element_type=jnp.float32) * scale
        s_c = s_c + tbl[:, :, t5_bucket(dist_c)]
        p_c = jax.nn.softmax(jnp.where(valid_c, s_c, NEG_INF), axis=-1) * valid_c
        o_c = jnp.einsum('bkgqn,bknd->bkgqd', p_c.astype(v_cmp.dtype), v_cmp)

        imp = jnp.einsum('bkgqn,nj->bkqj', p_c, overlap)
        cur = q_pos // SLC_LEN
        forced = ((blk_ids[None, :] == 0) | (blk_ids[None, :] == cur[:, None])
                  | (blk_ids[None, :] == cur[:, None] - 1))
        causal_blk = blk_ids[None, :] <= cur[:, None]
        imp = jnp.where(forced, FORCE_SCORE, jnp.where(causal_blk, imp, NEG_INF))
        _, sel = lax.top_k(imp, n_sel)
        k_sel = k_slc[b_ix, h_ix, sel].reshape(B, Hk, NSA_Q_BLOCK, n_sel * SLC_LEN, dk)
        v_sel = v_slc[b_ix, h_ix, sel].reshape(B, Hk, NSA_Q_BLOCK, n_sel * SLC_LEN, dk)
        pos_sel = (sel[..., None] * SLC_LEN + jnp.arange(SLC_LEN)).reshape(B, Hk, NSA_Q_BLOCK, n_sel * SLC_LEN)
        dist_s = q_pos[None, None, :, None] - pos_sel
        s_s = jnp.einsum('bkgqd,bkqnd->bkgqn', qb, k_sel, preferred_element_type=jnp.float32) * scale
        s_s = s_s + tbl[tk_ix, tg_ix, t5_bucket(dist_s)[:, :, None]]
        p_s = jax.nn.softmax(jnp.where((dist_s >= 0)[:, :, None], s_s, NEG_INF), axis=-1)
        o_s = jnp.einsum('bkgqn,bkqnd->bkgqd', p_s.astype(v_sel.dtype), v_sel)

        k_band = lax.dynamic_slice_in_dim(k_win, start, NSA_Q_BLOCK + WINDOW, axis=2)
        v_band = lax.dynamic_slice_in_dim(v_win, start, NSA_Q_BLOCK + WINDOW, axis=2)
        key_pos_w = start - WINDOW + jnp.arange(NSA_Q_BLOCK + WINDOW)
        dist_w = q_pos[:, None] - key_pos_w[None, :]
        valid_w = (dist_w >= 0) & (dist_w < WINDOW) & (key_pos_w[None, :] >= 0)
        s_w = jnp.einsum('bkgqd,bknd->bkgqn', qb, k_band, preferred_element_type=jnp.float32) * scale
        s_w = s_w + tbl[:, :, t5_bucket(dist_w)]
        p_w = jax.nn.softmax(jnp.where(valid_w, s_w, NEG_INF), axis=-1)
        o_w = jnp.einsum('bkgqn,bknd->bkgqd', p_w.astype(v_band.dtype), v_band)

        g = lax.dynamic_slice_in_dim(gates, start, NSA_Q_BLOCK, axis=3)
        return g[..., 0:1] * o_c + g[..., 1:2] * o_s + g[..., 2:3] * o_w

    o = lax.map(nsa_block, jnp.arange(S // NSA_Q_BLOCK))
    return o.transpose(1, 0, 4, 2, 3, 5).reshape(B, S, NSA_OUT)


def setup_inputs(seed: int = 0) -> dict:
    key = jax.random.key(seed)
    ks = jax.random.split(key, 24)
    f32 = jnp.float32

    def nrm(k, shape, fan_in):
        return jax.random.normal(k, shape, f32) * fan_in ** -0.5

    def gain(k, shape):
        return 1.0 + 0.02 * jax.random.normal(k, shape, f32)

    L = DEPTH
    return {
        'x': jax.random.normal(ks[0], (BATCH, SEQ, D_MODEL), f32),
        'norm_mix_g': gain(ks[1], (L, D_MODEL)),
        'w_in': nrm(ks[2], (L, D_MODEL, D_IN), D_MODEL),
        'mla_q_norm_g': gain(ks[3], (L, MLA_Q_LORA)),
        'mla_w_uq': nrm(ks[4], (L, MLA_Q_LORA, MLA_HEADS * (MLA_NOPE_DIM + MLA_ROPE_DIM)), MLA_Q_LORA),
        'mla_kv_norm_g': gain(ks[5], (L, MLA_KV_LORA)),
        'mla_w_ukv': nrm(ks[6], (L, MLA_KV_LORA, MLA_HEADS * (MLA_NOPE_DIM + MLA_V_DIM)), MLA_KV_LORA),
        'nsa_cmp_pos_k': 0.1 * jax.random.normal(ks[7], (L, CMP_LEN, NSA_HEAD_DIM), f32),
        'nsa_cmp_w1_k': nrm(ks[8], (L, CMP_LEN * NSA_HEAD_DIM, CMP_HIDDEN), CMP_LEN * NSA_HEAD_DIM),
        'nsa_cmp_w2_k': nrm(ks[9], (L, CMP_HIDDEN, NSA_HEAD_DIM), CMP_HIDDEN),
        'nsa_cmp_pos_v': 0.1 * jax.random.normal(ks[10], (L, CMP_LEN, NSA_HEAD_DIM), f32),
        'nsa_cmp_w1_v': nrm(ks[11], (L, CMP_LEN * NSA_HEAD_DIM, CMP_HIDDEN), CMP_LEN * NSA_HEAD_DIM),
        'nsa_cmp_w2_v': nrm(ks[12], (L, CMP_HIDDEN, NSA_HEAD_DIM), CMP_HIDDEN),
        't5_table': 0.2 * jax.random.normal(ks[13], (T5_BUCKETS, NSA_HEADS), f32),
        'out_norm_mla_g': gain(ks[14], (L, MLA_OUT)),
        'out_norm_nsa_g': gain(ks[15], (L, NSA_OUT)),
        'w_out': nrm(ks[16], (L, D_MIX, D_MODEL), D_MIX),
        'norm_ffn_g': gain(ks[17], (L, D_MODEL)),
        'w_gate': nrm(ks[18], (L, D_MODEL, D_FF), D_MODEL),
        'w_up': nrm(ks[19], (L, D_MODEL, D_FF), D_MODEL),
        'w_down': nrm(ks[20], (L, D_FF, D_MODEL), D_FF),
        'final_norm_g': gain(ks[21], (D_MODEL,)),
    }


def reference(x, norm_mix_g, w_in, mla_q_norm_g, mla_w_uq, mla_kv_norm_g, mla_w_ukv,
              nsa_cmp_pos_k, nsa_cmp_w1_k, nsa_cmp_w2_k, nsa_cmp_pos_v, nsa_cmp_w1_v, nsa_cmp_w2_v,
              t5_table, out_norm_mla_g, out_norm_nsa_g, w_out, norm_ffn_g, w_gate, w_up, w_down,
              final_norm_g):
    cuts = [int(c) for c in np.cumsum(IN_SPLITS)[:-1]]
    h = x
    for l in range(DEPTH):
        u = rmsnorm(h, norm_mix_g[l]) @ w_in[l]
        c_q, c_kv, k_rope, q_nsa, kv_cmp, kv_slc, kv_win, gate_logits = jnp.split(u, cuts, axis=-1)
        o_mla = mla_mixer(c_q, c_kv, k_rope, mla_q_norm_g[l], mla_w_uq[l], mla_kv_norm_g[l], mla_w_ukv[l])
        o_nsa = nsa_mixer(q_nsa, kv_cmp, kv_slc, kv_win, gate_logits,
                          nsa_cmp_pos_k[l], nsa_cmp_w1_k[l], nsa_cmp_w2_k[l],
                          nsa_cmp_pos_v[l], nsa_cmp_w1_v[l], nsa_cmp_w2_v[l], t5_table)
        mixed = jnp.concatenate([rmsnorm(o_mla, out_norm_mla_g[l]), rmsnorm(o_nsa, out_norm_nsa_g[l])], axis=-1)
        h = h + mixed @ w_out[l]
        f = rmsnorm(h, norm_ffn_g[l])
        h = h + (jax.nn.silu(f @ w_gate[l]) * (f @ w_up[l])) @ w_down[l]
    return rmsnorm(h, final_norm_g)
```

```python
import math
import os
from contextlib import ExitStack

import ml_dtypes
import numpy as np

import concourse.bass as bass
import concourse.mybir as mybir
from concourse.bass_utils import run_bass_kernel_spmd

F32 = mybir.dt.float32
BF16 = mybir.dt.bfloat16
AF = mybir.ActivationFunctionType
ALU = mybir.AluOpType
NPBF = ml_dtypes.bfloat16

S = 4096
D = 1024
NB = 2
NCORES = 8
DFF = 2816
NTB = S // 512
NT = S // 128
EPS = 1e-6
BIG = 30000.0
SC_M = 96 ** -0.5
HCL = 8176
ENGS = ("pe", "act", "dve", "pool", "sp")
RDMA = 8
STRICT_SAME = True
WARM_N = int(os.environ.get('MK_WARM', '128'))
LAG = int(os.environ.get('MK_LAG', '3'))


class Buf:
    __slots__ = ("name", "w", "r", "ep")

    def __init__(self, name):
        self.name = name
        self.w = None
        self.r = []
        self.ep = -1


class Op:
    __slots__ = ("eng", "fn", "deps", "dma", "n", "sig", "need", "dk", "dval", "bar", "tag")


def I(meth, *a, **k):
    return lambda e: getattr(e, meth)(*a, **k)


class Prog:
    def __init__(self, nc):
        self.nc = nc
        self.ops = {e: [] for e in ENGS}
        self.dmas = {e: [] for e in ENGS}
        self.epoch = 0
        self.tag = ""

    def op(self, eng, fn, r=(), w=(), dma=False, bar=False, extra=()):
        o = Op()
        o.eng, o.fn, o.dma, o.bar = eng, fn, dma, bar
        o.tag = self.tag
        o.need = False
        o.sig = None
        o.n = len(self.ops[eng])
        deps = {}
        for b in list(r) + list(w):
            if b.ep != self.epoch:
                b.w, b.r, b.ep = None, [], self.epoch
        for b in r:
            if b.w is not None:
                deps[b.w] = "raw"
        for b in w:
            if b.w is not None:
                deps.setdefault(b.w, "waw")
            for x in b.r:
                deps.setdefault(x, "war")
        for x in extra:
            deps[x] = "raw"
        fin = []
        for d, kind in deps.items():
            if d is o:
                continue
            if d.eng == eng and not d.dma and not dma and not bar:
                if eng == "pe":
                    continue
                if not STRICT_SAME and (kind != "raw" or o.n - d.n > 3):
                    continue
            fin.append(d)
        if dma:
            k = len(self.dmas[eng])
            o.dk = (eng, k % RDMA)
            o.dval = 16 * (k // RDMA + 1)
            if k >= RDMA:
                fin.append(self.dmas[eng][k - RDMA])
            self.dmas[eng].append(o)
        for d in fin:
            d.need = True
        o.deps = fin
        self.ops[eng].append(o)
        ws = set(id(b) for b in w)
        for b in w:
            b.w = o
            b.r = []
        for b in r:
            if id(b) not in ws:
                b.r.append(o)
        return o

    def barrier(self):
        last = []
        for e in ENGS:
            if self.ops[e]:
                last.append(self.ops[e][-1])
            last.extend(self.dmas[e][-RDMA:])
        bsp = self.op("sp", None, bar=True, extra=last)
        for e in ENGS:
            if e != "sp":
                self.op(e, None, bar=True, extra=[bsp])
        self.epoch += 1

    def check(self):
        done = set()
        pc = {e: 0 for e in ENGS}
        prog = True
        while prog:
            prog = False
            for e in ENGS:
                while pc[e] < len(self.ops[e]):
                    o = self.ops[e][pc[e]]
                    if all(id(d) in done for d in o.deps):
                        done.add(id(o))
                        pc[e] += 1
                        prog = True
                    else:
                        break
        bad = {e: pc[e] for e in ENGS if pc[e] < len(self.ops[e])}
        if bad:
            msg = []
            for e, i in bad.items():
                o = self.ops[e][i]
                msg.append("%s blocked at op %d/%d (%s) waiting on %s" % (
                    e, i, len(self.ops[e]), getattr(o, "tag", ""),
                    [(d.eng, d.n, getattr(d, "tag", "")) for d in o.deps if id(d) not in done]))
            raise RuntimeError("DEADLOCK: " + " | ".join(msg))

    def emit(self):
        self.check()
        nc = self.nc
        for e in ENGS:
            c = 0
            for o in self.ops[e]:
                if o.need and not o.dma:
                    c += 1
                    o.sig = c
        with ExitStack() as st:
            sem = {e: st.enter_context(nc.semaphore("s_" + e)) for e in ENGS}
            dsem = {}
            for e in ENGS:
                if self.dmas[e]:
                    for i in range(RDMA):
                        dsem[(e, i)] = st.enter_context(nc.semaphore("d_%s%d" % (e, i)))
            block = st.enter_context(nc.Block())

            def run(ename, eng):
                waited = {}
                for o in self.ops[ename]:
                    for d in o.deps:
                        if d.dma:
                            key, s_, v = d.dk, dsem[d.dk], d.dval
                        else:
                            key, s_, v = d.eng, sem[d.eng], d.sig
                        if waited.get(key, 0) >= v:
                            continue
                        eng.wait_ge(s_, v)
                        waited[key] = v
                    if o.bar:
                        if o.sig is not None:
                            eng.sem_inc(sem[ename], 1)
                        continue
                    ins = o.fn(eng)
                    if o.dma:
                        ins.then_inc(dsem[o.dk], 16)
                    elif o.sig is not None:
                        ins.then_inc(sem[ename], 1)
                if ename == "sp":
                    for q in ENGS:
                        for d in self.dmas[q][-RDMA:]:
                            if waited.get(d.dk, 0) < d.dval:
                                eng.wait_ge(dsem[d.dk], d.dval)
                                waited[d.dk] = d.dval

            @block.sync
            def _(e):
                run("sp", e)

            @block.tensor
            def _(e):
                run("pe", e)

            @block.scalar
            def _(e):
                run("act", e)

            @block.vector
            def _(e):
                run("dve", e)

            @block.gpsimd
            def _(e):
                run("pool", e)


class Ring:
    def __init__(self, items):
        self.items = items
        self.i = 0

    def next(self):
        x = self.items[self.i % len(self.items)]
        self.i += 1
        return x


def _bucket(d):
    n = np.maximum(d, 0)
    nf = np.maximum(n, 1).astype(np.float32)
    large = 16 + (np.log(nf / np.float32(16)) / np.float32(math.log(8.0)) * np.float32(16)).astype(np.int32)
    large = np.minimum(large, 31)
    return np.where(n < 16, n, large)


_CONST = None


def host_consts():
    global _CONST
    if _CONST is not None:
        return _CONST
    c = {}
    k = np.arange(128)
    c["tri"] = (k[:, None] <= k[None, :]).astype(NPBF)
    c["farm"] = (k[:, None] > k[None, :]).astype(NPBF)
    c["antiI"] = (k[:, None] == 127 - k[None, :]).astype(NPBF)
    c["ident"] = np.eye(128, dtype=np.float32)
    t = np.arange(S)
    c["ind"] = (t[None, :] // 64 == np.arange(64)[:, None]).astype(NPBF)
    d = np.arange(384) - 127
    oh = np.zeros((33, 384), np.float32)
    b = _bucket(d)
    for i in range(384):
        oh[32 if d[i] < 0 else b[i], i] = 1.0
    c["ohd"] = oh
    m = np.arange(HCL) - 4111
    oh = np.zeros((33, HCL), np.float32)
    b = _bucket(m)
    oh[np.where(m < 0, 32, b), np.arange(HCL)] = 1.0
    c["ohc"] = oh
    sel = np.zeros((NT, 128, 128), np.float32)
    j = np.arange(64)
    for qt in range(NT):
        q = qt * 128 + k
        cur = q // 64
        forced = (j[None, :] == 0) | (j[None, :] == cur[:, None]) | (j[None, :] == cur[:, None] - 1)
        causal = j[None, :] <= cur[:, None]
        sel[qt, :, :64] = (causal & ~forced)
        sel[qt, :, 64:] = np.where(forced, 1e30, np.where(causal, 0.0, -1e30))
    c["selc"] = sel
    ov = np.zeros((256, 64), np.float32)
    for npr in range(1, 256):
        n = 255 - npr
        lo = np.maximum(16 * n, 64 * j)
        hi = np.minimum(16 * n + 32, 64 * j + 64)
        ov[npr] = np.maximum(0, hi - lo) / 16.0
    c["ovl"] = ov.astype(NPBF)
    inv = (10000.0 ** (-np.arange(0, 32, 2, dtype=np.float32) / 32)).astype(np.float32)
    ang = t.astype(np.float32)[None, :] * inv[:, None]
    cos = np.cos(ang).astype(np.float32)
    sin = np.sin(ang).astype(np.float32)
    cosT = np.concatenate([cos, cos], 0)
    sinT = np.concatenate([-sin, sin], 0)
    rope = np.zeros((96, 4, S), np.float32)
    rope[64:96, 0] = cosT
    rope[64:96, 1] = sinT
    rope[64:96, 2] = cosT * np.float32(SC_M)
    rope[64:96, 3] = sinT * np.float32(SC_M)
    c["rope"] = rope
    _CONST = c
    return c


def build(debug=None):
    nc = bass.Bass("TRN2", target_bir_lowering=False)
    P = Prog(nc)
    es = ExitStack()

    def din(name, shape, dt=F32):
        return nc.dram_tensor(name, list(shape), dt, kind="ExternalInput")

    def dscr(name, shape, dt=F32):
        kind = "ExternalOutput" if (debug and name in debug) else "Internal"
        return nc.dram_tensor(name, list(shape), dt, kind=kind)

    xT = din("xT", [NB, D, S]).ap()
    w_in_d = din("w_in", [D, 1720]).ap()
    w_uq_d = din("w_uq", [256, 768]).ap()
    w_ukv_d = din("w_ukv", [128, 1024]).ap()
    w1k_d = din("w1k", [2048, 128]).ap()
    w1v_d = din("w1v", [2048, 128]).ap()
    w2k_d = din("w2k", [128, 64]).ap()
    w2v_d = din("w2v", [128, 64]).ap()
    posk_d = din("poskT", [64, 32]).ap()
    posv_d = din("posvT", [64, 32]).ap()
    t5_d = din("t5", [32, 8]).ap()
    w_out_d = din("w_out", [D, D]).ap()
    w_gate_d = din("w_gate", [D, DFF]).ap()
    w_up_d = din("w_up", [D, DFF]).ap()
    w_down_d = din("w_down", [DFF, D]).ap()
    gvec_d = din("gvec", [128, 32]).ap()
    gout_d = din("gout", [128, 1024]).ap()
    tri_d = din("tri", [128, 128], BF16).ap()
    farm_d = din("farm", [128, 128], BF16).ap()
    anti_d = din("antiI", [128, 128], BF16).ap()
    ident_d = din("ident", [128, 128]).ap()
    ind_d = din("ind", [64, S], BF16).ap()
    ohd_d = din("ohd", [33, 384]).ap()
    ohc_d = din("ohc", [33, HCL]).ap()
    selc_d = din("selc", [NT, 128, 128]).ap()
    ovl_d = din("ovl", [256, 64], BF16).ap()
    rope_d = din("rope", [96, 4, S]).ap()

    outT_h = nc.dram_tensor("outT", [NB, D, S], F32, kind="ExternalOutput")
    outT = outT_h.ap()

    QMs = dscr("QMs", [8, 96, S], BF16).ap()
    KMs = dscr("KMs", [8, 96, S], BF16).ap()
    VMs = dscr("VMs", [8, 128, NT, 65], BF16).ap()
    QNs = dscr("QNs", [8, 64, S], BF16).ap()
    KSs = dscr("KSs", [2, 64, S], BF16).ap()
    KWs = dscr("KWs", [2, 64, S], BF16).ap()
    VSs = dscr("VSs", [2, 128, NT, 65], BF16).ap()
    VWs = dscr("VWs", [2, 128, NT, 65], BF16).ap()
    NGs = dscr("NGs", [2, 64, S], BF16).ap()
    OMs = dscr("OMs", [S, 512]).ap()
    OCs = dscr("OCs", [S, 512]).ap()
    OSs = dscr("OSs", [S, 512]).ap()
    OWs = dscr("OWs", [S, 512]).ap()
    HTs = dscr("HTs", [NB, D, S]).ap()
    GD_h = dscr("GDs", [8, 384])
    HC_h = dscr("HCs", [8, HCL])
    GDs, HCs = GD_h.ap(), HC_h.ap()
    bQMs, bKMs, bVMs, bQNs = Buf("QMs"), Buf("KMs"), Buf("VMs"), Buf("QNs")
    bKSs, bKWs, bVSs, bVWs, bNGs = Buf("KSs"), Buf("KWs"), Buf("VSs"), Buf("VWs"), Buf("NGs")
    bOMs, bOCs, bOSs, bOWs = Buf("OMs"), Buf("OCs"), Buf("OSs"), Buf("OWs")
    bHTs = [Buf("HT0"), Buf("HT1")]
    bGDs, bHCs = Buf("GDs"), Buf("HCs")
    bOUT = Buf("out")

    uid = [0]

    def sb(stack, name, shape, dt=F32):
        uid[0] += 1
        return stack.enter_context(nc.sbuf_tensor("%s_%d" % (name, uid[0]), list(shape), dt))

    ps = [es.enter_context(nc.psum_tensor("ps%d" % i, [128, 512], F32)) for i in range(8)]
    psb = [Buf("ps%d" % i) for i in range(8)]
    PSR = Ring(list(zip(ps, psb)))

    GV = sb(es, "GV", [128, 32]); bGV = Buf("GV")
    ONES = sb(es, "ONES", [128, 128], BF16); bONES = Buf("ONES")
    IDN = sb(es, "IDN", [128, 128]); bIDN = Buf("IDN")
    P.op("sp", I("dma_start", out=GV[:], in_=gvec_d), w=[bGV], dma=True)
    P.op("sp", I("dma_start", out=IDN[:], in_=ident_d), w=[bIDN], dma=True)
    P.op("pool", I("memset", ONES[:], 1.0), w=[bONES])
    EPSB = sb(es, "EPSB", [128, 1]); bEPSB = Buf("EPSB")
    P.op("pool", I("memset", EPSB[:], EPS), w=[bEPSB])

    def load_cast(stack_stage, dst_ap, src_ap, dstbuf, rows, cols, ring, eng_i, indep=False):
        if indep:
            dstbuf = Buf("chunk")
        stg, sbuf_ = ring.next()
        P.op(("sp", "pool")[eng_i % 2] if indep else "sp", I("dma_start", out=stg[0:rows, 0:cols], in_=src_ap), w=[sbuf_], dma=True)
        eng = ("dve", "act", "pool", "dve", "act")[eng_i % 5]
        if eng == "act":
            P.op(eng, I("activation", out=dst_ap, in_=stg[0:rows, 0:cols], func=AF.Copy), r=[sbuf_], w=[dstbuf])
        else:
            P.op(eng, I("tensor_copy", out=dst_ap, in_=stg[0:rows, 0:cols]), r=[sbuf_], w=[dstbuf])

    A = ExitStack()
    W_in = sb(A, "W_in", [128, 8, 1720], BF16); bW_in = Buf("W_in")
    W_uq = sb(A, "W_uq", [128, 2, 768], BF16); bW_uq = Buf("W_uq")
    W_uqB = sb(A, "W_uqB", [128, 2, 8, 96], BF16); bW_uqB = Buf("W_uqB")
    WkrA = sb(A, "WkrA", [128, 8, 96], BF16); bWkrA = Buf("WkrA")
    WkrB = sb(A, "WkrB", [128, 8, 96], BF16); bWkrB = Buf("WkrB")
    W_ukv = sb(A, "W_ukv", [128, 1024], BF16); bW_ukv = Buf("W_ukv")
    W2 = [sb(A, "W2k", [128, 64], BF16), sb(A, "W2v", [128, 64], BF16)]
    bW2 = [Buf("W2k"), Buf("W2v")]
    POS = [sb(A, "POSk", [64, 32], BF16), sb(A, "POSv", [64, 32], BF16)]
    bPOS = [Buf("POSk"), Buf("POSv")]
    TRI = sb(A, "TRI", [128, 128], BF16); bTRI = Buf("TRI")
    FARM = sb(A, "FARM", [128, 128], BF16); bFARM = Buf("FARM")
    EM = sb(A, "EM", [128, 8, 256], BF16); bEM = Buf("EM")
    T31B = sb(A, "T31B", [128, 8]); bT31B = Buf("T31B")
    OVL = sb(A, "OVL", [128, 2, 64], BF16); bOVL = Buf("OVL")
    GATES = sb(A, "GATES", [128, NT, 24]); bGATES = Buf("GATES")
    KCR = sb(A, "KCR", [128, S], BF16); bKCR = Buf("KCR")
    VCR = sb(A, "VCR", [128, S], BF16); bVCR = Buf("VCR")
    KCT = sb(A, "KCT", [64, 2, 256], BF16); bKCT = Buf("KCT")
    VCA = sb(A, "VCA", [128, 2, 2, 65], BF16); bVCA = Buf("VCA")

    with ExitStack() as SU:
        stg = [(sb(SU, "stg%d" % i, [128, 4096]), Buf("stg%d" % i)) for i in range(2)]
        SR = Ring(stg)
        ei = 0
        wv = w_in_d.rearrange("(c p) n -> p c n", p=128)
        for c in range(8):
            load_cast(SU, W_in[:, c, :], wv[:, c, :], bW_in, 128, 1720, SR, ei); ei += 1
        wv = w_uq_d.rearrange("(c p) n -> p c n", p=128)
        for c in range(2):
            load_cast(SU, W_uq[:, c, :], wv[:, c, :], bW_uq, 128, 768, SR, ei); ei += 1
        load_cast(SU, W_ukv[:, :], w_ukv_d, bW_ukv, 128, 1024, SR, ei); ei += 1
        for kv, (wd, pd) in enumerate(((w2k_d, posk_d), (w2v_d, posv_d))):
            load_cast(SU, W2[kv][:, :], wd, bW2[kv], 128, 64, SR, ei); ei += 1
            load_cast(SU, POS[kv][:, :], pd, bPOS[kv], 64, 32, SR, ei); ei += 1
        P.op("pool", I("memset", W_uqB[:], 0.0), w=[bW_uqB])
        uq4 = W_uq[:, :, :].rearrange("p c (h e) -> p c h e", e=96)
        P.op("pool", I("tensor_copy", out=W_uqB[:, :, :, 64:80], in_=uq4[:, :, :, 80:96]), r=[bW_uq], w=[bW_uqB])
        P.op("pool", I("tensor_copy", out=W_uqB[:, :, :, 80:96], in_=uq4[:, :, :, 64:80]), r=[bW_uq], w=[bW_uqB])
        P.op("pool", I("memset", WkrA[:], 0.0), w=[bWkrA])
        P.op("pool", I("memset", WkrB[:], 0.0), w=[bWkrB])
        P.op("pool", I("tensor_copy", out=WkrA[:, :, 64:96], in_=W_in[:, :, 384:416]), r=[bW_in], w=[bWkrA])
        P.op("pool", I("tensor_copy", out=WkrB[:, :, 64:80], in_=W_in[:, :, 400:416]), r=[bW_in], w=[bWkrB])
        P.op("pool", I("tensor_copy", out=WkrB[:, :, 80:96], in_=W_in[:, :, 384:400]), r=[bW_in], w=[bWkrB])
        P.op("sp", I("dma_start", out=TRI[:], in_=tri_d), w=[bTRI], dma=True)
        P.op("sp", I("dma_start", out=FARM[:], in_=farm_d), w=[bFARM], dma=True)
        P.op("sp", I("dma_start", out=OVL[:], in_=ovl_d.rearrange("(t p) j -> p t j", p=128)), w=[bOVL], dma=True)
        P.op("sp", I("dma_start", out=T31B[:], in_=t5_d[31:32, :].to_broadcast([128, 8])), w=[bT31B], dma=True)
        TBLX = sb(SU, "TBLX", [33, 8]); bTBLX = Buf("TBLX")
        NT31 = sb(SU, "NT31", [8, 1]); bNT31 = Buf("NT31")
        OHD = sb(SU, "OHD", [33, 384]); bOHD = Buf("OHD")
        ANTI = sb(SU, "ANTI", [128, 128], BF16); bANTI = Buf("ANTI")
        P.op("pool", I("memset", TBLX[32:33, :], -BIG), w=[bTBLX])
        P.op("sp", I("dma_start", out=TBLX[0:32, :], in_=t5_d), w=[bTBLX], dma=True)
        P.op("sp", I("dma_start", out=NT31[:], in_=t5_d[31:32, :].rearrange("a h -> h a")), w=[bNT31], dma=True)
        P.op("dve", I("tensor_scalar", out=NT31[:], in0=NT31[:], scalar1=-1.0, scalar2=None, op0=ALU.mult),
             r=[bNT31], w=[bNT31])
        P.op("sp", I("dma_start", out=OHD[:], in_=ohd_d), w=[bOHD], dma=True)
        P.op("sp", I("dma_start", out=ANTI[:], in_=anti_d), w=[bANTI], dma=True)
        pt, pb = PSR.next()
        P.op("pe", I("matmul", pt[0:8, 0:384], lhsT=TBLX[:, :], rhs=OHD[:, :], start=True, stop=True),
             r=[bTBLX, bOHD], w=[pb])
        GT = sb(SU, "GT", [8, 384]); bGT = Buf("GT")
        P.op("act", I("activation", out=GT[:], in_=pt[0:8, 0:384], func=AF.Exp, bias=NT31[:, 0:1], scale=1.0),
             r=[pb, bNT31], w=[bGT])
        P.op("pool", I("dma_start", out=GDs, in_=GT[:]), r=[bGT], w=[bGDs], dma=True)
        EMF = sb(SU, "EMF", [128, 256]); bEMF = Buf("EMF")
        EMFb = sb(SU, "EMFb", [128, 256], BF16); bEMFb = Buf("EMFb")
        for h in range(8):
            hank = bass.AP(tensor=GD_h, offset=h * 384, ap=[[1, 128], [1, 256]])
            P.op("sp", I("dma_start", out=EMF[:], in_=hank), r=[bGDs], w=[bEMF], dma=True)
            P.op("dve", I("tensor_copy", out=EMFb[:], in_=EMF[:]), r=[bEMF], w=[bEMFb])
            pt, pb = PSR.next()
            P.op("pe", I("matmul", pt[:, 0:256], lhsT=ANTI[:, :], rhs=EMFb[:, :], start=True, stop=True),
                 r=[bANTI, bEMFb], w=[pb])
            P.op("act", I("activation", out=EM[:, h, :], in_=pt[:, 0:256], func=AF.Copy), r=[pb], w=[bEM])
        OHC = [(sb(SU, "OHC%d" % i, [33, 512]), Buf("OHC%d" % i)) for i in range(2)]
        HCT = [(sb(SU, "HCT%d" % i, [8, 512]), Buf("HCT%d" % i)) for i in range(2)]
        OR_, HR_ = Ring(OHC), Ring(HCT)
        for ch in range(16):
            n = min(512, HCL - ch * 512)
            ot, ob = OR_.next()
            ht, hb = HR_.next()
            P.op("sp", I("dma_start", out=ot[:, 0:n], in_=ohc_d[:, ch * 512:ch * 512 + n]), w=[ob], dma=True)
            pt, pb = PSR.next()
            P.op("pe", I("matmul", pt[0:8, 0:n], lhsT=TBLX[:, :], rhs=ot[:, 0:n], start=True, stop=True),
                 r=[bTBLX, ob], w=[pb])
            P.op("dve", I("tensor_copy", out=ht[:, 0:n], in_=pt[0:8, 0:n]), r=[pb], w=[hb])
            P.op("pool", I("dma_start", out=HCs[:, ch * 512:ch * 512 + n], in_=ht[:, 0:n]), r=[hb], w=[bHCs], dma=True)
        P.barrier()

    def stats_rstd(stack_tiles, src_sq_aps, nfeat, rbuf_list, RSTD, bRSTD):
        pt, pb = PSR.next()
        n = len(src_sq_aps)
        for i, (ap_, b_) in enumerate(src_sq_aps):
            P.op("pe", I("matmul", pt[:, :], lhsT=ONES[:, :], rhs=ap_, start=(i == 0), stop=(i == n - 1)),
                 r=[bONES, b_], w=[pb])
        P.op("act", I("activation", out=RSTD[:], in_=pt[:, :], func=AF.Sqrt, bias=EPSB[:, 0:1], scale=1.0 / nfeat),
             r=[pb, bEPSB], w=[bRSTD])
        P.op("dve", I("reciprocal", out=RSTD[:], in_=RSTD[:]), r=[bRSTD], w=[bRSTD])

    def phase_P(b):
        with ExitStack() as L:
            XR = [(sb(L, "X%d" % i, [128, 8, 512]), Buf("X%d" % i)) for i in range(2)]
            XNR = [(sb(L, "XN%d" % i, [128, 8, 512], BF16), Buf("XN%d" % i)) for i in range(2)]
            RSR = [(sb(L, "RSTD%d" % i, [128, 512]), Buf("RSTD%d" % i)) for i in range(2)]
            RPR = [(sb(L, "ROPE%d" % i, [96, 2, 512]), Buf("ROPE%d" % i)) for i in range(2)]
            SQ = sb(L, "SQ", [128, 8, 512], BF16); bSQ = Buf("SQ")
            RQ = sb(L, "RQ", [128, 512]); bRQ = Buf("RQ")
            RKV = sb(L, "RKV", [128, 512]); bRKV = Buf("RKV")
            CQf = sb(L, "CQf", [128, 2, 512]); bCQf = Buf("CQf")
            CQs = sb(L, "CQs", [128, 2, 512], BF16); bCQs = Buf("CQs")
            CQN = sb(L, "CQN", [128, 2, 512], BF16); bCQN = Buf("CQN")
            CKf = sb(L, "CKf", [128, 512]); bCKf = Buf("CKf")
            CKs = sb(L, "CKs", [128, 512], BF16); bCKs = Buf("CKs")
            CKN = sb(L, "CKN", [128, 512], BF16); bCKN = Buf("CKN")
            T1 = sb(L, "T1", [96, 512]); bT1 = Buf("T1")
            T2 = sb(L, "T2", [96, 512]); bT2 = Buf("T2")
            KPE = sb(L, "KPE", [96, 512], BF16); bKPE = Buf("KPE")
            QM = sb(L, "QM", [96, 8, 512], BF16); bQM = Buf("QM")
            KM = sb(L, "KM", [96, 8, 512], BF16); bKM = Buf("KM")
            VMR = Ring([(sb(L, "VM%d" % i, [128, 8, 4, 65], BF16), Buf("VM%d" % i)) for i in range(2)])
            QN = sb(L, "QN", [128, 4, 512], BF16); bQN = Buf("QN")
            KSR = Ring([(sb(L, "KS%d" % i, [128, 2, 512], BF16), Buf("KS%d" % i)) for i in range(2)])
            VSR = Ring([(sb(L, "VS%d" % i, [128, 4, 4, 65], BF16), Buf("VS%d" % i)) for i in range(2)])
            for (t_, b_) in VMR.items + VSR.items:
                P.op("pool", I("memset", t_[:], 1.0), w=[b_])
            xv = xT[b].rearrange("(c p) t -> p c t", p=128)

            def chain1(tb):
                t0 = tb * 512
                X, bX = XR[tb % 2]
                RSTD, bRSTD = RSR[tb % 2]
                ROPE, bROPE = RPR[tb % 2]
                P.op("sp", I("dma_start", out=X[:], in_=xv[:, :, t0:t0 + 512]), w=[bX], dma=True)
                P.op("sp", I("dma_start", out=ROPE[64:96, :, :], in_=rope_d[64:96, 0:2, t0:t0 + 512]), w=[bROPE], dma=True)
                P.op("act", I("activation", out=SQ[:], in_=X[:], func=AF.Square), r=[bX], w=[bSQ])
                stats_rstd(None, [(SQ[:, c, :], bSQ) for c in range(8)], 1024.0, None, RSTD, bRSTD)

            def chain2(tb):
                X, bX = XR[tb % 2]
                XN, bXN = XNR[tb % 2]
                RSTD, bRSTD = RSR[tb % 2]
                for c in range(8):
                    P.op("dve", I("scalar_tensor_tensor", out=XN[:, c, :], in0=X[:, c, :], scalar=GV[:, c:c + 1],
                                  in1=RSTD[:], op0=ALU.mult, op1=ALU.mult), r=[bX, bGV, bRSTD], w=[bXN])

            def body(tb, part):
                t0 = tb * 512
                XN, bXN = XNR[tb % 2]
                ROPE, bROPE = RPR[tb % 2]

                def proj(cols, M, wt=W_in, wb=bW_in):
                    pt, pb = PSR.next()
                    for c in range(8):
                        P.op("pe", I("matmul", pt[0:M, :], lhsT=wt[:, c, cols[0]:cols[1]], rhs=XN[:, c, :],
                                     start=(c == 0), stop=(c == 7)), r=[wb, bXN], w=[pb])
                    return pt, pb

                if part == 0:
                    for j in range(2):
                        pt, pb = proj((j * 128, (j + 1) * 128), 128)
                        P.op("act", I("activation", out=CQf[:, j, :], in_=pt[:, :], func=AF.Copy), r=[pb], w=[bCQf])
                        P.op("act", I("activation", out=CQs[:, j, :], in_=pt[:, :], func=AF.Square), r=[pb], w=[bCQs])
                    pt, pb = proj((256, 384), 128)
                    P.op("act", I("activation", out=CKf[:], in_=pt[:, :], func=AF.Copy), r=[pb], w=[bCKf])
                    P.op("act", I("activation", out=CKs[:], in_=pt[:, :], func=AF.Square), r=[pb], w=[bCKs])
                    pt, pb = proj((928, 1056), 128)
                    P.op("act", I("activation", out=KCR[:, t0:t0 + 512], in_=pt[:, :], func=AF.Copy), r=[pb], w=[bKCR])
                    pt, pb = proj((1056, 1184), 128)
                    P.op("dve", I("tensor_copy", out=VCR[:, t0:t0 + 512], in_=pt[:, :]), r=[pb], w=[bVCR])
                    stats_rstd(None, [(CQs[:, j, :], bCQs) for j in range(2)], 256.0, None, RQ, bRQ)
                    for j in range(2):
                        P.op("dve", I("scalar_tensor_tensor", out=CQN[:, j, :], in0=CQf[:, j, :], scalar=GV[:, 24 + j:25 + j],
                                      in1=RQ[:], op0=ALU.mult, op1=ALU.mult), r=[bCQf, bGV, bRQ], w=[bCQN])
                    stats_rstd(None, [(CKs[:], bCKs)], 128.0, None, RKV, bRKV)
                    P.op("dve", I("scalar_tensor_tensor", out=CKN[:], in0=CKf[:], scalar=GV[:, 26:27],
                                  in1=RKV[:], op0=ALU.mult, op1=ALU.mult), r=[bCKf, bGV, bRKV], w=[bCKN])
                    for j in range(4):
                        pt, pb = proj((416 + j * 128, 416 + (j + 1) * 128), 128)
                        if j % 2 == 0:
                            P.op("act", I("activation", out=QN[:, j, :], in_=pt[:, :], func=AF.Copy, scale=0.125),
                                 r=[pb], w=[bQN])
                        else:
                            P.op("dve", I("tensor_scalar", out=QN[:, j, :], in0=pt[:, :], scalar1=0.125, scalar2=None,
                                          op0=ALU.mult), r=[pb], w=[bQN])
                    P.op("pool", I("dma_start", out=QNs.rearrange("(j two) d t -> (two d) j t", two=2)[:, :, t0:t0 + 512],
                                   in_=QN[:]), r=[bQN], w=[bQNs], dma=True)
                    KS, bKS = KSR.next()
                    for i, c0 in enumerate((1184, 1440)):
                        pt, pb = proj((c0, c0 + 128), 128)
                        if i % 2 == 0:
                            P.op("act", I("activation", out=KS[:, i, :], in_=pt[:, :], func=AF.Copy), r=[pb], w=[bKS])
                        else:
                            P.op("dve", I("tensor_copy", out=KS[:, i, :], in_=pt[:, :]), r=[pb], w=[bKS])
                    P.op("pool", I("dma_start", out=KSs.rearrange("k d t -> (k d) t")[:, t0:t0 + 512], in_=KS[:, 0, :]),
                         r=[bKS], w=[bKSs], dma=True)
                    P.op("pool", I("dma_start", out=KWs.rearrange("k d t -> (k d) t")[:, t0:t0 + 512], in_=KS[:, 1, :]),
                         r=[bKS], w=[bKWs], dma=True)
                    return
                pa, pab = proj((0, 96), 96, wt=WkrA, wb=bWkrA)
                pbb_, pbbb = proj((0, 96), 96, wt=WkrB, wb=bWkrB)
                P.op("dve", I("tensor_tensor", out=T1[64:96, :], in0=pa[64:96, :], in1=ROPE[64:96, 0, :], op=ALU.mult),
                     r=[pab, bROPE], w=[bT1])
                P.op("dve", I("tensor_tensor", out=T2[64:96, :], in0=pbb_[64:96, :], in1=ROPE[64:96, 1, :], op=ALU.mult),
                     r=[pbbb, bROPE], w=[bT2])
                P.op("dve", I("tensor_tensor", out=KPE[64:96, :], in0=T1[64:96, :], in1=T2[64:96, :], op=ALU.add),
                     r=[bT1, bT2], w=[bKPE])
                for h in range(8):
                    pa, pab = PSR.next()
                    for j in range(2):
                        P.op("pe", I("matmul", pa[0:96, :], lhsT=W_uq[:, j, h * 96:(h + 1) * 96], rhs=CQN[:, j, :],
                                     start=(j == 0), stop=(j == 1)), r=[bW_uq, bCQN], w=[pab])
                    pq, pqb = PSR.next()
                    for j in range(2):
                        P.op("pe", I("matmul", pq[0:96, :], lhsT=W_uqB[:, j, h, :], rhs=CQN[:, j, :],
                                     start=(j == 0), stop=(j == 1)), r=[bW_uqB, bCQN], w=[pqb])
                    P.op("act", I("activation", out=QM[0:64, h, :], in_=pa[0:64, :], func=AF.Copy, scale=SC_M),
                         r=[pab], w=[bQM])
                    P.op("dve", I("scalar_tensor_tensor", out=T1[64:96, :], in0=pa[64:96, :], scalar=SC_M,
                                  in1=ROPE[64:96, 0, :], op0=ALU.mult, op1=ALU.mult), r=[pab, bROPE], w=[bT1])
                    P.op("dve", I("scalar_tensor_tensor", out=T2[64:96, :], in0=pq[64:96, :], scalar=SC_M,
                                  in1=ROPE[64:96, 1, :], op0=ALU.mult, op1=ALU.mult), r=[pqb, bROPE], w=[bT2])
                    P.op("dve", I("tensor_tensor", out=QM[64:96, h, :], in0=T1[64:96, :], in1=T2[64:96, :], op=ALU.add),
                         r=[bT1, bT2], w=[bQM])
                    pk, pkb = PSR.next()
                    P.op("pe", I("matmul", pk[:, :], lhsT=W_ukv[:, h * 128:(h + 1) * 128], rhs=CKN[:, :],
                                 start=True, stop=True), r=[bW_ukv, bCKN], w=[pkb])
                    P.op("act", I("activation", out=KM[0:64, h, :], in_=pk[0:64, :], func=AF.Copy), r=[pkb], w=[bKM])
                    P.op("pool", I("tensor_copy", out=KM[64:96, h, :], in_=KPE[64:96, :]), r=[bKPE], w=[bKM])
                P.op("pool", I("dma_start", out=QMs.rearrange("h d t -> d h t")[:, :, t0:t0 + 512], in_=QM[:]),
                     r=[bQM], w=[bQMs], dma=True)
                P.op("pool", I("dma_start", out=KMs.rearrange("h d t -> d h t")[:, :, t0:t0 + 512], in_=KM[:]),
                     r=[bKM], w=[bKMs], dma=True)
                VM, bVM = VMR.next()
                wv4 = W_ukv[:, :].rearrange("p (h e) -> p h e", e=128)
                for ts in range(4):
                    pt, pb = PSR.next()
                    P.op("pe", I("matmul", pt[:, :], lhsT=CKN[:, ts * 128:(ts + 1) * 128], rhs=wv4[:, :, 64:128],
                                 start=True, stop=True), r=[bCKN, bW_ukv], w=[pb])
                    P.op("act", I("activation", out=VM[:, :, ts, 0:64], in_=pt[:, :].rearrange("p (h e) -> p h e", e=64),
                                  func=AF.Copy), r=[pb], w=[bVM])
                for h in range(8):
                    P.op("pool", I("dma_start", out=VMs[h, :, tb * 4:tb * 4 + 4, :], in_=VM[:, h, :, :]),
                         r=[bVM], w=[bVMs], dma=True)
                VS, bVS = VSR.next()
                for ts in range(4):
                    pt, pb = PSR.next()
                    for c in range(8):
                        P.op("pe", I("matmul", pt[:, 0:128], lhsT=XN[:, c, ts * 128:(ts + 1) * 128], rhs=W_in[:, c, 1312:1440],
                                     start=(c == 0), stop=(c == 7)), r=[bXN, bW_in], w=[pb])
                    P.op("act", I("activation", out=VS[:, 0:2, ts, 0:64],
                                  in_=pt[:, 0:128].rearrange("p (k e) -> p k e", e=64), func=AF.Copy), r=[pb], w=[bVS])
                    pt, pb = PSR.next()
                    for c in range(8):
                        P.op("pe", I("matmul", pt[:, 0:152], lhsT=XN[:, c, ts * 128:(ts + 1) * 128], rhs=W_in[:, c, 1568:1720],
                                     start=(c == 0), stop=(c == 7)), r=[bXN, bW_in], w=[pb])
                    P.op("dve", I("tensor_copy", out=VS[:, 2:4, ts, 0:64],
                                  in_=pt[:, 0:128].rearrange("p (k e) -> p k e", e=64)), r=[pb], w=[bVS])
                    P.op("act", I("activation", out=GATES[:, tb * 4 + ts, :], in_=pt[:, 128:152], func=AF.Sigmoid),
                         r=[pb], w=[bGATES])
                for k_ in range(2):
                    P.op("pool", I("dma_start", out=VSs[k_, :, tb * 4:tb * 4 + 4, :], in_=VS[:, k_, :, :]),
                         r=[bVS], w=[bVSs], dma=True)
                    P.op("pool", I("dma_start", out=VWs[k_, :, tb * 4:tb * 4 + 4, :], in_=VS[:, 2 + k_, :, :]),
                         r=[bVS], w=[bVWs], dma=True)

            chain1(0)
            chain2(0)
            for tb in range(NTB):
                body(tb, 0)
                if tb + 1 < NTB:
                    chain1(tb + 1)
                body(tb, 1)
                if tb + 1 < NTB:
                    chain2(tb + 1)
            P.barrier()

    def phase_C():
        with ExitStack() as L:
            BIA = sb(L, "BIA", [128, 1]); bBIA = Buf("BIA")
            Hf = sb(L, "Hf", [128, 256]); bHf = Buf("Hf")
            H2 = sb(L, "H2", [128, 256]); bH2 = Buf("H2")
            SG = sb(L, "SG", [128, 256]); bSG = Buf("SG")
            GH = sb(L, "GH", [128, 256], BF16); bGH = Buf("GH")
            W1 = [sb(L, "W1k", [128, 32, 128], BF16), sb(L, "W1v", [128, 32, 128], BF16)]
            bW1 = [Buf("W1k"), Buf("W1v")]
            stg_ = sb(L, "cstg", [128, 4096]); sbuf_ = Buf("cstg")
            sv = stg_[:, :].rearrange("p (l h) -> p l h", h=128)
            for kv, wd in enumerate((w1k_d, w1v_d)):
                src = wd.rearrange("(l d) h -> d l h", d=64)
                for half in range(2):
                    P.op("sp", I("dma_start", out=sv[half * 64:(half + 1) * 64, :, :], in_=src), w=[sbuf_], dma=True)
                P.op("dve", I("tensor_copy", out=W1[kv][:, :, :], in_=sv[:, :, :]), r=[sbuf_], w=[bW1[kv]])
            P.op("pool", I("memset", VCA[:], 0.0), w=[bVCA])
            P.op("pool", I("memset", KCT[:], 0.0), w=[bKCT])
            for kv in range(2):
                RAW, bRAW = (KCR, bKCR) if kv == 0 else (VCR, bVCR)
                pbia, pbiab = PSR.next()
                for l in range(32):
                    P.op("pe", I("matmul", pbia[:, 0:1], lhsT=W1[kv][0:64, l, :], rhs=POS[kv][0:64, l:l + 1],
                                 start=(l == 0), stop=(l == 31)), r=[bW1[kv], bPOS[kv]], w=[pbiab])
                P.op("dve", I("tensor_copy", out=BIA[:], in_=pbia[:, 0:1]), r=[pbiab], w=[bBIA])
                for kh in range(2):
                    p0 = kh * 64
                    pt, pb = PSR.next()
                    for l in range(32):
                        P.op("pe", I("matmul", pt[:, 0:255], lhsT=W1[kv][p0:p0 + 64, l, :],
                                     rhs=RAW[p0:p0 + 64, l:l + 16 * 254 + 1:16], start=(l == 0), stop=(l == 31)),
                             r=[bW1[kv], bRAW], w=[pb])
                    P.op("act", I("activation", out=Hf[:, 0:255], in_=pt[:, 0:255], func=AF.Identity, bias=BIA[:, 0:1],
                                  scale=1.0), r=[pb, bBIA], w=[bHf])
                    P.op("dve", I("tensor_tensor", out=H2[:, 0:255], in0=Hf[:, 0:255], in1=Hf[:, 0:255], op=ALU.mult),
                         r=[bHf], w=[bH2])
                    P.op("dve", I("tensor_scalar", out=H2[:, 0:255], in0=H2[:, 0:255], scalar1=0.044715, scalar2=1.0,
                                  op0=ALU.mult, op1=ALU.add), r=[bH2], w=[bH2])
                    P.op("dve", I("tensor_tensor", out=H2[:, 0:255], in0=H2[:, 0:255], in1=Hf[:, 0:255], op=ALU.mult),
                         r=[bH2, bHf], w=[bH2])
                    P.op("act", I("activation", out=SG[:, 0:255], in_=H2[:, 0:255], func=AF.Sigmoid,
                                  scale=2.0 * math.sqrt(2.0 / math.pi)), r=[bH2], w=[bSG])
                    P.op("pool", I("memset", GH[:], 0.0), w=[bGH])
                    rev = bass.AP(tensor=GH, offset=GH[:, 255:256].offset, ap=[list(GH[:].ap[0]), [-1, 255]])
                    P.op("dve", I("tensor_tensor", out=rev, in0=SG[:, 0:255], in1=Hf[:, 0:255], op=ALU.mult),
                         r=[bSG, bHf], w=[bGH])
                    if kv == 0:
                        pt, pb = PSR.next()
                        P.op("pe", I("matmul", pt[0:64, 0:256], lhsT=W2[0][:, :], rhs=GH[:, :], start=True, stop=True),
                             r=[bW2[0], bGH], w=[pb])
                        P.op("act", I("activation", out=KCT[:, kh, :], in_=pt[0:64, 0:256], func=AF.Copy),
                             r=[pb], w=[bKCT])
                    else:
                        for nt in range(2):
                            pt, pb = PSR.next()
                            P.op("pe", I("matmul", pt[:, 0:64], lhsT=GH[:, nt * 128:(nt + 1) * 128], rhs=W2[1][:, :],
                                         start=True, stop=True), r=[bW2[1], bGH], w=[pb])
                            P.op("act", I("activation", out=VCA[:, kh, nt, 0:64], in_=pt[:, 0:64], func=AF.Copy),
                                 r=[pb], w=[bVCA])
            P.op("pool", I("memset", VCA[:, :, 1, 64:65], 1.0), w=[bVCA])
            P.op("pool", I("memset", VCA[:, :, 0, 64:65], 1.0), w=[bVCA])
            P.op("pool", I("memset", VCA[0:1, :, 0, :], 0.0), w=[bVCA])
            P.barrier()

    def run_attention(L, jobs):
        SR_ = Ring([(ps[i], psb[i]) for i in range(0, 4)])
        OR_ = Ring([(ps[i], psb[i]) for i in range(4, 6)])
        PTR = Ring([(sb(L, "PT%d" % i, [128, 512], BF16), Buf("PT%d" % i)) for i in range(6)])
        flat = []
        for ji, job in enumerate(jobs):
            nt_ = len(job["tiles"])
            job["ji"] = ji
            for ti in range(nt_):
                flat.append((job, ti, ti == 0, ti == nt_ - 1))
        state = {}
        loaded = set()
        LOOK = 24

        def stage1(i):
            job, ti, first, last = flat[i]
            for k2 in range(i, min(len(flat), i + LOOK)):
                j2 = flat[k2][0]
                if j2["ji"] > job["ji"] + 3:
                    break
                if id(j2) not in loaded:
                    loaded.add(id(j2))
                    if j2["load"] is not None:
                        j2["load"]()
            tl = job["tiles"][ti]
            kT, kb, vA, vb, lo, hi, masks = tl
            qap, qb_ = job["q"]
            st_, sbf = SR_.next()
            ptile, pbf = PTR.next()
            c0, c1 = lo * 128, hi * 128
            P.op("pe", I("matmul", st_[:, c0:c1], lhsT=kT, rhs=qap[:, c0:c1], start=True, stop=True),
                 r=[kb, qb_], w=[sbf])
            P.op("act", I("activation", out=ptile[:, c0:c1], in_=st_[:, c0:c1], func=AF.Exp, bias=job["bias"], scale=1.0),
                 r=[sbf] + job["rbias"], w=[pbf])
            for (qs, map_, mb) in masks:
                P.op("dve", I("tensor_tensor", out=ptile[:, qs * 128:(qs + 1) * 128], in0=ptile[:, qs * 128:(qs + 1) * 128],
                              in1=map_, op=ALU.mult), r=[pbf, mb], w=[pbf])
            state[i] = (ptile, pbf)

        OSR = Ring([(sb(L, "OSb%d" % i, [128, 512]), Buf("OSb%d" % i)) for i in range(2)])
        for (t_, b_) in OSR.items:
            P.op("pool", I("memset", t_[:], 0.0), w=[b_])
        TR_ = Ring([(ps[i], psb[i]) for i in range(6, 7)])
        DUMW = sb(L, "DUMW", [128, 512], BF16); bDUMW = Buf("DUMW")
        P.op("pool", I("memset", DUMW[:], 0.5), w=[bDUMW])
        bDUM = Buf("DUM")
        pending = []

        def stage2(i):
            job, ti, first, last = flat[i]
            kT, kb, vA, vb, lo, hi, masks = job["tiles"][ti]
            ptile, pbf = state.pop(i)
            if first:
                job["O"] = OR_.next()
                job["started"] = False
            O, Ob = job["O"]
            c0, c1 = lo * 128, hi * 128
            if WARM_N:
                P.op("pe", I("matmul", ps[7][:, 0:WARM_N], lhsT=ONES[:, :], rhs=DUMW[:, 0:WARM_N], start=True, stop=True),
                     r=[bONES, bDUMW], w=[bDUM])
            P.op("pe", I("matmul", O[0:65, c0:c1], lhsT=vA, rhs=ptile[:, c0:c1],
                         start=(not job["started"]), stop=last, skip_group_check=True), r=[pbf, vb], w=[Ob])
            job["started"] = True
            if last:
                OS_, bOS_ = OSR.next()
                P.op("dve", I("tensor_copy", out=OS_[0:65, :], in_=O[0:65, :]), r=[Ob], w=[bOS_])

                def fin(job=job, OS_=OS_, bOS_=bOS_):
                    Tt, Tb = TR_.next()
                    Tv = Tt[:, :].rearrange("p (s e) -> p s e", e=128)
                    for qs in range(4):
                        P.op("pe", I("transpose", out=Tv[:, qs, :], in_=OS_[:, qs * 128:(qs + 1) * 128],
                                     identity=IDN[:, :]), r=[bOS_, bIDN], w=[Tb])
                    job["evac"](Tv, Tb)
                pending.append((i + LAG + 2, fin))

        n = len(flat)
        for i in range(n + LAG):
            if i < n:
                stage1(i)
            if i >= LAG:
                stage2(i - LAG)
            while pending and pending[0][0] <= i:
                pending.pop(0)[1]()
        while pending:
            pending.pop(0)[1]()

    def std_evac(L, name):
        RR = Ring([(sb(L, name + "R%d" % i, [128, 4]), Buf(name + "R%d" % i)) for i in range(2)])
        OTR = Ring([(sb(L, name + "OT%d" % i, [128, 4, 64]), Buf(name + "OT%d" % i)) for i in range(2)])

        def mk(dst, dbuf, h, qb, gate_col):
            def evac(Ov, Ob):
                R, bR = RR.next()
                OT, bOT = OTR.next()
                P.op("dve", I("reciprocal", out=R[:, :], in_=Ov[:, :, 64]), r=[Ob], w=[bR])
                for qs in range(4):
                    if gate_col is None:
                        P.op("dve", I("tensor_scalar", out=OT[:, qs, :], in0=Ov[:, qs, 0:64], scalar1=R[:, qs:qs + 1],
                                      scalar2=None, op0=ALU.mult), r=[Ob, bR], w=[bOT])
                    else:
                        g = GATES[:, qb * 4 + qs, gate_col:gate_col + 1]
                        P.op("dve", I("tensor_scalar", out=OT[:, qs, :], in0=Ov[:, qs, 0:64], scalar1=R[:, qs:qs + 1],
                                      scalar2=g, op0=ALU.mult, op1=ALU.mult), r=[Ob, bR, bGATES], w=[bOT])
                dv = dst.rearrange("(t p) c -> p t c", p=128)[:, qb * 4:qb * 4 + 4, h * 64:(h + 1) * 64]
                P.op("pool", I("dma_start", out=dv, in_=OT[:]), r=[bOT], w=[dbuf], dma=True)
            return evac
        return mk

    def phase_MLA():
        with ExitStack() as L:
            KR = Ring([(sb(L, "K%d" % i, [96, S], BF16), Buf("K%d" % i)) for i in range(2)])
            VR = Ring([(sb(L, "V%d" % i, [128, NT, 65], BF16), Buf("V%d" % i)) for i in range(2)])
            QR = Ring([(sb(L, "Q%d" % i, [96, 512], BF16), Buf("Q%d" % i)) for i in range(6)])
            mk = std_evac(L, "m")
            jobs = []
            for h in range(8):
                kv = {}
                for qb in range(NTB):
                    job = {}

                    def load(h=h, qb=qb, job=job, kv=kv):
                        if qb == 0:
                            kv["K"] = KR.next()
                            kv["V"] = VR.next()
                            P.op("sp", I("dma_start", out=kv["K"][0][:], in_=KMs[h]), r=[bKMs], w=[kv["K"][1]], dma=True)
                            P.op("sp", I("dma_start", out=kv["V"][0][:], in_=VMs[h]), r=[bVMs], w=[kv["V"][1]], dma=True)
                        Q, bQ = QR.next()
                        P.op("sp", I("dma_start", out=Q[:], in_=QMs[h, :, qb * 512:(qb + 1) * 512]), r=[bQMs], w=[bQ], dma=True)
                        job["q"] = (Q, bQ)
                        K, bK = kv["K"]
                        V, bV = kv["V"]
                        tiles = []
                        for kt in range(4 * qb + 4):
                            lo = max(0, kt - 4 * qb)
                            masks = [(lo, TRI[:, :], bTRI)] if kt >= 4 * qb else []
                            tiles.append((K[:, kt * 128:(kt + 1) * 128], bK, V[:, kt, :], bV, lo, 4, masks))
                        job["tiles"][:] = tiles
                    job["load"] = load
                    job["tiles"] = [None] * (4 * qb + 4)
                    job["bias"] = 0.0
                    job["rbias"] = []
                    job["evac"] = mk(OMs, bOMs, h, qb, None)
                    jobs.append(job)
            run_attention(L, jobs)
            P.barrier()

    def phase_NC():
        with ExitStack() as L:
            QR = Ring([(sb(L, "cQ%d" % i, [64, 512], BF16), Buf("cQ%d" % i)) for i in range(4)])
            BCR = Ring([(sb(L, "BC%d" % i, [128, 512]), Buf("BC%d" % i)) for i in range(4)])
            SSR = Ring([(sb(L, "SS%d" % i, [128, 512]), Buf("SS%d" % i)) for i in range(3)])
            ER = Ring([(sb(L, "E%d" % i, [128, 512], BF16), Buf("E%d" % i)) for i in range(5)])
            RR = Ring([(sb(L, "cR%d" % i, [128, 4]), Buf("cR%d" % i)) for i in range(2)])
            OTR = Ring([(sb(L, "cOT%d" % i, [128, 4, 64]), Buf("cOT%d" % i)) for i in range(2)])
            IMP = sb(L, "IMP", [128, 4, 64]); bIMP = Buf("IMP")
            SELC = Ring([(sb(L, "SELC%d" % i, [128, 4, 128]), Buf("SELC%d" % i)) for i in range(2)])
            M8 = sb(L, "M8", [128, 4, 16]); bM8 = Buf("M8")
            TMPR = Ring([(sb(L, "TMP%d" % i, [128, 4, 64]), Buf("TMP%d" % i)) for i in range(2)])
            NGT = Ring([(sb(L, "NGT%d" % i, [64, 512], BF16), Buf("NGT%d" % i)) for i in range(2)])
            S_R = Ring([(ps[i], psb[i]) for i in range(0, 3)])
            O_R = Ring([(ps[i], psb[i]) for i in range(3, 5)])
            I_R = Ring([(ps[i], psb[i]) for i in range(5, 7)])
            T_R = Ring([(ps[7], psb[7])])
            units = []
            for kvh in range(2):
                for qb in range(NTB):
                    for g in range(4):
                        units.append((kvh, qb, g))
            ctx = {}

            def stageA(u):
                kvh, qb, g = units[u]
                h = kvh * 4 + g
                nts = [1] if qb < 4 else [0, 1]
                if g == 0:
                    SC_, bSC = SELC.next()
                    P.op("sp", I("dma_start", out=SC_[:], in_=selc_d[qb * 4:qb * 4 + 4].rearrange("t p c -> p t c")),
                         w=[bSC], dma=True)
                    ctx[(kvh, qb)] = (SC_, bSC)
                Q, bQ = QR.next()
                P.op("sp", I("dma_start", out=Q[:], in_=QNs[h, :, qb * 512:(qb + 1) * 512]), r=[bQNs], w=[bQ], dma=True)
                es_ = []
                for ni, nt in enumerate(nts):
                    BC, bBC = BCR.next()
                    src = bass.AP(tensor=HC_h, offset=h * HCL + 2048 * nt + 512 * qb, ap=[[16, 128], [1, 512]])
                    P.op("sp", I("dma_start", out=BC[:], in_=src), r=[bHCs], w=[bBC], dma=True)
                    St, Sb = S_R.next()
                    P.op("pe", I("matmul", St[:, :], lhsT=KCT[:, kvh, nt * 128:(nt + 1) * 128], rhs=Q[:, :],
                                 start=True, stop=True), r=[bKCT, bQ], w=[Sb])
                    SS, bSS = SSR.next()
                    P.op("dve", I("tensor_tensor", out=SS[:], in0=St[:, :], in1=BC[:], op=ALU.add),
                         r=[Sb, bBC], w=[bSS])
                    E, bE = ER.next()
                    P.op("act", I("activation", out=E[:], in_=SS[:], func=AF.Exp), r=[bSS], w=[bE])
                    es_.append((nt, E, bE))
                ctx[u] = es_

            def stageB(u):
                kvh, qb, g = units[u]
                h = kvh * 4 + g
                es_ = ctx.pop(u)
                O, Ob = O_R.next()
                Im, Imb = I_R.next()
                Ov = O[:, 0:260].rearrange("p (s e) -> p s e", e=65)
                Iv = Im[:, 0:256].rearrange("p (s e) -> p s e", e=64)
                first = True
                for ni, (nt, E, bE) in enumerate(es_):
                    for qs in range(4):
                        P.op("pe", I("matmul", Ov[:, qs, :], lhsT=E[:, qs * 128:(qs + 1) * 128], rhs=VCA[:, kvh, nt, :],
                                     start=first, stop=(ni == len(es_) - 1), skip_group_check=True),
                             r=[bE, bVCA], w=[Ob])
                        P.op("pe", I("matmul", Iv[:, qs, :], lhsT=E[:, qs * 128:(qs + 1) * 128], rhs=OVL[:, nt, :],
                                     start=first, stop=(ni == len(es_) - 1), skip_group_check=True),
                             r=[bE, bOVL], w=[Imb])
                        first = False
                R, bR = RR.next()
                OT, bOT = OTR.next()
                P.op("dve", I("tensor_scalar", out=R[:, :], in0=Ov[:, :, 64], scalar1=1e-30, scalar2=None,
                              op0=ALU.add), r=[Ob], w=[bR])
                P.op("dve", I("reciprocal", out=R[:, :], in_=R[:, :]), r=[bR], w=[bR])
                for qs in range(4):
                    gcol = GATES[:, qb * 4 + qs, 3 * h:3 * h + 1]
                    P.op("dve", I("tensor_scalar", out=OT[:, qs, :], in0=Ov[:, qs, 0:64], scalar1=R[:, qs:qs + 1],
                                  scalar2=gcol, op0=ALU.mult, op1=ALU.mult), r=[Ob, bR, bGATES], w=[bOT])
                    if g == 0:
                        P.op("dve", I("tensor_scalar", out=IMP[:, qs, :], in0=Iv[:, qs, :], scalar1=R[:, qs:qs + 1],
                                      scalar2=None, op0=ALU.mult), r=[Imb, bR], w=[bIMP])
                    else:
                        P.op("dve", I("scalar_tensor_tensor", out=IMP[:, qs, :], in0=Iv[:, qs, :],
                                      scalar=R[:, qs:qs + 1], in1=IMP[:, qs, :], op0=ALU.mult, op1=ALU.add),
                             r=[Imb, bR, bIMP], w=[bIMP])
                dv = OCs.rearrange("(t p) c -> p t c", p=128)[:, qb * 4:qb * 4 + 4, h * 64:(h + 1) * 64]
                P.op("pool", I("dma_start", out=dv, in_=OT[:]), r=[bOT], w=[bOCs], dma=True)
                if g == 3:
                    SC_, bSC = ctx.pop((kvh, qb))
                    P.op("dve", I("tensor_tensor", out=IMP[:], in0=IMP[:], in1=SC_[:, :, 0:64], op=ALU.mult),
                         r=[bIMP, bSC], w=[bIMP])
                    P.op("dve", I("tensor_tensor", out=IMP[:], in0=IMP[:], in1=SC_[:, :, 64:128], op=ALU.add),
                         r=[bIMP, bSC], w=[bIMP])
                    TMP, bTMP = TMPR.next()
                    for qs in range(4):
                        P.op("dve", I("max", out=M8[:, qs, 0:8], in_=IMP[:, qs, :]), r=[bIMP], w=[bM8])
                        P.op("dve", I("match_replace", out=TMP[:, qs, :], in_to_replace=M8[:, qs, 0:8], in_values=IMP[:, qs, :],
                                      imm_value=-3.0e38), r=[bM8, bIMP], w=[bTMP])
                        P.op("dve", I("max", out=M8[:, qs, 8:16], in_=TMP[:, qs, :]), r=[bTMP], w=[bM8])
                        P.op("dve", I("tensor_scalar", out=TMP[:, qs, :], in0=IMP[:, qs, :], scalar1=M8[:, qs, 15:16],
                                      scalar2=1.0, op0=ALU.is_ge, op1=ALU.subtract), r=[bIMP, bM8], w=[bTMP])

                    def topk_tail(kvh=kvh, qb=qb, TMP=TMP, bTMP=bTMP):
                        NG, bNG = NGT.next()
                        for qs in range(4):
                            Tt, Tb = T_R.next()
                            P.op("pe", I("transpose", out=Tt[0:64, 0:128], in_=TMP[:, qs, :], identity=IDN[:, :]),
                                 r=[bTMP, bIDN], w=[Tb])
                            P.op("act", I("activation", out=NG[:, qs * 128:(qs + 1) * 128], in_=Tt[0:64, 0:128], func=AF.Copy,
                                          scale=BIG), r=[Tb], w=[bNG])
                        P.op("pool", I("dma_start", out=NGs[kvh, :, qb * 512:(qb + 1) * 512], in_=NG[:]), r=[bNG], w=[bNGs], dma=True)
                    return topk_tail
                return None

            nu = len(units)
            tail = None
            for u in range(nu + 1):
                if u < nu:
                    stageA(u)
                if tail is not None:
                    tail()
                    tail = None
                if u >= 1:
                    tail = stageB(u - 1)
            if tail is not None:
                tail()
            P.barrier()

    def phase_NSW():
        with ExitStack() as L:
            KSA = Ring([(sb(L, "KSA%d" % i, [128, S], BF16), Buf("KSA%d" % i)) for i in range(2)])
            KWR = Ring([(sb(L, "KW%d" % i, [128, S], BF16), Buf("KW%d" % i)) for i in range(2)])
            for (t_, b_) in KWR.items:
                P.op("pool", I("memset", t_[64:128, :], 0.0), w=[b_])
            VSR = Ring([(sb(L, "VSa%d" % i, [128, NT, 65], BF16), Buf("VSa%d" % i)) for i in range(2)])
            VWR = Ring([(sb(L, "VWa%d" % i, [128, NT, 65], BF16), Buf("VWa%d" % i)) for i in range(2)])
            QR = Ring([(sb(L, "QA%d" % i, [128, 512], BF16), Buf("QA%d" % i)) for i in range(6)])
            mks = std_evac(L, "s")
            mkw = std_evac(L, "w")
            jobs = []
            for kvh in range(2):
                kv = {}
                for g in range(4):
                    h = kvh * 4 + g
                    for qb in range(NTB):
                        js, jw = {}, {}

                        def load(h=h, g=g, kvh=kvh, qb=qb, js=js, jw=jw, kv=kv):
                            if g == 0 and qb == 0:
                                kv["KS"], kv["KW"], kv["VS"], kv["VW"] = KSA.next(), KWR.next(), VSR.next(), VWR.next()
                                P.op("sp", I("dma_start", out=kv["KS"][0][0:64, :], in_=KSs[kvh]), r=[bKSs], w=[kv["KS"][1]], dma=True)
                                P.op("sp", I("dma_start", out=kv["KS"][0][64:128, :], in_=ind_d), w=[kv["KS"][1]], dma=True)
                                P.op("sp", I("dma_start", out=kv["KW"][0][0:64, :], in_=KWs[kvh]), r=[bKWs], w=[kv["KW"][1]], dma=True)
                                P.op("sp", I("dma_start", out=kv["VS"][0][:], in_=VSs[kvh]), r=[bVSs], w=[kv["VS"][1]], dma=True)
                                P.op("sp", I("dma_start", out=kv["VW"][0][:], in_=VWs[kvh]), r=[bVWs], w=[kv["VW"][1]], dma=True)
                            Q, bQ = QR.next()
                            P.op("sp", I("dma_start", out=Q[0:64, :], in_=QNs[h, :, qb * 512:(qb + 1) * 512]), r=[bQNs], w=[bQ], dma=True)
                            P.op("sp", I("dma_start", out=Q[64:128, :], in_=NGs[kvh, :, qb * 512:(qb + 1) * 512]), r=[bNGs], w=[bQ], dma=True)
                            js["q"] = (Q, bQ)
                            jw["q"] = (Q[:, :], bQ)
                            K, bK = kv["KS"]
                            V, bV = kv["VS"]
                            tiles = []
                            for kt in range(4 * qb + 4):
                                lo = max(0, kt - 4 * qb)
                                masks = []
                                for qs in range(lo, 4):
                                    qt = 4 * qb + qs
                                    if kt == qt:
                                        masks.append((qs, EM[:, h, 0:128], bEM))
                                    elif kt == qt - 1:
                                        masks.append((qs, EM[:, h, 128:256], bEM))
                                tiles.append((K[:, kt * 128:(kt + 1) * 128], bK, V[:, kt, :], bV, lo, 4, masks))
                            js["tiles"][:] = tiles
                            K, bK = kv["KW"]
                            V, bV = kv["VW"]
                            tiles = []
                            for kt in range(max(0, 4 * qb - 4), 4 * qb + 4):
                                lo = max(0, kt - 4 * qb)
                                hi = min(4, kt - 4 * qb + 5)
                                masks = []
                                for qs in range(lo, hi):
                                    qt = 4 * qb + qs
                                    if kt == qt:
                                        masks.append((qs, EM[:, h, 0:128], bEM))
                                    elif kt == qt - 1:
                                        masks.append((qs, EM[:, h, 128:256], bEM))
                                    elif kt == qt - 4:
                                        masks.append((qs, FARM[:, :], bFARM))
                                tiles.append((K[:, kt * 128:(kt + 1) * 128], bK, V[:, kt, :], bV, lo, hi, masks))
                            jw["tiles"][:] = tiles
                        js["load"] = load
                        jw["load"] = None
                        js["tiles"] = [None] * (4 * qb + 4)
                        jw["tiles"] = [None] * (4 * qb + 4 - max(0, 4 * qb - 4))
                        for j_, mk_, dst, dbuf, col in ((js, mks, OSs, bOSs, 3 * h + 1), (jw, mkw, OWs, bOWs, 3 * h + 2)):
                            j_["bias"] = T31B[:, h:h + 1]
                            j_["rbias"] = [bT31B]
                            j_["evac"] = mk_(dst, dbuf, h, qb, col)
                            jobs.append(j_)
            run_attention(L, jobs)
            P.barrier()

    def phase_M(b):
        with ExitStack() as L:
            OMR = Ring([(sb(L, "OM%d" % i, [128, 512]), Buf("OM%d" % i)) for i in range(4)])
            ONR = Ring([(sb(L, "ON%d" % i, [128, 3, 512]), Buf("ON%d" % i)) for i in range(4)])
            MXR = Ring([(sb(L, "MX%d" % i, [128, 1024]), Buf("MX%d" % i)) for i in range(3)])
            JNKR = Ring([(sb(L, "JNK%d" % i, [128, 512]), Buf("JNK%d" % i)) for i in range(2)])
            SSQR = Ring([(sb(L, "SSQ%d" % i, [128, 2]), Buf("SSQ%d" % i)) for i in range(4)])
            MXT = Ring([(sb(L, "MXT%d" % i, [128, 8, 512], BF16), Buf("MXT%d" % i)) for i in range(2)])
            XR = Ring([(sb(L, "Xm%d" % i, [128, 8, 512]), Buf("Xm%d" % i)) for i in range(2)])
            W_out = sb(L, "W_out", [128, 8, 1024], BF16); bW_out = Buf("W_out")
            GOUT = sb(L, "GOUT", [128, 1024]); bGOUT = Buf("GOUT")
            MSR = Ring([(sb(L, "mstg%d" % i, [128, 1024]), Buf("mstg%d" % i)) for i in range(2)])
            wv = w_out_d.rearrange("(c p) n -> p c n", p=128)
            for c in range(8):
                load_cast(L, W_out[:, c, :], wv[:, c, :], bW_out, 128, 1024, MSR, c)
            P.op("sp", I("dma_start", out=GOUT[:], in_=gout_d), w=[bGOUT], dma=True)
            xv = xT[b].rearrange("(c p) t -> p c t", p=128)
            hv = HTs[b].rearrange("(c p) t -> p c t", p=128)
            for tb in range(NTB):
                MT, bMT = MXT.next()
                X, bX = XR.next()
                P.op("sp", I("dma_start", out=X[:], in_=xv[:, :, tb * 512:(tb + 1) * 512]), w=[bX], dma=True)
                for ts in range(4):
                    t = tb * 4 + ts
                    OM, bOM = OMR.next()
                    ON, bON = ONR.next()
                    MX, bMX = MXR.next()
                    SSQ, bSSQ = SSQR.next()
                    P.op("sp", I("dma_start", out=OM[:], in_=OMs[t * 128:(t + 1) * 128, :]), r=[bOMs], w=[bOM], dma=True)
                    for i, (src, sbuf_) in enumerate(((OCs, bOCs), (OSs, bOSs), (OWs, bOWs))):
                        P.op(("pool", "sp", "pool")[i], I("dma_start", out=ON[:, i, :], in_=src[t * 128:(t + 1) * 128, :]),
                             r=[sbuf_], w=[bON], dma=True)
                    P.op("pool", I("tensor_tensor", out=ON[:, 0, :], in0=ON[:, 0, :], in1=ON[:, 1, :], op=ALU.add), r=[bON], w=[bON])
                    P.op("pool", I("tensor_tensor", out=ON[:, 0, :], in0=ON[:, 0, :], in1=ON[:, 2, :], op=ALU.add), r=[bON], w=[bON])
                    JNK, bJNK = JNKR.next()
                    P.op("act", I("activation", out=JNK[:], in_=OM[:], func=AF.Square, accum_out=SSQ[:, 0:1]), r=[bOM], w=[bJNK, bSSQ])
                    JNK, bJNK = JNKR.next()
                    P.op("act", I("activation", out=JNK[:], in_=ON[:, 0, :], func=AF.Square, accum_out=SSQ[:, 1:2]), r=[bON], w=[bJNK, bSSQ])
                    P.op("act", I("activation", out=SSQ[:], in_=SSQ[:], func=AF.Sqrt, bias=EPSB[:, 0:1], scale=1.0 / 512.0),
                         r=[bSSQ, bEPSB], w=[bSSQ])
                    P.op("dve", I("reciprocal", out=SSQ[:], in_=SSQ[:]), r=[bSSQ], w=[bSSQ])
                    P.op("dve", I("scalar_tensor_tensor", out=MX[:, 0:512], in0=OM[:], scalar=SSQ[:, 0:1], in1=GOUT[:, 0:512],
                                  op0=ALU.mult, op1=ALU.mult), r=[bOM, bSSQ, bGOUT], w=[bMX])
                    P.op("dve", I("scalar_tensor_tensor", out=MX[:, 512:1024], in0=ON[:, 0, :], scalar=SSQ[:, 1:2],
                                  in1=GOUT[:, 512:1024], op0=ALU.mult, op1=ALU.mult), r=[bON, bSSQ, bGOUT], w=[bMX])
                    for half in range(2):
                        pt, pb = PSR.next()
                        for c4 in range(4):
                            c = half * 4 + c4
                            P.op("pe", I("transpose", out=pt[:, c4 * 128:(c4 + 1) * 128], in_=MX[:, c * 128:(c + 1) * 128],
                                         identity=IDN[:, :]), r=[bMX, bIDN], w=[pb])
                        ov = MT[:, half * 4:half * 4 + 4, ts * 128:(ts + 1) * 128]
                        iv = pt[:, :].rearrange("p (c t) -> p c t", t=128)
                        if half == 0:
                            P.op("act", I("activation", out=ov, in_=iv, func=AF.Copy), r=[pb], w=[bMT])
                        else:
                            P.op("dve", I("tensor_copy", out=ov, in_=iv), r=[pb], w=[bMT])
                for m in range(8):
                    pt, pb = PSR.next()
                    for c in range(8):
                        P.op("pe", I("matmul", pt[:, :], lhsT=W_out[:, c, m * 128:(m + 1) * 128], rhs=MT[:, c, :],
                                     start=(c == 0), stop=(c == 7)), r=[bW_out, bMT], w=[pb])
                    P.op("dve", I("tensor_tensor", out=X[:, m, :], in0=X[:, m, :], in1=pt[:, :], op=ALU.add), r=[bX, pb], w=[bX])
                P.op("pool", I("dma_start", out=hv[:, :, tb * 512:(tb + 1) * 512], in_=X[:]), r=[bX], w=[bHTs[b]], dma=True)
            P.barrier()

    stop = os.environ.get("MK_STOP", "")
    for b in range(NB):
        phase_P(b)
        if stop == "P":
            break
        phase_C()
        phase_MLA()
        if stop == "MLA":
            break
        phase_NC()
        if stop == "NC":
            break
        phase_NSW()
        phase_M(b)
        if stop == "M":
            break
    A.close()
    if stop:
        with ExitStack() as L:
            Z = sb(L, "Z", [128, 512]); bZ = Buf("Z")
            P.op("pool", I("memset", Z[:], 0.0), w=[bZ])
            P.op("sp", I("dma_start", out=outT[0, 0:128, 0:512], in_=Z[:]), r=[bZ], w=[bOUT], dma=True)
            P.barrier()
        P.emit()
        es.close()
        return nc

    with ExitStack() as Fs:
        WG = sb(Fs, "WG", [128, 8, DFF], BF16); bWG = Buf("WG")
        WU = sb(Fs, "WU", [128, 8, DFF], BF16); bWU = Buf("WU")
        WD = sb(Fs, "WD", [128, 22, D], BF16); bWD = Buf("WD")
        with ExitStack() as SU:
            stg = [(sb(SU, "fstg%d" % i, [128, DFF]), Buf("fstg%d" % i)) for i in range(4)]
            SR = Ring(stg)
            ei = 0
            for (wd, wt, wb) in ((w_gate_d, WG, bWG), (w_up_d, WU, bWU)):
                wv = wd.rearrange("(c p) n -> p c n", p=128)
                for c in range(8):
                    load_cast(SU, wt[:, c, :], wv[:, c, :], wb, 128, DFF, SR, ei, indep=True); ei += 1
            wv = w_down_d.rearrange("(c p) n -> p c n", p=128)
            for c in range(22):
                load_cast(SU, WD[:, c, :], wv[:, c, :], bWD, 128, D, SR, ei, indep=True); ei += 1
            P.barrier()
        H = sb(Fs, "H", [128, 8, 512]); bH = Buf("H")
        OUT = sb(Fs, "OUT", [128, 8, 512]); bOUTt = Buf("OUTt")
        HN = sb(Fs, "HN", [128, 8, 512], BF16); bHN = Buf("HN")
        RSa = sb(Fs, "RSa", [128, 512]); bRSa = Buf("RSa")
        RSb = sb(Fs, "RSb", [128, 512]); bRSb = Buf("RSb")
        AT = sb(Fs, "AT", [128, 22, 512], BF16); bAT = [Buf("AT%d" % i) for i in range(22)]
        SGR = Ring([(sb(Fs, "SGf%d" % i, [128, 512]), Buf("SGf%d" % i)) for i in range(2)])
        SQR = Ring([(sb(Fs, "SQf%d" % i, [128, 512], BF16), Buf("SQf%d" % i)) for i in range(4)])
        blocks = [(b, tb) for b in range(NB) for tb in range(NTB)]

        def stats(src, bsrc, RS, bRS):
            pt, pb = PSR.next()
            for c in range(8):
                sq, bsq = SQR.next()
                P.op("act", I("activation", out=sq[:], in_=src[:, c, :], func=AF.Square), r=[bsrc], w=[bsq])
                P.op("pe", I("matmul", pt[:, :], lhsT=ONES[:, :], rhs=sq[:], start=(c == 0), stop=(c == 7)),
                     r=[bONES, bsq], w=[pb])
            P.op("act", I("activation", out=RS[:], in_=pt[:, :], func=AF.Sqrt, bias=EPSB[:, 0:1], scale=1.0 / 1024.0),
                 r=[pb, bEPSB], w=[bRS])
            P.op("dve", I("reciprocal", out=RS[:], in_=RS[:]), r=[bRS], w=[bRS])

        def chain1(k):
            b, tb = blocks[k]
            hv = HTs[b].rearrange("(c p) t -> p c t", p=128)
            P.op("sp", I("dma_start", out=H[:], in_=hv[:, :, tb * 512:(tb + 1) * 512]), r=[bHTs[b]], w=[bH], dma=True)
            stats(H, bH, RSa, bRSa)

        def chain2(k):
            for c in range(8):
                P.op("dve", I("scalar_tensor_tensor", out=HN[:, c, :], in0=H[:, c, :], scalar=GV[:, 8 + c:9 + c],
                              in1=RSa[:], op0=ALU.mult, op1=ALU.mult), r=[bH, bGV, bRSa], w=[bHN])

        def copies(k):
            for c in range(8):
                P.op("pool", I("tensor_copy", out=OUT[:, c, :], in_=H[:, c, :]), r=[bH], w=[bOUTt])

        def gateup(k, f):
            pg, pgb = PSR.next()
            for c in range(8):
                P.op("pe", I("matmul", pg[:, :], lhsT=WG[:, c, f * 128:(f + 1) * 128], rhs=HN[:, c, :],
                             start=(c == 0), stop=(c == 7)), r=[bWG, bHN], w=[pgb])
            pu, pub = PSR.next()
            for c in range(8):
                P.op("pe", I("matmul", pu[:, :], lhsT=WU[:, c, f * 128:(f + 1) * 128], rhs=HN[:, c, :],
                             start=(c == 0), stop=(c == 7)), r=[bWU, bHN], w=[pub])
            SG, bSG = SGR.next()
            P.op("act", I("activation", out=SG[:], in_=pg[:, :], func=AF.Silu), r=[pgb], w=[bSG])
            P.op("dve", I("tensor_tensor", out=AT[:, f, :], in0=SG[:], in1=pu[:, :], op=ALU.mult),
                 r=[bSG, pub], w=[bAT[f]])

        def down(k):
            for m in range(8):
                pt, pb = PSR.next()
                for f in range(22):
                    P.op("pe", I("matmul", pt[:, :], lhsT=WD[:, f, m * 128:(m + 1) * 128], rhs=AT[:, f, :],
                                 start=(f == 0), stop=(f == 21)), r=[bWD, bAT[f]], w=[pb])
                P.op("dve", I("tensor_tensor", out=OUT[:, m, :], in0=OUT[:, m, :], in1=pt[:, :], op=ALU.add),
                     r=[bOUTt, pb], w=[bOUTt])

        def final(k):
            b, tb = blocks[k]
            ov = outT[b].rearrange("(c p) t -> p c t", p=128)
            stats(OUT, bOUTt, RSb, bRSb)
            for c in range(8):
                P.op("dve", I("scalar_tensor_tensor", out=OUT[:, c, :], in0=OUT[:, c, :], scalar=GV[:, 16 + c:17 + c],
                              in1=RSb[:], op0=ALU.mult, op1=ALU.mult), r=[bOUTt, bGV, bRSb], w=[bOUTt])
            P.op("pool", I("dma_start", out=ov[:, :, tb * 512:(tb + 1) * 512], in_=OUT[:]), r=[bOUTt], w=[bOUT], dma=True)

        nblk = len(blocks)
        chain1(0)
        for k in range(nblk):
            chain2(k)
            for f in range(22):
                gateup(k, f)
                if f == 1:
                    if k > 0:
                        final(k - 1)
                    copies(k)
                if f == 14 and k + 1 < nblk:
                    chain1(k + 1)
            down(k)
        final(nblk - 1)
        P.barrier()
    P.emit()
    es.close()
    return nc


def prep_inputs(inp):
    f = lambda a: np.ascontiguousarray(np.asarray(a, dtype=np.float32))
    c = host_consts()
    shared = {
        "w_in": f(inp["w_in"][0]), "w_uq": f(inp["mla_w_uq"][0]), "w_ukv": f(inp["mla_w_ukv"][0]),
        "w1k": f(inp["nsa_cmp_w1_k"][0]), "w1v": f(inp["nsa_cmp_w1_v"][0]),
        "w2k": f(inp["nsa_cmp_w2_k"][0]), "w2v": f(inp["nsa_cmp_w2_v"][0]),
        "poskT": f(np.asarray(inp["nsa_cmp_pos_k"][0]).T), "posvT": f(np.asarray(inp["nsa_cmp_pos_v"][0]).T),
        "t5": f(inp["t5_table"]), "w_out": f(inp["w_out"][0]),
        "w_gate": f(inp["w_gate"][0]), "w_up": f(inp["w_up"][0]), "w_down": f(inp["w_down"][0]),
    }
    gv = np.zeros((128, 32), np.float32)
    gv[:, 0:8] = np.asarray(inp["norm_mix_g"][0], np.float32).reshape(8, 128).T
    gv[:, 8:16] = np.asarray(inp["norm_ffn_g"][0], np.float32).reshape(8, 128).T
    gv[:, 16:24] = np.asarray(inp["final_norm_g"], np.float32).reshape(8, 128).T
    gv[:, 24:26] = np.asarray(inp["mla_q_norm_g"][0], np.float32).reshape(2, 128).T
    gv[:, 26] = np.asarray(inp["mla_kv_norm_g"][0], np.float32)
    shared["gvec"] = gv
    go = np.concatenate([np.asarray(inp["out_norm_mla_g"][0], np.float32), np.asarray(inp["out_norm_nsa_g"][0], np.float32)])
    shared["gout"] = np.ascontiguousarray(np.broadcast_to(go[None, :], (128, 1024)))
    for k in ("tri", "farm", "antiI", "ident", "ind", "ohd", "ohc", "selc", "ovl", "rope"):
        shared[k] = c[k]
    x = np.asarray(inp["x"], np.float32)
    maps = []
    for i in range(NCORES):
        m = dict(shared)
        m["xT"] = np.ascontiguousarray(x[i * NB:(i + 1) * NB].transpose(0, 2, 1))
        maps.append(m)
    return maps


def kernel(**inputs):
    nc = build()
    maps = prep_inputs(inputs)
    res = run_bass_kernel_spmd(nc, maps, core_ids=list(range(NCORES)))
    out = np.empty((NCORES * NB, S, D), np.float32)
    for i in range(NCORES):
        o = np.asarray(res.results[i]["outT"], np.float32)
        out[i * NB:(i + 1) * NB] = o.transpose(0, 2, 1)
    return out
```

```python
import math
import os
from contextlib import ExitStack

import ml_dtypes
import numpy as np

import concourse.bass as bass
import concourse.mybir as mybir
from concourse.bass_utils import run_bass_kernel_spmd

F32 = mybir.dt.float32
BF16 = mybir.dt.bfloat16
AF = mybir.ActivationFunctionType
ALU = mybir.AluOpType
NPBF = ml_dtypes.bfloat16

S = 4096
D = 1024
NB = 2
NCORES = 8
DFF = 2816
NTB = S // 512
NT = S // 128
EPS = 1e-6
BIG = 30000.0
SC_M = 96 ** -0.5
HCL = 8176
ENGS = ("pe", "act", "dve", "pool", "sp")
RDMA = 8
STRICT_SAME = True
WARM_N = int(os.environ.get('MK_WARM', '128'))
LAG = int(os.environ.get('MK_LAG', '2'))


class Buf:
    __slots__ = ("name", "w", "r", "ep")

    def __init__(self, name):
        self.name = name
        self.w = None
        self.r = []
        self.ep = -1


class Op:
    __slots__ = ("eng", "fn", "deps", "dma", "n", "sig", "need", "dk", "dval", "bar", "tag")


def I(meth, *a, **k):
    return lambda e: getattr(e, meth)(*a, **k)


class Prog:
    def __init__(self, nc):
        self.nc = nc
        self.ops = {e: [] for e in ENGS}
        self.dmas = {e: [] for e in ENGS}
        self.epoch = 0
        self.tag = ""

    def op(self, eng, fn, r=(), w=(), dma=False, bar=False, extra=(), disjoint=False):
        o = Op()
        o.eng, o.fn, o.dma, o.bar = eng, fn, dma, bar
        o.tag = self.tag
        o.need = False
        o.sig = None
        o.n = len(self.ops[eng])
        deps = {}
        for b in list(r) + list(w):
            if b.ep != self.epoch:
                b.w, b.r, b.ep = None, [], self.epoch
        for b in r:
            if b.w is not None:
                deps[b.w] = "raw"
        for b in w:
            if b.w is not None and not disjoint:
                deps.setdefault(b.w, "waw")
            for x in b.r:
                deps.setdefault(x, "war")
        for x in extra:
            deps[x] = "raw"
        fin = []
        for d, kind in deps.items():
            if d is o:
                continue
            if d.eng == eng and not d.dma and not dma and not bar:
                if eng == "pe":
                    continue
                if not STRICT_SAME and (kind != "raw" or o.n - d.n > 3):
                    continue
            fin.append(d)
        if dma:
            k = len(self.dmas[eng])
            o.dk = (eng, k % RDMA)
            o.dval = 16 * (k // RDMA + 1)
            if k >= RDMA:
                fin.append(self.dmas[eng][k - RDMA])
            self.dmas[eng].append(o)
        for d in fin:
            d.need = True
        o.deps = fin
        self.ops[eng].append(o)
        ws = set(id(b) for b in w)
        for b in w:
            b.w = o
            b.r = []
        for b in r:
            if id(b) not in ws:
                b.r.append(o)
        return o

    def barrier(self):
        last = []
        for e in ENGS:
            if self.ops[e]:
                last.append(self.ops[e][-1])
            last.extend(self.dmas[e][-RDMA:])
        bsp = self.op("sp", None, bar=True, extra=last)
        for e in ENGS:
            if e != "sp":
                self.op(e, None, bar=True, extra=[bsp])
        self.epoch += 1

    def check(self):
        done = set()
        pc = {e: 0 for e in ENGS}
        prog = True
        while prog:
            prog = False
            for e in ENGS:
                while pc[e] < len(self.ops[e]):
                    o = self.ops[e][pc[e]]
                    if all(id(d) in done for d in o.deps):
                        done.add(id(o))
                        pc[e] += 1
                        prog = True
                    else:
                        break
        bad = {e: pc[e] for e in ENGS if pc[e] < len(self.ops[e])}
        if bad:
            msg = []
            for e, i in bad.items():
                o = self.ops[e][i]
                msg.append("%s blocked at op %d/%d (%s) waiting on %s" % (
                    e, i, len(self.ops[e]), getattr(o, "tag", ""),
                    [(d.eng, d.n, getattr(d, "tag", "")) for d in o.deps if id(d) not in done]))
            raise RuntimeError("DEADLOCK: " + " | ".join(msg))

    def emit(self):
        self.check()
        nc = self.nc
        for e in ENGS:
            c = 0
            for o in self.ops[e]:
                if o.need and not o.dma:
                    c += 1
                    o.sig = c
        with ExitStack() as st:
            sem = {e: st.enter_context(nc.semaphore("s_" + e)) for e in ENGS}
            dsem = {}
            for e in ENGS:
                if self.dmas[e]:
                    for i in range(RDMA):
                        dsem[(e, i)] = st.enter_context(nc.semaphore("d_%s%d" % (e, i)))
            block = st.enter_context(nc.Block())

            def run(ename, eng):
                waited = {}
                for o in self.ops[ename]:
                    for d in o.deps:
                        if d.dma:
                            key, s_, v = d.dk, dsem[d.dk], d.dval
                        else:
                            key, s_, v = d.eng, sem[d.eng], d.sig
                        if waited.get(key, 0) >= v:
                            continue
                        eng.wait_ge(s_, v)
                        waited[key] = v
                    if o.bar:
                        if o.sig is not None:
                            eng.sem_inc(sem[ename], 1)
                        continue
                    ins = o.fn(eng)
                    if o.dma:
                        ins.then_inc(dsem[o.dk], 16)
                    elif o.sig is not None:
                        ins.then_inc(sem[ename], 1)
                if ename == "sp":
                    for q in ENGS:
                        for d in self.dmas[q][-RDMA:]:
                            if waited.get(d.dk, 0) < d.dval:
                                eng.wait_ge(dsem[d.dk], d.dval)
                                waited[d.dk] = d.dval

            @block.sync
            def _(e):
                run("sp", e)

            @block.tensor
            def _(e):
                run("pe", e)

            @block.scalar
            def _(e):
                run("act", e)

            @block.vector
            def _(e):
                run("dve", e)

            @block.gpsimd
            def _(e):
                run("pool", e)


class Ring:
    def __init__(self, items):
        self.items = items
        self.i = 0

    def next(self):
        x = self.items[self.i % len(self.items)]
        self.i += 1
        return x


def _bucket(d):
    n = np.maximum(d, 0)
    nf = np.maximum(n, 1).astype(np.float32)
    large = 16 + (np.log(nf / np.float32(16)) / np.float32(math.log(8.0)) * np.float32(16)).astype(np.int32)
    large = np.minimum(large, 31)
    return np.where(n < 16, n, large)


_CONST = None


def host_consts():
    global _CONST
    if _CONST is not None:
        return _CONST
    c = {}
    k = np.arange(128)
    c["tri"] = (k[:, None] <= k[None, :]).astype(NPBF)
    c["farm"] = (k[:, None] > k[None, :]).astype(NPBF)
    c["antiI"] = (k[:, None] == 127 - k[None, :]).astype(NPBF)
    c["ident"] = np.eye(128, dtype=np.float32)
    t = np.arange(S)
    c["ind"] = (t[None, :] // 64 == np.arange(64)[:, None]).astype(NPBF)
    d = np.arange(384) - 127
    oh = np.zeros((33, 384), np.float32)
    b = _bucket(d)
    for i in range(384):
        oh[32 if d[i] < 0 else b[i], i] = 1.0
    c["ohd"] = oh
    m = np.arange(HCL) - 4111
    oh = np.zeros((33, HCL), np.float32)
    b = _bucket(m)
    oh[np.where(m < 0, 32, b), np.arange(HCL)] = 1.0
    c["ohc"] = oh
    sel = np.zeros((NT, 128, 128), np.float32)
    j = np.arange(64)
    for qt in range(NT):
        q = qt * 128 + k
        cur = q // 64
        forced = (j[None, :] == 0) | (j[None, :] == cur[:, None]) | (j[None, :] == cur[:, None] - 1)
        causal = j[None, :] <= cur[:, None]
        sel[qt, :, :64] = (causal & ~forced)
        sel[qt, :, 64:] = np.where(forced, 1e30, np.where(causal, 0.0, -1e30))
    c["selc"] = sel
    ov = np.zeros((256, 64), np.float32)
    for npr in range(1, 256):
        n = 255 - npr
        lo = np.maximum(16 * n, 64 * j)
        hi = np.minimum(16 * n + 32, 64 * j + 64)
        ov[npr] = np.maximum(0, hi - lo) / 16.0
    c["ovl"] = ov.astype(NPBF)
    inv = (10000.0 ** (-np.arange(0, 32, 2, dtype=np.float32) / 32)).astype(np.float32)
    ang = t.astype(np.float32)[None, :] * inv[:, None]
    cos = np.cos(ang).astype(np.float32)
    sin = np.sin(ang).astype(np.float32)
    cosT = np.concatenate([cos, cos], 0)
    sinT = np.concatenate([-sin, sin], 0)
    rope = np.zeros((96, 4, S), np.float32)
    rope[64:96, 0] = cosT
    rope[64:96, 1] = sinT
    rope[64:96, 2] = cosT * np.float32(SC_M)
    rope[64:96, 3] = sinT * np.float32(SC_M)
    c["rope"] = rope
    _CONST = c
    return c


def build(debug=None):
    nc = bass.Bass("TRN2", target_bir_lowering=False)
    P = Prog(nc)
    es = ExitStack()

    def din(name, shape, dt=F32):
        return nc.dram_tensor(name, list(shape), dt, kind="ExternalInput")

    def dscr(name, shape, dt=F32):
        kind = "ExternalOutput" if (debug and name in debug) else "Internal"
        return nc.dram_tensor(name, list(shape), dt, kind=kind)

    xT = din("xT", [NB, D, S]).ap()
    w_in_d = din("w_in", [D, 1720]).ap()
    w_uq_d = din("w_uq", [256, 768]).ap()
    w_ukv_d = din("w_ukv", [128, 1024]).ap()
    w1k_d = din("w1k", [2048, 128]).ap()
    w1v_d = din("w1v", [2048, 128]).ap()
    w2k_d = din("w2k", [128, 64]).ap()
    w2v_d = din("w2v", [128, 64]).ap()
    posk_d = din("poskT", [64, 32]).ap()
    posv_d = din("posvT", [64, 32]).ap()
    t5_d = din("t5", [32, 8]).ap()
    w_out_d = din("w_out", [D, D]).ap()
    w_gate_d = din("w_gate", [D, DFF]).ap()
    w_up_d = din("w_up", [D, DFF]).ap()
    w_down_d = din("w_down", [DFF, D]).ap()
    gvec_d = din("gvec", [128, 32]).ap()
    gout_d = din("gout", [128, 1024]).ap()
    tri_d = din("tri", [128, 128], BF16).ap()
    farm_d = din("farm", [128, 128], BF16).ap()
    anti_d = din("antiI", [128, 128], BF16).ap()
    ident_d = din("ident", [128, 128]).ap()
    ind_d = din("ind", [64, S], BF16).ap()
    ohd_d = din("ohd", [33, 384]).ap()
    ohc_d = din("ohc", [33, HCL]).ap()
    selc_d = din("selc", [NT, 128, 128]).ap()
    ovl_d = din("ovl", [256, 64], BF16).ap()
    rope_d = din("rope", [96, 4, S]).ap()

    outT_h = nc.dram_tensor("outT", [NB, D, S], F32, kind="ExternalOutput")
    outT = outT_h.ap()

    QMs = dscr("QMs", [8, 96, S], BF16).ap()
    KMs = dscr("KMs", [8, 96, S], BF16).ap()
    VMs = dscr("VMs", [8, 128, NT, 65], BF16).ap()
    QNs = dscr("QNs", [8, 64, S], BF16).ap()
    KSs = dscr("KSs", [2, 64, S], BF16).ap()
    KWs = dscr("KWs", [2, 64, S], BF16).ap()
    VSs = dscr("VSs", [2, 128, NT, 65], BF16).ap()
    VWs = dscr("VWs", [2, 128, NT, 65], BF16).ap()
    NGs = dscr("NGs", [2, 64, S], BF16).ap()
    OMs = dscr("OMs", [S, 512]).ap()
    OCs = dscr("OCs", [S, 512]).ap()
    OSs = dscr("OSs", [S, 512]).ap()
    OWs = dscr("OWs", [S, 512]).ap()
    HTs = dscr("HTs", [NB, D, S]).ap()
    GD_h = dscr("GDs", [8, 384])
    HC_h = dscr("HCs", [8, HCL], BF16)
    GDs, HCs = GD_h.ap(), HC_h.ap()
    bQMs, bKMs, bVMs, bQNs = Buf("QMs"), Buf("KMs"), Buf("VMs"), Buf("QNs")
    bKSs, bKWs, bVSs, bVWs, bNGs = Buf("KSs"), Buf("KWs"), Buf("VSs"), Buf("VWs"), Buf("NGs")
    bOMs, bOCs, bOSs, bOWs = Buf("OMs"), Buf("OCs"), Buf("OSs"), Buf("OWs")
    bHTs = [Buf("HT0"), Buf("HT1")]
    bGDs, bHCs = Buf("GDs"), Buf("HCs")
    bOUT = Buf("out")

    uid = [0]

    def sb(stack, name, shape, dt=F32):
        uid[0] += 1
        return stack.enter_context(nc.sbuf_tensor("%s_%d" % (name, uid[0]), list(shape), dt))

    ps = [es.enter_context(nc.psum_tensor("ps%d" % i, [128, 512], F32)) for i in range(8)]
    psb = [Buf("ps%d" % i) for i in range(8)]
    PSR = Ring(list(zip(ps, psb)))

    GV = sb(es, "GV", [128, 32]); bGV = Buf("GV")
    ONES = sb(es, "ONES", [128, 128], BF16); bONES = Buf("ONES")
    IDN = sb(es, "IDN", [128, 128]); bIDN = Buf("IDN")
    P.op("sp", I("dma_start", out=GV[:], in_=gvec_d), w=[bGV], dma=True)
    P.op("sp", I("dma_start", out=IDN[:], in_=ident_d), w=[bIDN], dma=True)
    P.op("pool", I("memset", ONES[:], 1.0), w=[bONES])
    IDNB = sb(es, "IDNB", [128, 128], BF16); bIDNB = Buf("IDNB")
    P.op("dve", I("tensor_copy", out=IDNB[:], in_=IDN[:]), r=[bIDN], w=[bIDNB])
    EPSB = sb(es, "EPSB", [128, 1]); bEPSB = Buf("EPSB")
    P.op("pool", I("memset", EPSB[:], EPS), w=[bEPSB])

    def load_cast(stack_stage, dst_ap, src_ap, dstbuf, rows, cols, ring, eng_i, indep=False):
        if indep:
            dstbuf = Buf("chunk")
        stg, sbuf_ = ring.next()
        P.op("sp", I("dma_start", out=stg[0:rows, 0:cols], in_=src_ap), w=[sbuf_], dma=True)
        eng = ("dve", "act", "pool", "dve", "act")[eng_i % 5]
        if eng == "act":
            P.op(eng, I("activation", out=dst_ap, in_=stg[0:rows, 0:cols], func=AF.Copy), r=[sbuf_], w=[dstbuf])
        else:
            P.op(eng, I("tensor_copy", out=dst_ap, in_=stg[0:rows, 0:cols]), r=[sbuf_], w=[dstbuf])

    A = ExitStack()
    W_in = sb(A, "W_in", [128, 8, 1720], BF16); bW_in = Buf("W_in")
    W_uq = sb(A, "W_uq", [128, 2, 768], BF16); bW_uq = Buf("W_uq")
    W_uqB = sb(A, "W_uqB", [128, 2, 8, 96], BF16); bW_uqB = Buf("W_uqB")
    WkrA = sb(A, "WkrA", [128, 8, 96], BF16); bWkrA = Buf("WkrA")
    WkrB = sb(A, "WkrB", [128, 8, 96], BF16); bWkrB = Buf("WkrB")
    W_ukv = sb(A, "W_ukv", [128, 1024], BF16); bW_ukv = Buf("W_ukv")
    W2 = [sb(A, "W2k", [128, 64], BF16), sb(A, "W2v", [128, 64], BF16)]
    bW2 = [Buf("W2k"), Buf("W2v")]
    POS = [sb(A, "POSk", [64, 32], BF16), sb(A, "POSv", [64, 32], BF16)]
    bPOS = [Buf("POSk"), Buf("POSv")]
    TRI = sb(A, "TRI", [128, 128], BF16); bTRI = Buf("TRI")
    FARM = sb(A, "FARM", [128, 128], BF16); bFARM = Buf("FARM")
    EM = sb(A, "EM", [128, 8, 256], BF16); bEM = Buf("EM")
    T31B = sb(A, "T31B", [128, 8]); bT31B = Buf("T31B")
    OVL = sb(A, "OVL", [128, 2, 64], BF16); bOVL = Buf("OVL")
    GATES = sb(A, "GATES", [128, NT, 24]); bGATES = Buf("GATES")
    KCR = sb(A, "KCR", [128, S], BF16); bKCR = Buf("KCR")
    VCR = sb(A, "VCR", [128, S], BF16); bVCR = Buf("VCR")
    KCT = sb(A, "KCT", [64, 2, 256], BF16); bKCT = Buf("KCT")
    VCA = sb(A, "VCA", [128, 2, 2, 65], BF16); bVCA = Buf("VCA")

    with ExitStack() as SU:
        stg = [(sb(SU, "stg%d" % i, [128, 4096]), Buf("stg%d" % i)) for i in range(2)]
        SR = Ring(stg)
        ei = 0
        wv = w_in_d.rearrange("(c p) n -> p c n", p=128)
        for c in range(8):
            load_cast(SU, W_in[:, c, :], wv[:, c, :], bW_in, 128, 1720, SR, ei); ei += 1
        wv = w_uq_d.rearrange("(c p) n -> p c n", p=128)
        for c in range(2):
            load_cast(SU, W_uq[:, c, :], wv[:, c, :], bW_uq, 128, 768, SR, ei); ei += 1
        load_cast(SU, W_ukv[:, :], w_ukv_d, bW_ukv, 128, 1024, SR, ei); ei += 1
        for kv, (wd, pd) in enumerate(((w2k_d, posk_d), (w2v_d, posv_d))):
            load_cast(SU, W2[kv][:, :], wd, bW2[kv], 128, 64, SR, ei); ei += 1
            load_cast(SU, POS[kv][:, :], pd, bPOS[kv], 64, 32, SR, ei); ei += 1
        P.op("pool", I("memset", W_uqB[:], 0.0), w=[bW_uqB])
        uq4 = W_uq[:, :, :].rearrange("p c (h e) -> p c h e", e=96)
        P.op("pool", I("tensor_copy", out=W_uqB[:, :, :, 64:80], in_=uq4[:, :, :, 80:96]), r=[bW_uq], w=[bW_uqB])
        P.op("pool", I("tensor_copy", out=W_uqB[:, :, :, 80:96], in_=uq4[:, :, :, 64:80]), r=[bW_uq], w=[bW_uqB])
        P.op("pool", I("memset", WkrA[:], 0.0), w=[bWkrA])
        P.op("pool", I("memset", WkrB[:], 0.0), w=[bWkrB])
        P.op("pool", I("tensor_copy", out=WkrA[:, :, 64:96], in_=W_in[:, :, 384:416]), r=[bW_in], w=[bWkrA])
        P.op("pool", I("tensor_copy", out=WkrB[:, :, 64:80], in_=W_in[:, :, 400:416]), r=[bW_in], w=[bWkrB])
        P.op("pool", I("tensor_copy", out=WkrB[:, :, 80:96], in_=W_in[:, :, 384:400]), r=[bW_in], w=[bWkrB])
        P.op("sp", I("dma_start", out=TRI[:], in_=tri_d), w=[bTRI], dma=True)
        P.op("sp", I("dma_start", out=FARM[:], in_=farm_d), w=[bFARM], dma=True)
        P.op("sp", I("dma_start", out=OVL[:], in_=ovl_d.rearrange("(t p) j -> p t j", p=128)), w=[bOVL], dma=True)
        P.op("sp", I("dma_start", out=T31B[:], in_=t5_d[31:32, :].to_broadcast([128, 8])), w=[bT31B], dma=True)
        TBLX = sb(SU, "TBLX", [33, 8]); bTBLX = Buf("TBLX")
        NT31 = sb(SU, "NT31", [8, 1]); bNT31 = Buf("NT31")
        OHD = sb(SU, "OHD", [33, 384]); bOHD = Buf("OHD")
        ANTI = sb(SU, "ANTI", [128, 128], BF16); bANTI = Buf("ANTI")
        P.op("pool", I("memset", TBLX[32:33, :], -BIG), w=[bTBLX])
        P.op("sp", I("dma_start", out=TBLX[0:32, :], in_=t5_d), w=[bTBLX], dma=True)
        P.op("sp", I("dma_start", out=NT31[:], in_=t5_d[31:32, :].rearrange("a h -> h a")), w=[bNT31], dma=True)
        P.op("dve", I("tensor_scalar", out=NT31[:], in0=NT31[:], scalar1=-1.0, scalar2=None, op0=ALU.mult),
             r=[bNT31], w=[bNT31])
        P.op("sp", I("dma_start", out=OHD[:], in_=ohd_d), w=[bOHD], dma=True)
        P.op("sp", I("dma_start", out=ANTI[:], in_=anti_d), w=[bANTI], dma=True)
        pt, pb = PSR.next()
        P.op("pe", I("matmul", pt[0:8, 0:384], lhsT=TBLX[:, :], rhs=OHD[:, :], start=True, stop=True),
             r=[bTBLX, bOHD], w=[pb])
        GT = sb(SU, "GT", [8, 384]); bGT = Buf("GT")
        P.op("act", I("activation", out=GT[:], in_=pt[0:8, 0:384], func=AF.Exp, bias=NT31[:, 0:1], scale=1.0),
             r=[pb, bNT31], w=[bGT])
        P.op("pool", I("dma_start", out=GDs, in_=GT[:]), r=[bGT], w=[bGDs], dma=True)
        EMF = sb(SU, "EMF", [128, 256]); bEMF = Buf("EMF")
        EMFb = sb(SU, "EMFb", [128, 256], BF16); bEMFb = Buf("EMFb")
        for h in range(8):
            hank = bass.AP(tensor=GD_h, offset=h * 384, ap=[[1, 128], [1, 256]])
            P.op("sp", I("dma_start", out=EMF[:], in_=hank), r=[bGDs], w=[bEMF], dma=True)
            P.op("dve", I("tensor_copy", out=EMFb[:], in_=EMF[:]), r=[bEMF], w=[bEMFb])
            pt, pb = PSR.next()
            P.op("pe", I("matmul", pt[:, 0:256], lhsT=ANTI[:, :], rhs=EMFb[:, :], start=True, stop=True),
                 r=[bANTI, bEMFb], w=[pb])
            P.op("act", I("activation", out=EM[:, h, :], in_=pt[:, 0:256], func=AF.Copy), r=[pb], w=[bEM])
        OHC = [(sb(SU, "OHC%d" % i, [33, 512]), Buf("OHC%d" % i)) for i in range(2)]
        HCT = [(sb(SU, "HCT%d" % i, [8, 512], BF16), Buf("HCT%d" % i)) for i in range(2)]
        OR_, HR_ = Ring(OHC), Ring(HCT)
        for ch in range(16):
            n = min(512, HCL - ch * 512)
            ot, ob = OR_.next()
            ht, hb = HR_.next()
            P.op("sp", I("dma_start", out=ot[:, 0:n], in_=ohc_d[:, ch * 512:ch * 512 + n]), w=[ob], dma=True)
            pt, pb = PSR.next()
            P.op("pe", I("matmul", pt[0:8, 0:n], lhsT=TBLX[:, :], rhs=ot[:, 0:n], start=True, stop=True),
                 r=[bTBLX, ob], w=[pb])
            P.op("dve", I("tensor_copy", out=ht[:, 0:n], in_=pt[0:8, 0:n]), r=[pb], w=[hb])
            P.op("pool", I("dma_start", out=HCs[:, ch * 512:ch * 512 + n], in_=ht[:, 0:n]), r=[hb], w=[bHCs], dma=True)
        P.barrier()

    def stats_rstd(stack_tiles, src_sq_aps, nfeat, rbuf_list, RSTD, bRSTD):
        pt, pb = PSR.next()
        n = len(src_sq_aps)
        for i, (ap_, b_) in enumerate(src_sq_aps):
            P.op("pe", I("matmul", pt[:, :], lhsT=ONES[:, :], rhs=ap_, start=(i == 0), stop=(i == n - 1)),
                 r=[bONES, b_], w=[pb])
        P.op("act", I("activation", out=RSTD[:], in_=pt[:, :], func=AF.Sqrt, bias=EPSB[:, 0:1], scale=1.0 / nfeat),
             r=[pb, bEPSB], w=[bRSTD])
        P.op("dve", I("reciprocal", out=RSTD[:], in_=RSTD[:]), r=[bRSTD], w=[bRSTD])

    def phase_P(b):
        with ExitStack() as L:
            XR = [(sb(L, "X%d" % i, [128, 8, 512]), Buf("X%d" % i)) for i in range(2)]
            XNR = [(sb(L, "XN%d" % i, [128, 8, 512], BF16), Buf("XN%d" % i)) for i in range(2)]
            RSR = [(sb(L, "RSTD%d" % i, [128, 512]), Buf("RSTD%d" % i)) for i in range(2)]
            RPR = [(sb(L, "ROPE%d" % i, [96, 2, 512]), Buf("ROPE%d" % i)) for i in range(2)]
            SQ = sb(L, "SQ", [128, 8, 512], BF16); bSQ = Buf("SQ")
            RQ = sb(L, "RQ", [128, 512]); bRQ = Buf("RQ")
            RKV = sb(L, "RKV", [128, 512]); bRKV = Buf("RKV")
            CQf = sb(L, "CQf", [128, 2, 512]); bCQf = Buf("CQf")
            CQs = sb(L, "CQs", [128, 2, 512], BF16); bCQs = Buf("CQs")
            CQN = sb(L, "CQN", [128, 2, 512], BF16); bCQN = Buf("CQN")
            CKf = sb(L, "CKf", [128, 512]); bCKf = Buf("CKf")
            CKs = sb(L, "CKs", [128, 512], BF16); bCKs = Buf("CKs")
            CKN = sb(L, "CKN", [128, 512], BF16); bCKN = Buf("CKN")
            T1 = sb(L, "T1", [96, 512]); bT1 = Buf("T1")
            T2 = sb(L, "T2", [96, 512]); bT2 = Buf("T2")
            KPE = sb(L, "KPE", [96, 512], BF16); bKPE = Buf("KPE")
            QM = sb(L, "QM", [96, 8, 512], BF16); bQM = Buf("QM")
            KM = sb(L, "KM", [96, 8, 512], BF16); bKM = Buf("KM")
            VMR = Ring([(sb(L, "VM%d" % i, [128, 8, 4, 65], BF16), Buf("VM%d" % i)) for i in range(2)])
            QN = sb(L, "QN", [128, 4, 512], BF16); bQN = Buf("QN")
            KSR = Ring([(sb(L, "KS%d" % i, [128, 2, 512], BF16), Buf("KS%d" % i)) for i in range(2)])
            VSR = Ring([(sb(L, "VS%d" % i, [128, 4, 4, 65], BF16), Buf("VS%d" % i)) for i in range(2)])
            for (t_, b_) in VMR.items + VSR.items:
                P.op("pool", I("memset", t_[:], 1.0), w=[b_])
            xv = xT[b].rearrange("(c p) t -> p c t", p=128)

            def chain1(tb):
                t0 = tb * 512
                X, bX = XR[tb % 2]
                RSTD, bRSTD = RSR[tb % 2]
                ROPE, bROPE = RPR[tb % 2]
                P.op("sp", I("dma_start", out=X[:], in_=xv[:, :, t0:t0 + 512]), w=[bX], dma=True)
                P.op("sp", I("dma_start", out=ROPE[64:96, :, :], in_=rope_d[64:96, 0:2, t0:t0 + 512]), w=[bROPE], dma=True)
                P.op("act", I("activation", out=SQ[:], in_=X[:], func=AF.Square), r=[bX], w=[bSQ])
                stats_rstd(None, [(SQ[:, c, :], bSQ) for c in range(8)], 1024.0, None, RSTD, bRSTD)

            def chain2(tb):
                X, bX = XR[tb % 2]
                XN, bXN = XNR[tb % 2]
                RSTD, bRSTD = RSR[tb % 2]
                for c in range(8):
                    P.op("dve", I("scalar_tensor_tensor", out=XN[:, c, :], in0=X[:, c, :], scalar=GV[:, c:c + 1],
                                  in1=RSTD[:], op0=ALU.mult, op1=ALU.mult), r=[bX, bGV, bRSTD], w=[bXN])

            def body(tb, part):
                t0 = tb * 512
                XN, bXN = XNR[tb % 2]
                ROPE, bROPE = RPR[tb % 2]

                def proj(cols, M, wt=W_in, wb=bW_in):
                    pt, pb = PSR.next()
                    for c in range(8):
                        P.op("pe", I("matmul", pt[0:M, :], lhsT=wt[:, c, cols[0]:cols[1]], rhs=XN[:, c, :],
                                     start=(c == 0), stop=(c == 7)), r=[wb, bXN], w=[pb])
                    return pt, pb

                if part == 0:
                    for j in range(2):
                        pt, pb = proj((j * 128, (j + 1) * 128), 128)
                        P.op("act", I("activation", out=CQf[:, j, :], in_=pt[:, :], func=AF.Copy), r=[pb], w=[bCQf])
                        P.op("act", I("activation", out=CQs[:, j, :], in_=pt[:, :], func=AF.Square), r=[pb], w=[bCQs])
                    pt, pb = proj((256, 384), 128)
                    P.op("act", I("activation", out=CKf[:], in_=pt[:, :], func=AF.Copy), r=[pb], w=[bCKf])
                    P.op("act", I("activation", out=CKs[:], in_=pt[:, :], func=AF.Square), r=[pb], w=[bCKs])
                    pt, pb = proj((928, 1056), 128)
                    P.op("act", I("activation", out=KCR[:, t0:t0 + 512], in_=pt[:, :], func=AF.Copy), r=[pb], w=[bKCR])
                    pt, pb = proj((1056, 1184), 128)
                    P.op("dve", I("tensor_copy", out=VCR[:, t0:t0 + 512], in_=pt[:, :]), r=[pb], w=[bVCR])
                    stats_rstd(None, [(CQs[:, j, :], bCQs) for j in range(2)], 256.0, None, RQ, bRQ)
                    for j in range(2):
                        P.op("dve", I("scalar_tensor_tensor", out=CQN[:, j, :], in0=CQf[:, j, :], scalar=GV[:, 24 + j:25 + j],
                                      in1=RQ[:], op0=ALU.mult, op1=ALU.mult), r=[bCQf, bGV, bRQ], w=[bCQN])
                    stats_rstd(None, [(CKs[:], bCKs)], 128.0, None, RKV, bRKV)
                    P.op("dve", I("scalar_tensor_tensor", out=CKN[:], in0=CKf[:], scalar=GV[:, 26:27],
                                  in1=RKV[:], op0=ALU.mult, op1=ALU.mult), r=[bCKf, bGV, bRKV], w=[bCKN])
                    for j in range(4):
                        pt, pb = proj((416 + j * 128, 416 + (j + 1) * 128), 128)
                        if j % 2 == 0:
                            P.op("act", I("activation", out=QN[:, j, :], in_=pt[:, :], func=AF.Copy, scale=0.125),
                                 r=[pb], w=[bQN])
                        else:
                            P.op("dve", I("tensor_scalar", out=QN[:, j, :], in0=pt[:, :], scalar1=0.125, scalar2=None,
                                          op0=ALU.mult), r=[pb], w=[bQN])
                    P.op("pool", I("dma_start", out=QNs.rearrange("(j two) d t -> (two d) j t", two=2)[:, :, t0:t0 + 512],
                                   in_=QN[:]), r=[bQN], w=[bQNs], dma=True, disjoint=True)
                    KS, bKS = KSR.next()
                    for i, c0 in enumerate((1184, 1440)):
                        pt, pb = proj((c0, c0 + 128), 128)
                        if i % 2 == 0:
                            P.op("act", I("activation", out=KS[:, i, :], in_=pt[:, :], func=AF.Copy), r=[pb], w=[bKS])
                        else:
                            P.op("dve", I("tensor_copy", out=KS[:, i, :], in_=pt[:, :]), r=[pb], w=[bKS])
                    P.op("pool", I("dma_start", out=KSs.rearrange("k d t -> (k d) t")[:, t0:t0 + 512], in_=KS[:, 0, :]),
                         r=[bKS], w=[bKSs], dma=True, disjoint=True)
                    P.op("pool", I("dma_start", out=KWs.rearrange("k d t -> (k d) t")[:, t0:t0 + 512], in_=KS[:, 1, :]),
                         r=[bKS], w=[bKWs], dma=True, disjoint=True)
                    return
                pa, pab = proj((0, 96), 96, wt=WkrA, wb=bWkrA)
                pbb_, pbbb = proj((0, 96), 96, wt=WkrB, wb=bWkrB)
                P.op("dve", I("tensor_tensor", out=T1[64:96, :], in0=pa[64:96, :], in1=ROPE[64:96, 0, :], op=ALU.mult),
                     r=[pab, bROPE], w=[bT1])
                P.op("dve", I("tensor_tensor", out=T2[64:96, :], in0=pbb_[64:96, :], in1=ROPE[64:96, 1, :], op=ALU.mult),
                     r=[pbbb, bROPE], w=[bT2])
                P.op("dve", I("tensor_tensor", out=KPE[64:96, :], in0=T1[64:96, :], in1=T2[64:96, :], op=ALU.add),
                     r=[bT1, bT2], w=[bKPE])
                for h in range(8):
                    pa, pab = PSR.next()
                    for j in range(2):
                        P.op("pe", I("matmul", pa[0:96, :], lhsT=W_uq[:, j, h * 96:(h + 1) * 96], rhs=CQN[:, j, :],
                                     start=(j == 0), stop=(j == 1)), r=[bW_uq, bCQN], w=[pab])
                    pq, pqb = PSR.next()
                    for j in range(2):
                        P.op("pe", I("matmul", pq[0:96, :], lhsT=W_uqB[:, j, h, :], rhs=CQN[:, j, :],
                                     start=(j == 0), stop=(j == 1)), r=[bW_uqB, bCQN], w=[pqb])
                    P.op("act", I("activation", out=QM[0:64, h, :], in_=pa[0:64, :], func=AF.Copy, scale=SC_M),
                         r=[pab], w=[bQM])
                    P.op("dve", I("scalar_tensor_tensor", out=T1[64:96, :], in0=pa[64:96, :], scalar=SC_M,
                                  in1=ROPE[64:96, 0, :], op0=ALU.mult, op1=ALU.mult), r=[pab, bROPE], w=[bT1])
                    P.op("dve", I("scalar_tensor_tensor", out=T2[64:96, :], in0=pq[64:96, :], scalar=SC_M,
                                  in1=ROPE[64:96, 1, :], op0=ALU.mult, op1=ALU.mult), r=[pqb, bROPE], w=[bT2])
                    P.op("dve", I("tensor_tensor", out=QM[64:96, h, :], in0=T1[64:96, :], in1=T2[64:96, :], op=ALU.add),
                         r=[bT1, bT2], w=[bQM])
                    pk, pkb = PSR.next()
                    P.op("pe", I("matmul", pk[:, :], lhsT=W_ukv[:, h * 128:(h + 1) * 128], rhs=CKN[:, :],
                                 start=True, stop=True), r=[bW_ukv, bCKN], w=[pkb])
                    P.op("act", I("activation", out=KM[0:64, h, :], in_=pk[0:64, :], func=AF.Copy), r=[pkb], w=[bKM])
                    P.op("pool", I("tensor_copy", out=KM[64:96, h, :], in_=KPE[64:96, :]), r=[bKPE], w=[bKM])
                P.op("pool", I("dma_start", out=QMs.rearrange("h d t -> d h t")[:, :, t0:t0 + 512], in_=QM[:]),
                     r=[bQM], w=[bQMs], dma=True, disjoint=True)
                P.op("pool", I("dma_start", out=KMs.rearrange("h d t -> d h t")[:, :, t0:t0 + 512], in_=KM[:]),
                     r=[bKM], w=[bKMs], dma=True, disjoint=True)
                VM, bVM = VMR.next()
                wv4 = W_ukv[:, :].rearrange("p (h e) -> p h e", e=128)
                for ts in range(4):
                    pt, pb = PSR.next()
                    P.op("pe", I("matmul", pt[:, :], lhsT=CKN[:, ts * 128:(ts + 1) * 128], rhs=wv4[:, :, 64:128],
                                 start=True, stop=True), r=[bCKN, bW_ukv], w=[pb])
                    P.op("act", I("activation", out=VM[:, :, ts, 0:64], in_=pt[:, :].rearrange("p (h e) -> p h e", e=64),
                                  func=AF.Copy), r=[pb], w=[bVM])
                for h in range(8):
                    P.op("pool", I("dma_start", out=VMs[h, :, tb * 4:tb * 4 + 4, :], in_=VM[:, h, :, :]),
                         r=[bVM], w=[bVMs], dma=True, disjoint=True)
                VS, bVS = VSR.next()
                for ts in range(4):
                    pt, pb = PSR.next()
                    for c in range(8):
                        P.op("pe", I("matmul", pt[:, 0:128], lhsT=XN[:, c, ts * 128:(ts + 1) * 128], rhs=W_in[:, c, 1312:1440],
                                     start=(c == 0), stop=(c == 7)), r=[bXN, bW_in], w=[pb])
                    P.op("act", I("activation", out=VS[:, 0:2, ts, 0:64],
                                  in_=pt[:, 0:128].rearrange("p (k e) -> p k e", e=64), func=AF.Copy), r=[pb], w=[bVS])
                    pt, pb = PSR.next()
                    for c in range(8):
                        P.op("pe", I("matmul", pt[:, 0:152], lhsT=XN[:, c, ts * 128:(ts + 1) * 128], rhs=W_in[:, c, 1568:1720],
                                     start=(c == 0), stop=(c == 7)), r=[bXN, bW_in], w=[pb])
                    P.op("dve", I("tensor_copy", out=VS[:, 2:4, ts, 0:64],
                                  in_=pt[:, 0:128].rearrange("p (k e) -> p k e", e=64)), r=[pb], w=[bVS])
                    P.op("act", I("activation", out=GATES[:, tb * 4 + ts, :], in_=pt[:, 128:152], func=AF.Sigmoid),
                         r=[pb], w=[bGATES])
                for k_ in range(2):
                    P.op("pool", I("dma_start", out=VSs[k_, :, tb * 4:tb * 4 + 4, :], in_=VS[:, k_, :, :]),
                         r=[bVS], w=[bVSs], dma=True, disjoint=True)
                    P.op("pool", I("dma_start", out=VWs[k_, :, tb * 4:tb * 4 + 4, :], in_=VS[:, 2 + k_, :, :]),
                         r=[bVS], w=[bVWs], dma=True, disjoint=True)

            chain1(0)
            chain2(0)
            for tb in range(NTB):
                body(tb, 0)
                if tb + 1 < NTB:
                    chain1(tb + 1)
                body(tb, 1)
                if tb + 1 < NTB:
                    chain2(tb + 1)
            P.barrier()

    def phase_C():
        with ExitStack() as L:
            BIA = sb(L, "BIA", [128, 1]); bBIA = Buf("BIA")
            Hf = sb(L, "Hf", [128, 256]); bHf = Buf("Hf")
            H2 = sb(L, "H2", [128, 256]); bH2 = Buf("H2")
            SG = sb(L, "SG", [128, 256]); bSG = Buf("SG")
            GH = sb(L, "GH", [128, 256], BF16); bGH = Buf("GH")
            W1 = [sb(L, "W1k", [128, 32, 128], BF16), sb(L, "W1v", [128, 32, 128], BF16)]
            bW1 = [Buf("W1k"), Buf("W1v")]
            stg_ = sb(L, "cstg", [128, 4096]); sbuf_ = Buf("cstg")
            sv = stg_[:, :].rearrange("p (l h) -> p l h", h=128)
            for kv, wd in enumerate((w1k_d, w1v_d)):
                src = wd.rearrange("(l d) h -> d l h", d=64)
                for half in range(2):
                    P.op("sp", I("dma_start", out=sv[half * 64:(half + 1) * 64, :, :], in_=src), w=[sbuf_], dma=True)
                P.op("dve", I("tensor_copy", out=W1[kv][:, :, :], in_=sv[:, :, :]), r=[sbuf_], w=[bW1[kv]])
            P.op("pool", I("memset", VCA[:], 0.0), w=[bVCA])
            P.op("pool", I("memset", KCT[:], 0.0), w=[bKCT])
            for kv in range(2):
                RAW, bRAW = (KCR, bKCR) if kv == 0 else (VCR, bVCR)
                pbia, pbiab = PSR.next()
                for l in range(32):
                    P.op("pe", I("matmul", pbia[:, 0:1], lhsT=W1[kv][0:64, l, :], rhs=POS[kv][0:64, l:l + 1],
                                 start=(l == 0), stop=(l == 31)), r=[bW1[kv], bPOS[kv]], w=[pbiab])
                P.op("dve", I("tensor_copy", out=BIA[:], in_=pbia[:, 0:1]), r=[pbiab], w=[bBIA])
                for kh in range(2):
                    p0 = kh * 64
                    pt, pb = PSR.next()
                    for l in range(32):
                        P.op("pe", I("matmul", pt[:, 0:255], lhsT=W1[kv][p0:p0 + 64, l, :],
                                     rhs=RAW[p0:p0 + 64, l:l + 16 * 254 + 1:16], start=(l == 0), stop=(l == 31)),
                             r=[bW1[kv], bRAW], w=[pb])
                    P.op("act", I("activation", out=Hf[:, 0:255], in_=pt[:, 0:255], func=AF.Identity, bias=BIA[:, 0:1],
                                  scale=1.0), r=[pb, bBIA], w=[bHf])
                    P.op("dve", I("tensor_tensor", out=H2[:, 0:255], in0=Hf[:, 0:255], in1=Hf[:, 0:255], op=ALU.mult),
                         r=[bHf], w=[bH2])
                    P.op("dve", I("tensor_scalar", out=H2[:, 0:255], in0=H2[:, 0:255], scalar1=0.044715, scalar2=1.0,
                                  op0=ALU.mult, op1=ALU.add), r=[bH2], w=[bH2])
                    P.op("dve", I("tensor_tensor", out=H2[:, 0:255], in0=H2[:, 0:255], in1=Hf[:, 0:255], op=ALU.mult),
                         r=[bH2, bHf], w=[bH2])
                    P.op("act", I("activation", out=SG[:, 0:255], in_=H2[:, 0:255], func=AF.Sigmoid,
                                  scale=2.0 * math.sqrt(2.0 / math.pi)), r=[bH2], w=[bSG])
                    P.op("pool", I("memset", GH[:], 0.0), w=[bGH])
                    rev = bass.AP(tensor=GH, offset=GH[:, 255:256].offset, ap=[list(GH[:].ap[0]), [-1, 255]])
                    P.op("dve", I("tensor_tensor", out=rev, in0=SG[:, 0:255], in1=Hf[:, 0:255], op=ALU.mult),
                         r=[bSG, bHf], w=[bGH])
                    if kv == 0:
                        pt, pb = PSR.next()
                        P.op("pe", I("matmul", pt[0:64, 0:256], lhsT=W2[0][:, :], rhs=GH[:, :], start=True, stop=True),
                             r=[bW2[0], bGH], w=[pb])
                        P.op("act", I("activation", out=KCT[:, kh, :], in_=pt[0:64, 0:256], func=AF.Copy),
                             r=[pb], w=[bKCT])
                    else:
                        for nt in range(2):
                            pt, pb = PSR.next()
                            P.op("pe", I("matmul", pt[:, 0:64], lhsT=GH[:, nt * 128:(nt + 1) * 128], rhs=W2[1][:, :],
                                         start=True, stop=True), r=[bW2[1], bGH], w=[pb])
                            P.op("act", I("activation", out=VCA[:, kh, nt, 0:64], in_=pt[:, 0:64], func=AF.Copy),
                                 r=[pb], w=[bVCA])
            P.op("pool", I("memset", VCA[:, :, 1, 64:65], 1.0), w=[bVCA])
            P.op("pool", I("memset", VCA[:, :, 0, 64:65], 1.0), w=[bVCA])
            P.op("pool", I("memset", VCA[0:1, :, 0, :], 0.0), w=[bVCA])
            P.barrier()

    def run_attention(L, jobs):
        SR_ = Ring([(ps[i], psb[i]) for i in range(0, 4)])
        OR_ = Ring([(ps[i], psb[i]) for i in range(4, 6)])
        PTR = Ring([(sb(L, "PT%d" % i, [128, 512], BF16), Buf("PT%d" % i)) for i in range(5)])
        flat = []
        for ji, job in enumerate(jobs):
            nt_ = len(job["tiles"])
            job["ji"] = ji
            for ti in range(nt_):
                flat.append((job, ti, ti == 0, ti == nt_ - 1))
        state = {}
        loaded = set()
        LOOK = 24

        def stage1(i):
            job, ti, first, last = flat[i]
            for k2 in range(i, min(len(flat), i + LOOK)):
                j2 = flat[k2][0]
                if j2["ji"] > job["ji"] + 3:
                    break
                if id(j2) not in loaded:
                    loaded.add(id(j2))
                    if j2["load"] is not None:
                        j2["load"]()
            tl = job["tiles"][ti]
            kT, kb, vA, vb, lo, hi, masks = tl
            qap, qb_ = job["q"]
            st_, sbf = SR_.next()
            ptile, pbf = PTR.next()
            c0, c1 = lo * 128, hi * 128
            P.op("pe", I("matmul", st_[:, c0:c1], lhsT=kT, rhs=qap[:, c0:c1], start=True, stop=True),
                 r=[kb, qb_], w=[sbf])
            P.op("act", I("activation", out=ptile[:, c0:c1], in_=st_[:, c0:c1], func=AF.Exp, bias=job["bias"], scale=1.0),
                 r=[sbf] + job["rbias"], w=[pbf])
            for (qs, map_, mb) in masks:
                P.op("dve", I("tensor_tensor", out=ptile[:, qs * 128:(qs + 1) * 128], in0=ptile[:, qs * 128:(qs + 1) * 128],
                              in1=map_, op=ALU.mult), r=[pbf, mb], w=[pbf])
            state[i] = (ptile, pbf)

        OSR = Ring([(sb(L, "OSb%d" % i, [128, 512]), Buf("OSb%d" % i)) for i in range(2)])
        for (t_, b_) in OSR.items:
            P.op("pool", I("memset", t_[:], 0.0), w=[b_])
        TR_ = Ring([(ps[i], psb[i]) for i in range(6, 7)])
        DUMW = sb(L, "DUMW", [128, 512], BF16); bDUMW = Buf("DUMW")
        P.op("pool", I("memset", DUMW[:], 0.5), w=[bDUMW])
        bDUM = Buf("DUM")
        pending = []

        def stage2(i):
            job, ti, first, last = flat[i]
            kT, kb, vA, vb, lo, hi, masks = job["tiles"][ti]
            ptile, pbf = state.pop(i)
            if first:
                job["O"] = OR_.next()
                job["started"] = False
            O, Ob = job["O"]
            c0, c1 = lo * 128, hi * 128
            if WARM_N:
                P.op("pe", I("matmul", ps[7][:, 0:WARM_N], lhsT=ONES[:, :], rhs=DUMW[:, 0:WARM_N], start=True, stop=True),
                     r=[bONES, bDUMW], w=[bDUM])
            P.op("pe", I("matmul", O[0:65, c0:c1], lhsT=vA, rhs=ptile[:, c0:c1],
                         start=(not job["started"]), stop=last, skip_group_check=True), r=[pbf, vb], w=[Ob])
            job["started"] = True
            if last:
                OS_, bOS_ = OSR.next()
                P.op("dve", I("tensor_copy", out=OS_[0:65, :], in_=O[0:65, :]), r=[Ob], w=[bOS_])

                def fin(job=job, OS_=OS_, bOS_=bOS_):
                    Tt, Tb = TR_.next()
                    Tv = Tt[:, :].rearrange("p (s e) -> p s e", e=128)
                    for qs in range(4):
                        P.op("pe", I("transpose", out=Tv[:, qs, :], in_=OS_[:, qs * 128:(qs + 1) * 128],
                                     identity=IDN[:, :]), r=[bOS_, bIDN], w=[Tb])
                    job["evac"](Tv, Tb)
                pending.append((i + LAG + 2, fin))

        n = len(flat)
        for i in range(n + LAG):
            if i < n:
                stage1(i)
            if i >= LAG:
                stage2(i - LAG)
            while pending and pending[0][0] <= i:
                pending.pop(0)[1]()
        while pending:
            pending.pop(0)[1]()

    def std_evac(L, name):
        RR = Ring([(sb(L, name + "R%d" % i, [128, 4]), Buf(name + "R%d" % i)) for i in range(2)])
        OTR = Ring([(sb(L, name + "OT%d" % i, [128, 4, 64]), Buf(name + "OT%d" % i)) for i in range(2)])

        def mk(dst, dbuf, h, qb, gate_col):
            def evac(Ov, Ob):
                R, bR = RR.next()
                OT, bOT = OTR.next()
                P.op("dve", I("reciprocal", out=R[:, :], in_=Ov[:, :, 64]), r=[Ob], w=[bR])
                for qs in range(4):
                    if gate_col is None:
                        P.op("dve", I("tensor_scalar", out=OT[:, qs, :], in0=Ov[:, qs, 0:64], scalar1=R[:, qs:qs + 1],
                                      scalar2=None, op0=ALU.mult), r=[Ob, bR], w=[bOT])
                    else:
                        g = GATES[:, qb * 4 + qs, gate_col:gate_col + 1]
                        P.op("dve", I("tensor_scalar", out=OT[:, qs, :], in0=Ov[:, qs, 0:64], scalar1=R[:, qs:qs + 1],
                                      scalar2=g, op0=ALU.mult, op1=ALU.mult), r=[Ob, bR, bGATES], w=[bOT])
                dv = dst.rearrange("(t p) c -> p t c", p=128)[:, qb * 4:qb * 4 + 4, h * 64:(h + 1) * 64]
                P.op("pool", I("dma_start", out=dv, in_=OT[:]), r=[bOT], w=[dbuf], dma=True, disjoint=True)
            return evac
        return mk

    def phase_MLA():
        with ExitStack() as L:
            KR = Ring([(sb(L, "K%d" % i, [96, S], BF16), Buf("K%d" % i)) for i in range(2)])
            VR = Ring([(sb(L, "V%d" % i, [128, NT, 65], BF16), Buf("V%d" % i)) for i in range(2)])
            QR = Ring([(sb(L, "Q%d" % i, [96, 512], BF16), Buf("Q%d" % i)) for i in range(6)])
            mk = std_evac(L, "m")
            jobs = []
            for h in range(8):
                kv = {}
                for qb in range(NTB):
                    job = {}

                    def load(h=h, qb=qb, job=job, kv=kv):
                        if qb == 0:
                            kv["K"] = KR.next()
                            kv["V"] = VR.next()
                            P.op("sp", I("dma_start", out=kv["K"][0][:], in_=KMs[h]), r=[bKMs], w=[kv["K"][1]], dma=True)
                            P.op("sp", I("dma_start", out=kv["V"][0][:], in_=VMs[h]), r=[bVMs], w=[kv["V"][1]], dma=True)
                        Q, bQ = QR.next()
                        P.op("sp", I("dma_start", out=Q[:], in_=QMs[h, :, qb * 512:(qb + 1) * 512]), r=[bQMs], w=[bQ], dma=True)
                        job["q"] = (Q, bQ)
                        K, bK = kv["K"]
                        V, bV = kv["V"]
                        tiles = []
                        for kt in range(4 * qb + 4):
                            lo = max(0, kt - 4 * qb)
                            masks = [(lo, TRI[:, :], bTRI)] if kt >= 4 * qb else []
                            tiles.append((K[:, kt * 128:(kt + 1) * 128], bK, V[:, kt, :], bV, lo, 4, masks))
                        job["tiles"][:] = tiles
                    job["load"] = load
                    job["tiles"] = [None] * (4 * qb + 4)
                    job["bias"] = 0.0
                    job["rbias"] = []
                    job["evac"] = mk(OMs, bOMs, h, qb, None)
                    jobs.append(job)
            run_attention(L, jobs)
            P.barrier()

    def phase_NC():
        with ExitStack() as L:
            QR = Ring([(sb(L, "cQ%d" % i, [64, 512], BF16), Buf("cQ%d" % i)) for i in range(4)])
            BCR = Ring([(sb(L, "BC%d" % i, [128, 512], BF16), Buf("BC%d" % i)) for i in range(4)])
            SSR = Ring([(sb(L, "SS%d" % i, [128, 512]), Buf("SS%d" % i)) for i in range(3)])
            ER = Ring([(sb(L, "E%d" % i, [128, 512], BF16), Buf("E%d" % i)) for i in range(5)])
            RR = Ring([(sb(L, "cR%d" % i, [128, 4]), Buf("cR%d" % i)) for i in range(2)])
            OTR = Ring([(sb(L, "cOT%d" % i, [128, 4, 64]), Buf("cOT%d" % i)) for i in range(2)])
            IMP = sb(L, "IMP", [128, 4, 64]); bIMP = Buf("IMP")
            SELC = Ring([(sb(L, "SELC%d" % i, [128, 4, 128]), Buf("SELC%d" % i)) for i in range(2)])
            M8 = sb(L, "M8", [128, 4, 16]); bM8 = Buf("M8")
            TMPR = Ring([(sb(L, "TMP%d" % i, [128, 4, 64]), Buf("TMP%d" % i)) for i in range(2)])
            NGT = Ring([(sb(L, "NGT%d" % i, [64, 512], BF16), Buf("NGT%d" % i)) for i in range(2)])
            S_R = Ring([(ps[i], psb[i]) for i in range(0, 3)])
            O_R = Ring([(ps[i], psb[i]) for i in range(3, 5)])
            I_R = Ring([(ps[i], psb[i]) for i in range(5, 7)])
            T_R = Ring([(ps[7], psb[7])])
            units = []
            for kvh in range(2):
                for qb in range(NTB):
                    for g in range(4):
                        units.append((kvh, qb, g))
            ctx = {}

            def stageA(u):
                kvh, qb, g = units[u]
                h = kvh * 4 + g
                nts = [1] if qb < 4 else [0, 1]
                if g == 0:
                    SC_, bSC = SELC.next()
                    P.op("sp", I("dma_start", out=SC_[:], in_=selc_d[qb * 4:qb * 4 + 4].rearrange("t p c -> p t c")),
                         w=[bSC], dma=True)
                    ctx[(kvh, qb)] = (SC_, bSC)
                Q, bQ = QR.next()
                P.op("sp", I("dma_start", out=Q[:], in_=QNs[h, :, qb * 512:(qb + 1) * 512]), r=[bQNs], w=[bQ], dma=True)
                es_ = []
                for ni, nt in enumerate(nts):
                    BC, bBC = BCR.next()
                    src = bass.AP(tensor=HC_h, offset=h * HCL + 2048 * nt + 512 * qb, ap=[[16, 128], [1, 512]])
                    P.op("sp", I("dma_start", out=BC[:], in_=src), r=[bHCs], w=[bBC], dma=True)
                    St, Sb = S_R.next()
                    P.op("pe", I("matmul", St[:, :], lhsT=KCT[:, kvh, nt * 128:(nt + 1) * 128], rhs=Q[:, :],
                                 start=True, stop=False), r=[bKCT, bQ], w=[Sb])
                    P.op("pe", I("matmul", St[:, :], lhsT=IDNB[:, :], rhs=BC[:, :], start=False, stop=True),
                         r=[bIDNB, bBC], w=[Sb])
                    E, bE = ER.next()
                    P.op("act", I("activation", out=E[:], in_=St[:, :], func=AF.Exp), r=[Sb], w=[bE])
                    es_.append((nt, E, bE))
                ctx[u] = es_

            def stageB(u):
                kvh, qb, g = units[u]
                h = kvh * 4 + g
                es_ = ctx.pop(u)
                O, Ob = O_R.next()
                Im, Imb = I_R.next()
                Ov = O[:, 0:260].rearrange("p (s e) -> p s e", e=65)
                Iv = Im[:, 0:256].rearrange("p (s e) -> p s e", e=64)
                first = True
                for ni, (nt, E, bE) in enumerate(es_):
                    for qs in range(4):
                        P.op("pe", I("matmul", Ov[:, qs, :], lhsT=E[:, qs * 128:(qs + 1) * 128], rhs=VCA[:, kvh, nt, :],
                                     start=first, stop=(ni == len(es_) - 1), skip_group_check=True),
                             r=[bE, bVCA], w=[Ob])
                        P.op("pe", I("matmul", Iv[:, qs, :], lhsT=E[:, qs * 128:(qs + 1) * 128], rhs=OVL[:, nt, :],
                                     start=first, stop=(ni == len(es_) - 1), skip_group_check=True),
                             r=[bE, bOVL], w=[Imb])
                        first = False
                R, bR = RR.next()
                OT, bOT = OTR.next()
                P.op("dve", I("tensor_scalar", out=R[:, :], in0=Ov[:, :, 64], scalar1=1e-30, scalar2=None,
                              op0=ALU.add), r=[Ob], w=[bR])
                P.op("dve", I("reciprocal", out=R[:, :], in_=R[:, :]), r=[bR], w=[bR])
                for qs in range(4):
                    gcol = GATES[:, qb * 4 + qs, 3 * h:3 * h + 1]
                    P.op("dve", I("tensor_scalar", out=OT[:, qs, :], in0=Ov[:, qs, 0:64], scalar1=R[:, qs:qs + 1],
                                  scalar2=gcol, op0=ALU.mult, op1=ALU.mult), r=[Ob, bR, bGATES], w=[bOT])
                    if g == 0:
                        P.op("dve", I("tensor_scalar", out=IMP[:, qs, :], in0=Iv[:, qs, :], scalar1=R[:, qs:qs + 1],
                                      scalar2=None, op0=ALU.mult), r=[Imb, bR], w=[bIMP])
                    else:
                        P.op("dve", I("scalar_tensor_tensor", out=IMP[:, qs, :], in0=Iv[:, qs, :],
                                      scalar=R[:, qs:qs + 1], in1=IMP[:, qs, :], op0=ALU.mult, op1=ALU.add),
                             r=[Imb, bR, bIMP], w=[bIMP])
                dv = OCs.rearrange("(t p) c -> p t c", p=128)[:, qb * 4:qb * 4 + 4, h * 64:(h + 1) * 64]
                P.op("pool", I("dma_start", out=dv, in_=OT[:]), r=[bOT], w=[bOCs], dma=True, disjoint=True)
                if g == 3:
                    SC_, bSC = ctx.pop((kvh, qb))
                    P.op("dve", I("tensor_tensor", out=IMP[:], in0=IMP[:], in1=SC_[:, :, 0:64], op=ALU.mult),
                         r=[bIMP, bSC], w=[bIMP])
                    P.op("dve", I("tensor_tensor", out=IMP[:], in0=IMP[:], in1=SC_[:, :, 64:128], op=ALU.add),
                         r=[bIMP, bSC], w=[bIMP])
                    TMP, bTMP = TMPR.next()
                    for qs in range(4):
                        P.op("dve", I("max", out=M8[:, qs, 0:8], in_=IMP[:, qs, :]), r=[bIMP], w=[bM8])
                        P.op("dve", I("match_replace", out=TMP[:, qs, :], in_to_replace=M8[:, qs, 0:8], in_values=IMP[:, qs, :],
                                      imm_value=-3.0e38), r=[bM8, bIMP], w=[bTMP])
                        P.op("dve", I("max", out=M8[:, qs, 8:16], in_=TMP[:, qs, :]), r=[bTMP], w=[bM8])
                        P.op("dve", I("tensor_scalar", out=TMP[:, qs, :], in0=IMP[:, qs, :], scalar1=M8[:, qs, 15:16],
                                      scalar2=1.0, op0=ALU.is_ge, op1=ALU.subtract), r=[bIMP, bM8], w=[bTMP])

                    def topk_tail(kvh=kvh, qb=qb, TMP=TMP, bTMP=bTMP):
                        NG, bNG = NGT.next()
                        for qs in range(4):
                            Tt, Tb = T_R.next()
                            P.op("pe", I("transpose", out=Tt[0:64, 0:128], in_=TMP[:, qs, :], identity=IDN[:, :]),
                                 r=[bTMP, bIDN], w=[Tb])
                            P.op("act", I("activation", out=NG[:, qs * 128:(qs + 1) * 128], in_=Tt[0:64, 0:128], func=AF.Copy,
                                          scale=BIG), r=[Tb], w=[bNG])
                        P.op("pool", I("dma_start", out=NGs[kvh, :, qb * 512:(qb + 1) * 512], in_=NG[:]), r=[bNG], w=[bNGs], dma=True, disjoint=True)
                    return topk_tail
                return None

            nu = len(units)
            tail = None
            for u in range(nu + 1):
                if u < nu:
                    stageA(u)
                if tail is not None:
                    tail()
                    tail = None
                if u >= 1:
                    tail = stageB(u - 1)
            if tail is not None:
                tail()
            P.barrier()

    def phase_NSW():
        with ExitStack() as L:
            KSA = Ring([(sb(L, "KSA%d" % i, [128, S], BF16), Buf("KSA%d" % i)) for i in range(2)])
            KWR = Ring([(sb(L, "KW%d" % i, [128, S], BF16), Buf("KW%d" % i)) for i in range(2)])
            for (t_, b_) in KWR.items:
                P.op("pool", I("memset", t_[64:128, :], 0.0), w=[b_])
            VSR = Ring([(sb(L, "VSa%d" % i, [128, NT, 65], BF16), Buf("VSa%d" % i)) for i in range(2)])
            VWR = Ring([(sb(L, "VWa%d" % i, [128, NT, 65], BF16), Buf("VWa%d" % i)) for i in range(2)])
            QR = Ring([(sb(L, "QA%d" % i, [128, 512], BF16), Buf("QA%d" % i)) for i in range(6)])
            mks = std_evac(L, "s")
            mkw = std_evac(L, "w")
            jobs = []
            for kvh in range(2):
                kv = {}
                for g in range(4):
                    h = kvh * 4 + g
                    for qb in range(NTB):
                        js, jw = {}, {}

                        def load(h=h, g=g, kvh=kvh, qb=qb, js=js, jw=jw, kv=kv):
                            if g == 0 and qb == 0:
                                kv["KS"], kv["KW"], kv["VS"], kv["VW"] = KSA.next(), KWR.next(), VSR.next(), VWR.next()
                                P.op("sp", I("dma_start", out=kv["KS"][0][0:64, :], in_=KSs[kvh]), r=[bKSs], w=[kv["KS"][1]], dma=True)
                                P.op("sp", I("dma_start", out=kv["KS"][0][64:128, :], in_=ind_d), w=[kv["KS"][1]], dma=True)
                                P.op("sp", I("dma_start", out=kv["KW"][0][0:64, :], in_=KWs[kvh]), r=[bKWs], w=[kv["KW"][1]], dma=True)
                                P.op("sp", I("dma_start", out=kv["VS"][0][:], in_=VSs[kvh]), r=[bVSs], w=[kv["VS"][1]], dma=True)
                                P.op("sp", I("dma_start", out=kv["VW"][0][:], in_=VWs[kvh]), r=[bVWs], w=[kv["VW"][1]], dma=True)
                            Q, bQ = QR.next()
                            P.op("sp", I("dma_start", out=Q[0:64, :], in_=QNs[h, :, qb * 512:(qb + 1) * 512]), r=[bQNs], w=[bQ], dma=True)
                            P.op("sp", I("dma_start", out=Q[64:128, :], in_=NGs[kvh, :, qb * 512:(qb + 1) * 512]), r=[bNGs], w=[bQ], dma=True)
                            js["q"] = (Q, bQ)
                            jw["q"] = (Q[:, :], bQ)
                            K, bK = kv["KS"]
                            V, bV = kv["VS"]
                            tiles = []
                            for kt in range(4 * qb + 4):
                                lo = max(0, kt - 4 * qb)
                                masks = []
                                for qs in range(lo, 4):
                                    qt = 4 * qb + qs
                                    if kt == qt:
                                        masks.append((qs, EM[:, h, 0:128], bEM))
                                    elif kt == qt - 1:
                                        masks.append((qs, EM[:, h, 128:256], bEM))
                                tiles.append((K[:, kt * 128:(kt + 1) * 128], bK, V[:, kt, :], bV, lo, 4, masks))
                            js["tiles"][:] = tiles
                            K, bK = kv["KW"]
                            V, bV = kv["VW"]
                            tiles = []
                            for kt in range(max(0, 4 * qb - 4), 4 * qb + 4):
                                lo = max(0, kt - 4 * qb)
                                hi = min(4, kt - 4 * qb + 5)
                                masks = []
                                for qs in range(lo, hi):
                                    qt = 4 * qb + qs
                                    if kt == qt:
                                        masks.append((qs, EM[:, h, 0:128], bEM))
                                    elif kt == qt - 1:
                                        masks.append((qs, EM[:, h, 128:256], bEM))
                                    elif kt == qt - 4:
                                        masks.append((qs, FARM[:, :], bFARM))
                                tiles.append((K[:, kt * 128:(kt + 1) * 128], bK, V[:, kt, :], bV, lo, hi, masks))
                            jw["tiles"][:] = tiles
                        js["load"] = load
                        jw["load"] = None
                        js["tiles"] = [None] * (4 * qb + 4)
                        jw["tiles"] = [None] * (4 * qb + 4 - max(0, 4 * qb - 4))
                        for j_, mk_, dst, dbuf, col in ((js, mks, OSs, bOSs, 3 * h + 1), (jw, mkw, OWs, bOWs, 3 * h + 2)):
                            j_["bias"] = T31B[:, h:h + 1]
                            j_["rbias"] = [bT31B]
                            j_["evac"] = mk_(dst, dbuf, h, qb, col)
                            jobs.append(j_)
            run_attention(L, jobs)
            P.barrier()

    def phase_M(b):
        with ExitStack() as L:
            OMR = Ring([(sb(L, "OM%d" % i, [128, 512]), Buf("OM%d" % i)) for i in range(4)])
            ONR = Ring([(sb(L, "ON%d" % i, [128, 3, 512]), [Buf("ON%d_%d" % (i, j)) for j in range(3)]) for i in range(4)])
            MXR = Ring([(sb(L, "MX%d" % i, [128, 1024]), Buf("MX%d" % i)) for i in range(3)])
            JNKR = Ring([(sb(L, "JNK%d" % i, [128, 512]), Buf("JNK%d" % i)) for i in range(2)])
            SSQR = Ring([(sb(L, "SSQ%d" % i, [128, 2]), Buf("SSQ%d" % i)) for i in range(4)])
            MXT = Ring([(sb(L, "MXT%d" % i, [128, 8, 512], BF16), Buf("MXT%d" % i)) for i in range(2)])
            XR = Ring([(sb(L, "Xm%d" % i, [128, 8, 512]), Buf("Xm%d" % i)) for i in range(2)])
            W_out = sb(L, "W_out", [128, 8, 1024], BF16); bW_out = Buf("W_out")
            GOUT = sb(L, "GOUT", [128, 1024]); bGOUT = Buf("GOUT")
            MSR = Ring([(sb(L, "mstg%d" % i, [128, 1024]), Buf("mstg%d" % i)) for i in range(2)])
            wv = w_out_d.rearrange("(c p) n -> p c n", p=128)
            for c in range(8):
                load_cast(L, W_out[:, c, :], wv[:, c, :], bW_out, 128, 1024, MSR, c)
            P.op("sp", I("dma_start", out=GOUT[:], in_=gout_d), w=[bGOUT], dma=True)
            xv = xT[b].rearrange("(c p) t -> p c t", p=128)
            hv = HTs[b].rearrange("(c p) t -> p c t", p=128)
            for tb in range(NTB):
                MT, bMT = MXT.next()
                X, bX = XR.next()
                P.op("sp", I("dma_start", out=X[:], in_=xv[:, :, tb * 512:(tb + 1) * 512]), w=[bX], dma=True)
                for ts in range(4):
                    t = tb * 4 + ts
                    OM, bOM = OMR.next()
                    ON, bONs = ONR.next()
                    MX, bMX = MXR.next()
                    SSQ, bSSQ = SSQR.next()
                    P.op("sp", I("dma_start", out=OM[:], in_=OMs[t * 128:(t + 1) * 128, :]), r=[bOMs], w=[bOM], dma=True)
                    for i, (src, sbuf_) in enumerate(((OCs, bOCs), (OSs, bOSs), (OWs, bOWs))):
                        P.op(("act", "sp", "act")[i], I("dma_start", out=ON[:, i, :], in_=src[t * 128:(t + 1) * 128, :]),
                             r=[sbuf_], w=[bONs[i]], dma=True)
                    bON = bONs[0]
                    P.op("pool", I("tensor_tensor", out=ON[:, 0, :], in0=ON[:, 0, :], in1=ON[:, 1, :], op=ALU.add), r=[bONs[0], bONs[1]], w=[bONs[0]])
                    P.op("pool", I("tensor_tensor", out=ON[:, 0, :], in0=ON[:, 0, :], in1=ON[:, 2, :], op=ALU.add), r=[bONs[0], bONs[2]], w=[bONs[0]])
                    JNK, bJNK = JNKR.next()
                    P.op("act", I("activation", out=JNK[:], in_=OM[:], func=AF.Square, accum_out=SSQ[:, 0:1]), r=[bOM], w=[bJNK, bSSQ])
                    JNK, bJNK = JNKR.next()
                    P.op("act", I("activation", out=JNK[:], in_=ON[:, 0, :], func=AF.Square, accum_out=SSQ[:, 1:2]), r=[bON], w=[bJNK, bSSQ])
                    P.op("act", I("activation", out=SSQ[:], in_=SSQ[:], func=AF.Sqrt, bias=EPSB[:, 0:1], scale=1.0 / 512.0),
                         r=[bSSQ, bEPSB], w=[bSSQ])
                    P.op("dve", I("reciprocal", out=SSQ[:], in_=SSQ[:]), r=[bSSQ], w=[bSSQ])
                    P.op("dve", I("scalar_tensor_tensor", out=MX[:, 0:512], in0=OM[:], scalar=SSQ[:, 0:1], in1=GOUT[:, 0:512],
                                  op0=ALU.mult, op1=ALU.mult), r=[bOM, bSSQ, bGOUT], w=[bMX])
                    P.op("dve", I("scalar_tensor_tensor", out=MX[:, 512:1024], in0=ON[:, 0, :], scalar=SSQ[:, 1:2],
                                  in1=GOUT[:, 512:1024], op0=ALU.mult, op1=ALU.mult), r=[bON, bSSQ, bGOUT], w=[bMX])
                    for half in range(2):
                        pt, pb = PSR.next()
                        for c4 in range(4):
                            c = half * 4 + c4
                            P.op("pe", I("transpose", out=pt[:, c4 * 128:(c4 + 1) * 128], in_=MX[:, c * 128:(c + 1) * 128],
                                         identity=IDN[:, :]), r=[bMX, bIDN], w=[pb])
                        ov = MT[:, half * 4:half * 4 + 4, ts * 128:(ts + 1) * 128]
                        iv = pt[:, :].rearrange("p (c t) -> p c t", t=128)
                        if half == 0:
                            P.op("act", I("activation", out=ov, in_=iv, func=AF.Copy), r=[pb], w=[bMT])
                        else:
                            P.op("dve", I("tensor_copy", out=ov, in_=iv), r=[pb], w=[bMT])
                for m in range(8):
                    pt, pb = PSR.next()
                    for c in range(8):
                        P.op("pe", I("matmul", pt[:, :], lhsT=W_out[:, c, m * 128:(m + 1) * 128], rhs=MT[:, c, :],
                                     start=(c == 0), stop=(c == 7)), r=[bW_out, bMT], w=[pb])
                    P.op("dve", I("tensor_tensor", out=X[:, m, :], in0=X[:, m, :], in1=pt[:, :], op=ALU.add), r=[bX, pb], w=[bX])
                P.op("pool", I("dma_start", out=hv[:, :, tb * 512:(tb + 1) * 512], in_=X[:]), r=[bX], w=[bHTs[b]], dma=True, disjoint=True)
            P.barrier()

    stop = os.environ.get("MK_STOP", "")
    for b in range(NB):
        phase_P(b)
        if stop == "P":
            break
        phase_C()
        phase_MLA()
        if stop == "MLA":
            break
        phase_NC()
        if stop == "NC":
            break
        phase_NSW()
        phase_M(b)
        if stop == "M":
            break
    A.close()
    if stop:
        with ExitStack() as L:
            Z = sb(L, "Z", [128, 512]); bZ = Buf("Z")
            P.op("pool", I("memset", Z[:], 0.0), w=[bZ])
            P.op("sp", I("dma_start", out=outT[0, 0:128, 0:512], in_=Z[:]), r=[bZ], w=[bOUT], dma=True, disjoint=True)
            P.barrier()
        P.emit()
        es.close()
        return nc

    with ExitStack() as Fs:
        WG = sb(Fs, "WG", [128, 8, DFF], BF16); bWG = Buf("WG")
        WU = sb(Fs, "WU", [128, 8, DFF], BF16); bWU = Buf("WU")
        WD = sb(Fs, "WD", [128, 22, D], BF16); bWD = Buf("WD")
        with ExitStack() as SU:
            stg = [(sb(SU, "fstg%d" % i, [128, DFF]), Buf("fstg%d" % i)) for i in range(4)]
            SR = Ring(stg)
            ei = 0
            for (wd, wt, wb) in ((w_gate_d, WG, bWG), (w_up_d, WU, bWU)):
                wv = wd.rearrange("(c p) n -> p c n", p=128)
                for c in range(8):
                    load_cast(SU, wt[:, c, :], wv[:, c, :], wb, 128, DFF, SR, ei, indep=True); ei += 1
            wv = w_down_d.rearrange("(c p) n -> p c n", p=128)
            for c in range(22):
                load_cast(SU, WD[:, c, :], wv[:, c, :], bWD, 128, D, SR, ei, indep=True); ei += 1
            P.barrier()
        H = sb(Fs, "H", [128, 8, 512]); bH = Buf("H")
        OUT = sb(Fs, "OUT", [128, 8, 512]); bOUTt = Buf("OUTt")
        HN = sb(Fs, "HN", [128, 8, 512], BF16); bHN = Buf("HN")
        RSa = sb(Fs, "RSa", [128, 512]); bRSa = Buf("RSa")
        RSb = sb(Fs, "RSb", [128, 512]); bRSb = Buf("RSb")
        AT = sb(Fs, "AT", [128, 22, 512], BF16); bAT = [Buf("AT%d" % i) for i in range(22)]
        SGR = Ring([(sb(Fs, "SGf%d" % i, [128, 512]), Buf("SGf%d" % i)) for i in range(2)])
        SQR = Ring([(sb(Fs, "SQf%d" % i, [128, 512], BF16), Buf("SQf%d" % i)) for i in range(4)])
        blocks = [(b, tb) for b in range(NB) for tb in range(NTB)]

        def stats(src, bsrc, RS, bRS):
            pt, pb = PSR.next()
            for c in range(8):
                sq, bsq = SQR.next()
                P.op("act", I("activation", out=sq[:], in_=src[:, c, :], func=AF.Square), r=[bsrc], w=[bsq])
                P.op("pe", I("matmul", pt[:, :], lhsT=ONES[:, :], rhs=sq[:], start=(c == 0), stop=(c == 7)),
                     r=[bONES, bsq], w=[pb])
            P.op("act", I("activation", out=RS[:], in_=pt[:, :], func=AF.Sqrt, bias=EPSB[:, 0:1], scale=1.0 / 1024.0),
                 r=[pb, bEPSB], w=[bRS])
            P.op("dve", I("reciprocal", out=RS[:], in_=RS[:]), r=[bRS], w=[bRS])

        def chain1(k):
            b, tb = blocks[k]
            hv = HTs[b].rearrange("(c p) t -> p c t", p=128)
            P.op("sp", I("dma_start", out=H[:], in_=hv[:, :, tb * 512:(tb + 1) * 512]), r=[bHTs[b]], w=[bH], dma=True)
            stats(H, bH, RSa, bRSa)

        def chain2(k):
            for c in range(8):
                P.op("dve", I("scalar_tensor_tensor", out=HN[:, c, :], in0=H[:, c, :], scalar=GV[:, 8 + c:9 + c],
                              in1=RSa[:], op0=ALU.mult, op1=ALU.mult), r=[bH, bGV, bRSa], w=[bHN])

        def copies(k):
            for c in range(8):
                P.op("pool", I("tensor_copy", out=OUT[:, c, :], in_=H[:, c, :]), r=[bH], w=[bOUTt])

        def gateup(k, f):
            pg, pgb = PSR.next()
            for c in range(8):
                P.op("pe", I("matmul", pg[:, :], lhsT=WG[:, c, f * 128:(f + 1) * 128], rhs=HN[:, c, :],
                             start=(c == 0), stop=(c == 7)), r=[bWG, bHN], w=[pgb])
            pu, pub = PSR.next()
            for c in range(8):
                P.op("pe", I("matmul", pu[:, :], lhsT=WU[:, c, f * 128:(f + 1) * 128], rhs=HN[:, c, :],
                             start=(c == 0), stop=(c == 7)), r=[bWU, bHN], w=[pub])
            SG, bSG = SGR.next()
            P.op("act", I("activation", out=SG[:], in_=pg[:, :], func=AF.Silu), r=[pgb], w=[bSG])
            P.op("dve", I("tensor_tensor", out=AT[:, f, :], in0=SG[:], in1=pu[:, :], op=ALU.mult),
                 r=[bSG, pub], w=[bAT[f]])

        def down(k):
            for m in range(8):
                pt, pb = PSR.next()
                for f in range(22):
                    P.op("pe", I("matmul", pt[:, :], lhsT=WD[:, f, m * 128:(m + 1) * 128], rhs=AT[:, f, :],
                                 start=(f == 0), stop=(f == 21)), r=[bWD, bAT[f]], w=[pb])
                P.op("dve", I("tensor_tensor", out=OUT[:, m, :], in0=OUT[:, m, :], in1=pt[:, :], op=ALU.add),
                     r=[bOUTt, pb], w=[bOUTt])

        def final(k):
            b, tb = blocks[k]
            ov = outT[b].rearrange("(c p) t -> p c t", p=128)
            stats(OUT, bOUTt, RSb, bRSb)
            for c in range(8):
                P.op("dve", I("scalar_tensor_tensor", out=OUT[:, c, :], in0=OUT[:, c, :], scalar=GV[:, 16 + c:17 + c],
                              in1=RSb[:], op0=ALU.mult, op1=ALU.mult), r=[bOUTt, bGV, bRSb], w=[bOUTt])
            P.op("pool", I("dma_start", out=ov[:, :, tb * 512:(tb + 1) * 512], in_=OUT[:]), r=[bOUTt], w=[bOUT], dma=True, disjoint=True)

        nblk = len(blocks)
        chain1(0)
        for k in range(nblk):
            chain2(k)
            for f in range(22):
                gateup(k, f)
                if f == 1:
                    if k > 0:
                        final(k - 1)
                    copies(k)
                if f == 14 and k + 1 < nblk:
                    chain1(k + 1)
            down(k)
        final(nblk - 1)
        P.barrier()
    P.emit()
    es.close()
    return nc


def prep_inputs(inp):
    f = lambda a: np.ascontiguousarray(np.asarray(a, dtype=np.float32))
    c = host_consts()
    shared = {
        "w_in": f(inp["w_in"][0]), "w_uq": f(inp["mla_w_uq"][0]), "w_ukv": f(inp["mla_w_ukv"][0]),
        "w1k": f(inp["nsa_cmp_w1_k"][0]), "w1v": f(inp["nsa_cmp_w1_v"][0]),
        "w2k": f(inp["nsa_cmp_w2_k"][0]), "w2v": f(inp["nsa_cmp_w2_v"][0]),
        "poskT": f(np.asarray(inp["nsa_cmp_pos_k"][0]).T), "posvT": f(np.asarray(inp["nsa_cmp_pos_v"][0]).T),
        "t5": f(inp["t5_table"]), "w_out": f(inp["w_out"][0]),
        "w_gate": f(inp["w_gate"][0]), "w_up": f(inp["w_up"][0]), "w_down": f(inp["w_down"][0]),
    }
    gv = np.zeros((128, 32), np.float32)
    gv[:, 0:8] = np.asarray(inp["norm_mix_g"][0], np.float32).reshape(8, 128).T
    gv[:, 8:16] = np.asarray(inp["norm_ffn_g"][0], np.float32).reshape(8, 128).T
    gv[:, 16:24] = np.asarray(inp["final_norm_g"], np.float32).reshape(8, 128).T
    gv[:, 24:26] = np.asarray(inp["mla_q_norm_g"][0], np.float32).reshape(2, 128).T
    gv[:, 26] = np.asarray(inp["mla_kv_norm_g"][0], np.float32)
    shared["gvec"] = gv
    go = np.concatenate([np.asarray(inp["out_norm_mla_g"][0], np.float32), np.asarray(inp["out_norm_nsa_g"][0], np.float32)])
    shared["gout"] = np.ascontiguousarray(np.broadcast_to(go[None, :], (128, 1024)))
    for k in ("tri", "farm", "antiI", "ident", "ind", "ohd", "ohc", "selc", "ovl", "rope"):
        shared[k] = c[k]
    x = np.asarray(inp["x"], np.float32)
    maps = []
    for i in range(NCORES):
        m = dict(shared)
        m["xT"] = np.ascontiguousarray(x[i * NB:(i + 1) * NB].transpose(0, 2, 1))
        maps.append(m)
    return maps


def kernel(**inputs):
    nc = build()
    maps = prep_inputs(inputs)
    res = run_bass_kernel_spmd(nc, maps, core_ids=list(range(NCORES)))
    out = np.empty((NCORES * NB, S, D), np.float32)
    for i in range(NCORES):
        o = np.asarray(res.results[i]["outT"], np.float32)
        out[i * NB:(i + 1) * NB] = o.transpose(0, 2, 1)
    return out
```

```python
import math
import os
from contextlib import ExitStack

import ml_dtypes
import numpy as np

import concourse.bass as bass
import concourse.mybir as mybir
from concourse.bass_utils import run_bass_kernel_spmd

F32 = mybir.dt.float32
BF16 = mybir.dt.bfloat16
AF = mybir.ActivationFunctionType
ALU = mybir.AluOpType
NPBF = ml_dtypes.bfloat16

S = 4096
D = 1024
NB = 2
NCORES = 8
DFF = 2816
NTB = S // 512
NT = S // 128
EPS = 1e-6
BIG = 30000.0
SC_M = 96 ** -0.5
HCL = 8176
ENGS = ("pe", "act", "dve", "pool", "sp")
RDMA = 8
STRICT_SAME = True
WARM_N = int(os.environ.get('MK_WARM', '128'))
LAG = int(os.environ.get('MK_LAG', '2'))


class Buf:
    __slots__ = ("name", "w", "r", "ep")

    def __init__(self, name):
        self.name = name
        self.w = None
        self.r = []
        self.ep = -1


class Op:
    __slots__ = ("eng", "fn", "deps", "dma", "n", "sig", "need", "dk", "dval", "bar", "tag")


def I(meth, *a, **k):
    return lambda e: getattr(e, meth)(*a, **k)


class Prog:
    def __init__(self, nc):
        self.nc = nc
        self.ops = {e: [] for e in ENGS}
        self.dmas = {e: [] for e in ENGS}
        self.epoch = 0
        self.tag = ""

    def op(self, eng, fn, r=(), w=(), dma=False, bar=False, extra=(), disjoint=False):
        o = Op()
        o.eng, o.fn, o.dma, o.bar = eng, fn, dma, bar
        o.tag = self.tag
        o.need = False
        o.sig = None
        o.n = len(self.ops[eng])
        deps = {}
        for b in list(r) + list(w):
            if b.ep != self.epoch:
                b.w, b.r, b.ep = None, [], self.epoch
        for b in r:
            if b.w is not None:
                deps[b.w] = "raw"
        for b in w:
            if b.w is not None and not disjoint:
                deps.setdefault(b.w, "waw")
            for x in b.r:
                deps.setdefault(x, "war")
        for x in extra:
            deps[x] = "raw"
        fin = []
        for d, kind in deps.items():
            if d is o:
                continue
            if d.eng == eng and not d.dma and not dma and not bar:
                if eng == "pe":
                    continue
                if not STRICT_SAME and (kind != "raw" or o.n - d.n > 3):
                    continue
            fin.append(d)
        if dma:
            k = len(self.dmas[eng])
            o.dk = (eng, k % RDMA)
            o.dval = 16 * (k // RDMA + 1)
            if k >= RDMA:
                fin.append(self.dmas[eng][k - RDMA])
            self.dmas[eng].append(o)
        for d in fin:
            d.need = True
        o.deps = fin
        self.ops[eng].append(o)
        ws = set(id(b) for b in w)
        for b in w:
            b.w = o
            b.r = []
        for b in r:
            if id(b) not in ws:
                b.r.append(o)
        return o

    def barrier(self):
        last = []
        for e in ENGS:
            if self.ops[e]:
                last.append(self.ops[e][-1])
            last.extend(self.dmas[e][-RDMA:])
        bsp = self.op("sp", None, bar=True, extra=last)
        for e in ENGS:
            if e != "sp":
                self.op(e, None, bar=True, extra=[bsp])
        self.epoch += 1

    def check(self):
        done = set()
        pc = {e: 0 for e in ENGS}
        prog = True
        while prog:
            prog = False
            for e in ENGS:
                while pc[e] < len(self.ops[e]):
                    o = self.ops[e][pc[e]]
                    if all(id(d) in done for d in o.deps):
                        done.add(id(o))
                        pc[e] += 1
                        prog = True
                    else:
                        break
        bad = {e: pc[e] for e in ENGS if pc[e] < len(self.ops[e])}
        if bad:
            msg = []
            for e, i in bad.items():
                o = self.ops[e][i]
                msg.append("%s blocked at op %d/%d (%s) waiting on %s" % (
                    e, i, len(self.ops[e]), getattr(o, "tag", ""),
                    [(d.eng, d.n, getattr(d, "tag", "")) for d in o.deps if id(d) not in done]))
            raise RuntimeError("DEADLOCK: " + " | ".join(msg))

    def emit(self):
        self.check()
        nc = self.nc
        for e in ENGS:
            c = 0
            for o in self.ops[e]:
                if o.need and not o.dma:
                    c += 1
                    o.sig = c
        with ExitStack() as st:
            sem = {e: st.enter_context(nc.semaphore("s_" + e)) for e in ENGS}
            dsem = {}
            for e in ENGS:
                if self.dmas[e]:
                    for i in range(RDMA):
                        dsem[(e, i)] = st.enter_context(nc.semaphore("d_%s%d" % (e, i)))
            block = st.enter_context(nc.Block())

            def run(ename, eng):
                waited = {}
                for o in self.ops[ename]:
                    for d in o.deps:
                        if d.dma:
                            key, s_, v = d.dk, dsem[d.dk], d.dval
                        else:
                            key, s_, v = d.eng, sem[d.eng], d.sig
                        if waited.get(key, 0) >= v:
                            continue
                        eng.wait_ge(s_, v)
                        waited[key] = v
                    if o.bar:
                        if o.sig is not None:
                            eng.sem_inc(sem[ename], 1)
                        continue
                    ins = o.fn(eng)
                    if o.dma:
                        ins.then_inc(dsem[o.dk], 16)
                    elif o.sig is not None:
                        ins.then_inc(sem[ename], 1)
                if ename == "sp":
                    for q in ENGS:
                        for d in self.dmas[q][-RDMA:]:
                            if waited.get(d.dk, 0) < d.dval:
                                eng.wait_ge(dsem[d.dk], d.dval)
                                waited[d.dk] = d.dval

            @block.sync
            def _(e):
                run("sp", e)

            @block.tensor
            def _(e):
                run("pe", e)

            @block.scalar
            def _(e):
                run("act", e)

            @block.vector
            def _(e):
                run("dve", e)

            @block.gpsimd
            def _(e):
                run("pool", e)


class Ring:
    def __init__(self, items):
        self.items = items
        self.i = 0

    def next(self):
        x = self.items[self.i % len(self.items)]
        self.i += 1
        return x


def _bucket(d):
    n = np.maximum(d, 0)
    nf = np.maximum(n, 1).astype(np.float32)
    large = 16 + (np.log(nf / np.float32(16)) / np.float32(math.log(8.0)) * np.float32(16)).astype(np.int32)
    large = np.minimum(large, 31)
    return np.where(n < 16, n, large)


_CONST = None


def host_consts():
    global _CONST
    if _CONST is not None:
        return _CONST
    c = {}
    k = np.arange(128)
    c["tri"] = (k[:, None] <= k[None, :]).astype(NPBF)
    c["farm"] = (k[:, None] > k[None, :]).astype(NPBF)
    c["antiI"] = (k[:, None] == 127 - k[None, :]).astype(NPBF)
    c["ident"] = np.eye(128, dtype=np.float32)
    t = np.arange(S)
    c["ind"] = (t[None, :] // 64 == np.arange(64)[:, None]).astype(NPBF)
    d = np.arange(384) - 127
    oh = np.zeros((33, 384), np.float32)
    b = _bucket(d)
    for i in range(384):
        oh[32 if d[i] < 0 else b[i], i] = 1.0
    c["ohd"] = oh
    m = np.arange(HCL) - 4111
    oh = np.zeros((33, HCL), np.float32)
    b = _bucket(m)
    oh[np.where(m < 0, 32, b), np.arange(HCL)] = 1.0
    c["ohc"] = oh
    sel = np.zeros((NT, 128, 128), np.float32)
    j = np.arange(64)
    for qt in range(NT):
        q = qt * 128 + k
        cur = q // 64
        forced = (j[None, :] == 0) | (j[None, :] == cur[:, None]) | (j[None, :] == cur[:, None] - 1)
        causal = j[None, :] <= cur[:, None]
        sel[qt, :, :64] = (causal & ~forced)
        sel[qt, :, 64:] = np.where(forced, 1e30, np.where(causal, 0.0, -1e30))
    c["selc"] = sel
    ov = np.zeros((256, 64), np.float32)
    for npr in range(1, 256):
        n = 255 - npr
        lo = np.maximum(16 * n, 64 * j)
        hi = np.minimum(16 * n + 32, 64 * j + 64)
        ov[npr] = np.maximum(0, hi - lo) / 16.0
    c["ovl"] = ov.astype(NPBF)
    inv = (10000.0 ** (-np.arange(0, 32, 2, dtype=np.float32) / 32)).astype(np.float32)
    ang = t.astype(np.float32)[None, :] * inv[:, None]
    cos = np.cos(ang).astype(np.float32)
    sin = np.sin(ang).astype(np.float32)
    cosT = np.concatenate([cos, cos], 0)
    sinT = np.concatenate([-sin, sin], 0)
    rope = np.zeros((96, 4, S), np.float32)
    rope[64:96, 0] = cosT
    rope[64:96, 1] = sinT
    rope[64:96, 2] = cosT * np.float32(SC_M)
    rope[64:96, 3] = sinT * np.float32(SC_M)
    c["rope"] = rope
    _CONST = c
    return c


def build(debug=None):
    nc = bass.Bass("TRN2", target_bir_lowering=False)
    P = Prog(nc)
    es = ExitStack()

    def din(name, shape, dt=F32):
        return nc.dram_tensor(name, list(shape), dt, kind="ExternalInput")

    def dscr(name, shape, dt=F32):
        kind = "ExternalOutput" if (debug and name in debug) else "Internal"
        return nc.dram_tensor(name, list(shape), dt, kind=kind)

    xT = din("xT", [NB, D, S]).ap()
    w_in_d = din("w_in", [D, 1720]).ap()
    w_uq_d = din("w_uq", [256, 768]).ap()
    w_ukv_d = din("w_ukv", [128, 1024]).ap()
    w1k_d = din("w1k", [2048, 128]).ap()
    w1v_d = din("w1v", [2048, 128]).ap()
    w2k_d = din("w2k", [128, 64]).ap()
    w2v_d = din("w2v", [128, 64]).ap()
    posk_d = din("poskT", [64, 32]).ap()
    posv_d = din("posvT", [64, 32]).ap()
    t5_d = din("t5", [32, 8]).ap()
    w_out_d = din("w_out", [D, D]).ap()
    w_gate_d = din("w_gate", [D, DFF]).ap()
    w_up_d = din("w_up", [D, DFF]).ap()
    w_down_d = din("w_down", [DFF, D]).ap()
    gvec_d = din("gvec", [128, 32]).ap()
    gout_d = din("gout", [128, 1024]).ap()
    tri_d = din("tri", [128, 128], BF16).ap()
    farm_d = din("farm", [128, 128], BF16).ap()
    anti_d = din("antiI", [128, 128], BF16).ap()
    ident_d = din("ident", [128, 128]).ap()
    ind_d = din("ind", [64, S], BF16).ap()
    ohd_d = din("ohd", [33, 384]).ap()
    ohc_d = din("ohc", [33, HCL]).ap()
    selc_d = din("selc", [NT, 128, 128]).ap()
    ovl_d = din("ovl", [256, 64], BF16).ap()
    rope_d = din("rope", [96, 4, S]).ap()

    outT_h = nc.dram_tensor("outT", [NB, D, S], F32, kind="ExternalOutput")
    outT = outT_h.ap()

    QMs = dscr("QMs", [8, 96, S], BF16).ap()
    KMs = dscr("KMs", [8, 96, S], BF16).ap()
    VMs = dscr("VMs", [8, 128, NT, 65], BF16).ap()
    QNs = dscr("QNs", [8, 64, S], BF16).ap()
    KSs = dscr("KSs", [2, 64, S], BF16).ap()
    KWs = dscr("KWs", [2, 64, S], BF16).ap()
    VSs = dscr("VSs", [2, 128, NT, 65], BF16).ap()
    VWs = dscr("VWs", [2, 128, NT, 65], BF16).ap()
    NGs = dscr("NGs", [2, 64, S], BF16).ap()
    OMs = dscr("OMs", [S, 512]).ap()
    OCs = dscr("OCs", [S, 512]).ap()
    OSs = dscr("OSs", [S, 512]).ap()
    OWs = dscr("OWs", [S, 512]).ap()
    HTs = dscr("HTs", [NB, D, S]).ap()
    GD_h = dscr("GDs", [8, 384])
    HC_h = dscr("HCs", [8, HCL], BF16)
    GDs, HCs = GD_h.ap(), HC_h.ap()
    bQMs, bKMs, bVMs, bQNs = Buf("QMs"), Buf("KMs"), Buf("VMs"), Buf("QNs")
    bKSs, bKWs, bVSs, bVWs, bNGs = Buf("KSs"), Buf("KWs"), Buf("VSs"), Buf("VWs"), Buf("NGs")
    bOMs, bOCs, bOSs, bOWs = Buf("OMs"), Buf("OCs"), Buf("OSs"), Buf("OWs")
    bHTs = [Buf("HT0"), Buf("HT1")]
    bGDs, bHCs = Buf("GDs"), Buf("HCs")
    bOUT = Buf("out")

    uid = [0]

    def sb(stack, name, shape, dt=F32):
        uid[0] += 1
        return stack.enter_context(nc.sbuf_tensor("%s_%d" % (name, uid[0]), list(shape), dt))

    ps = [es.enter_context(nc.psum_tensor("ps%d" % i, [128, 512], F32)) for i in range(8)]
    psb = [Buf("ps%d" % i) for i in range(8)]
    PSR = Ring(list(zip(ps, psb)))

    GV = sb(es, "GV", [128, 32]); bGV = Buf("GV")
    ONES = sb(es, "ONES", [128, 128], BF16); bONES = Buf("ONES")
    IDN = sb(es, "IDN", [128, 128]); bIDN = Buf("IDN")
    P.op("sp", I("dma_start", out=GV[:], in_=gvec_d), w=[bGV], dma=True)
    P.op("sp", I("dma_start", out=IDN[:], in_=ident_d), w=[bIDN], dma=True)
    P.op("pool", I("memset", ONES[:], 1.0), w=[bONES])
    IDNB = sb(es, "IDNB", [128, 128], BF16); bIDNB = Buf("IDNB")
    P.op("dve", I("tensor_copy", out=IDNB[:], in_=IDN[:]), r=[bIDN], w=[bIDNB])
    EPSB = sb(es, "EPSB", [128, 1]); bEPSB = Buf("EPSB")
    P.op("pool", I("memset", EPSB[:], EPS), w=[bEPSB])

    def load_cast(stack_stage, dst_ap, src_ap, dstbuf, rows, cols, ring, eng_i, indep=False):
        if indep:
            dstbuf = Buf("chunk")
        stg, sbuf_ = ring.next()
        P.op(("sp", "act")[eng_i % 2] if indep else "sp", I("dma_start", out=stg[0:rows, 0:cols], in_=src_ap),
             w=[sbuf_], dma=True)
        eng = ("dve", "pool", "dve", "act", "pool")[eng_i % 5] if indep else ("dve", "act", "pool", "dve", "act")[eng_i % 5]
        if eng == "act":
            P.op(eng, I("activation", out=dst_ap, in_=stg[0:rows, 0:cols], func=AF.Copy), r=[sbuf_], w=[dstbuf])
        else:
            P.op(eng, I("tensor_copy", out=dst_ap, in_=stg[0:rows, 0:cols]), r=[sbuf_], w=[dstbuf])

    A = ExitStack()
    W_in = sb(A, "W_in", [128, 8, 1720], BF16); bW_in = Buf("W_in")
    W_uq = sb(A, "W_uq", [128, 2, 768], BF16); bW_uq = Buf("W_uq")
    W_uqB = sb(A, "W_uqB", [128, 2, 8, 96], BF16); bW_uqB = Buf("W_uqB")
    WkrA = sb(A, "WkrA", [128, 8, 96], BF16); bWkrA = Buf("WkrA")
    WkrB = sb(A, "WkrB", [128, 8, 96], BF16); bWkrB = Buf("WkrB")
    W_ukv = sb(A, "W_ukv", [128, 1024], BF16); bW_ukv = Buf("W_ukv")
    W2 = [sb(A, "W2k", [128, 64], BF16), sb(A, "W2v", [128, 64], BF16)]
    bW2 = [Buf("W2k"), Buf("W2v")]
    POS = [sb(A, "POSk", [64, 32], BF16), sb(A, "POSv", [64, 32], BF16)]
    bPOS = [Buf("POSk"), Buf("POSv")]
    TRI = sb(A, "TRI", [128, 128], BF16); bTRI = Buf("TRI")
    FARM = sb(A, "FARM", [128, 128], BF16); bFARM = Buf("FARM")
    EM = sb(A, "EM", [128, 8, 256], BF16); bEM = Buf("EM")
    T31B = sb(A, "T31B", [128, 8]); bT31B = Buf("T31B")
    OVL = sb(A, "OVL", [128, 2, 64], BF16); bOVL = Buf("OVL")
    GATES = sb(A, "GATES", [128, NT, 24]); bGATES = Buf("GATES")
    KCR = sb(A, "KCR", [128, S], BF16); bKCR = Buf("KCR")
    VCR = sb(A, "VCR", [128, S], BF16); bVCR = Buf("VCR")
    KCT = sb(A, "KCT", [64, 2, 256], BF16); bKCT = Buf("KCT")
    VCA = sb(A, "VCA", [128, 2, 2, 65], BF16); bVCA = Buf("VCA")

    with ExitStack() as SU:
        stg = [(sb(SU, "stg%d" % i, [128, 4096]), Buf("stg%d" % i)) for i in range(2)]
        SR = Ring(stg)
        ei = 0
        wv = w_in_d.rearrange("(c p) n -> p c n", p=128)
        for c in range(8):
            load_cast(SU, W_in[:, c, :], wv[:, c, :], bW_in, 128, 1720, SR, ei); ei += 1
        wv = w_uq_d.rearrange("(c p) n -> p c n", p=128)
        for c in range(2):
            load_cast(SU, W_uq[:, c, :], wv[:, c, :], bW_uq, 128, 768, SR, ei); ei += 1
        load_cast(SU, W_ukv[:, :], w_ukv_d, bW_ukv, 128, 1024, SR, ei); ei += 1
        for kv, (wd, pd) in enumerate(((w2k_d, posk_d), (w2v_d, posv_d))):
            load_cast(SU, W2[kv][:, :], wd, bW2[kv], 128, 64, SR, ei); ei += 1
            load_cast(SU, POS[kv][:, :], pd, bPOS[kv], 64, 32, SR, ei); ei += 1
        P.op("pool", I("memset", W_uqB[:], 0.0), w=[bW_uqB])
        uq4 = W_uq[:, :, :].rearrange("p c (h e) -> p c h e", e=96)
        P.op("pool", I("tensor_copy", out=W_uqB[:, :, :, 64:80], in_=uq4[:, :, :, 80:96]), r=[bW_uq], w=[bW_uqB])
        P.op("pool", I("tensor_copy", out=W_uqB[:, :, :, 80:96], in_=uq4[:, :, :, 64:80]), r=[bW_uq], w=[bW_uqB])
        P.op("pool", I("memset", WkrA[:], 0.0), w=[bWkrA])
        P.op("pool", I("memset", WkrB[:], 0.0), w=[bWkrB])
        P.op("pool", I("tensor_copy", out=WkrA[:, :, 64:96], in_=W_in[:, :, 384:416]), r=[bW_in], w=[bWkrA])
        P.op("pool", I("tensor_copy", out=WkrB[:, :, 64:80], in_=W_in[:, :, 400:416]), r=[bW_in], w=[bWkrB])
        P.op("pool", I("tensor_copy", out=WkrB[:, :, 80:96], in_=W_in[:, :, 384:400]), r=[bW_in], w=[bWkrB])
        P.op("sp", I("dma_start", out=TRI[:], in_=tri_d), w=[bTRI], dma=True)
        P.op("sp", I("dma_start", out=FARM[:], in_=farm_d), w=[bFARM], dma=True)
        P.op("sp", I("dma_start", out=OVL[:], in_=ovl_d.rearrange("(t p) j -> p t j", p=128)), w=[bOVL], dma=True)
        P.op("sp", I("dma_start", out=T31B[:], in_=t5_d[31:32, :].to_broadcast([128, 8])), w=[bT31B], dma=True)
        TBLX = sb(SU, "TBLX", [33, 8]); bTBLX = Buf("TBLX")
        NT31 = sb(SU, "NT31", [8, 1]); bNT31 = Buf("NT31")
        OHD = sb(SU, "OHD", [33, 384]); bOHD = Buf("OHD")
        ANTI = sb(SU, "ANTI", [128, 128], BF16); bANTI = Buf("ANTI")
        P.op("pool", I("memset", TBLX[32:33, :], -BIG), w=[bTBLX])
        P.op("sp", I("dma_start", out=TBLX[0:32, :], in_=t5_d), w=[bTBLX], dma=True)
        P.op("sp", I("dma_start", out=NT31[:], in_=t5_d[31:32, :].rearrange("a h -> h a")), w=[bNT31], dma=True)
        P.op("dve", I("tensor_scalar", out=NT31[:], in0=NT31[:], scalar1=-1.0, scalar2=None, op0=ALU.mult),
             r=[bNT31], w=[bNT31])
        P.op("sp", I("dma_start", out=OHD[:], in_=ohd_d), w=[bOHD], dma=True)
        P.op("sp", I("dma_start", out=ANTI[:], in_=anti_d), w=[bANTI], dma=True)
        pt, pb = PSR.next()
        P.op("pe", I("matmul", pt[0:8, 0:384], lhsT=TBLX[:, :], rhs=OHD[:, :], start=True, stop=True),
             r=[bTBLX, bOHD], w=[pb])
        GT = sb(SU, "GT", [8, 384]); bGT = Buf("GT")
        P.op("act", I("activation", out=GT[:], in_=pt[0:8, 0:384], func=AF.Exp, bias=NT31[:, 0:1], scale=1.0),
             r=[pb, bNT31], w=[bGT])
        P.op("pool", I("dma_start", out=GDs, in_=GT[:]), r=[bGT], w=[bGDs], dma=True)
        EMF = sb(SU, "EMF", [128, 256]); bEMF = Buf("EMF")
        EMFb = sb(SU, "EMFb", [128, 256], BF16); bEMFb = Buf("EMFb")
        for h in range(8):
            hank = bass.AP(tensor=GD_h, offset=h * 384, ap=[[1, 128], [1, 256]])
            P.op("sp", I("dma_start", out=EMF[:], in_=hank), r=[bGDs], w=[bEMF], dma=True)
            P.op("dve", I("tensor_copy", out=EMFb[:], in_=EMF[:]), r=[bEMF], w=[bEMFb])
            pt, pb = PSR.next()
            P.op("pe", I("matmul", pt[:, 0:256], lhsT=ANTI[:, :], rhs=EMFb[:, :], start=True, stop=True),
                 r=[bANTI, bEMFb], w=[pb])
            P.op("act", I("activation", out=EM[:, h, :], in_=pt[:, 0:256], func=AF.Copy), r=[pb], w=[bEM])
        OHC = [(sb(SU, "OHC%d" % i, [33, 512]), Buf("OHC%d" % i)) for i in range(2)]
        HCT = [(sb(SU, "HCT%d" % i, [8, 512], BF16), Buf("HCT%d" % i)) for i in range(2)]
        OR_, HR_ = Ring(OHC), Ring(HCT)
        for ch in range(16):
            n = min(512, HCL - ch * 512)
            ot, ob = OR_.next()
            ht, hb = HR_.next()
            P.op("sp", I("dma_start", out=ot[:, 0:n], in_=ohc_d[:, ch * 512:ch * 512 + n]), w=[ob], dma=True)
            pt, pb = PSR.next()
            P.op("pe", I("matmul", pt[0:8, 0:n], lhsT=TBLX[:, :], rhs=ot[:, 0:n], start=True, stop=True),
                 r=[bTBLX, ob], w=[pb])
            P.op("dve", I("tensor_copy", out=ht[:, 0:n], in_=pt[0:8, 0:n]), r=[pb], w=[hb])
            P.op("pool", I("dma_start", out=HCs[:, ch * 512:ch * 512 + n], in_=ht[:, 0:n]), r=[hb], w=[bHCs], dma=True)
        P.barrier()

    def stats_rstd(stack_tiles, src_sq_aps, nfeat, rbuf_list, RSTD, bRSTD):
        pt, pb = PSR.next()
        n = len(src_sq_aps)
        for i, (ap_, b_) in enumerate(src_sq_aps):
            P.op("pe", I("matmul", pt[:, :], lhsT=ONES[:, :], rhs=ap_, start=(i == 0), stop=(i == n - 1)),
                 r=[bONES, b_], w=[pb])
        P.op("act", I("activation", out=RSTD[:], in_=pt[:, :], func=AF.Sqrt, bias=EPSB[:, 0:1], scale=1.0 / nfeat),
             r=[pb, bEPSB], w=[bRSTD])
        P.op("dve", I("reciprocal", out=RSTD[:], in_=RSTD[:]), r=[bRSTD], w=[bRSTD])

    def phase_P(b):
        with ExitStack() as L:
            XR = [(sb(L, "X%d" % i, [128, 8, 512]), Buf("X%d" % i)) for i in range(2)]
            XNR = [(sb(L, "XN%d" % i, [128, 8, 512], BF16), Buf("XN%d" % i)) for i in range(2)]
            RSR = [(sb(L, "RSTD%d" % i, [128, 512]), Buf("RSTD%d" % i)) for i in range(2)]
            RPR = [(sb(L, "ROPE%d" % i, [96, 2, 512]), Buf("ROPE%d" % i)) for i in range(2)]
            SQ = sb(L, "SQ", [128, 8, 512], BF16); bSQ = Buf("SQ")
            RQ = sb(L, "RQ", [128, 512]); bRQ = Buf("RQ")
            RKV = sb(L, "RKV", [128, 512]); bRKV = Buf("RKV")
            CQf = sb(L, "CQf", [128, 2, 512]); bCQf = Buf("CQf")
            CQs = sb(L, "CQs", [128, 2, 512], BF16); bCQs = Buf("CQs")
            CQN = sb(L, "CQN", [128, 2, 512], BF16); bCQN = Buf("CQN")
            CKf = sb(L, "CKf", [128, 512]); bCKf = Buf("CKf")
            CKs = sb(L, "CKs", [128, 512], BF16); bCKs = Buf("CKs")
            CKN = sb(L, "CKN", [128, 512], BF16); bCKN = Buf("CKN")
            T1 = sb(L, "T1", [96, 512]); bT1 = Buf("T1")
            T2 = sb(L, "T2", [96, 512]); bT2 = Buf("T2")
            KPE = sb(L, "KPE", [96, 512], BF16); bKPE = Buf("KPE")
            QM = sb(L, "QM", [96, 8, 512], BF16); bQM = Buf("QM")
            KM = sb(L, "KM", [96, 8, 512], BF16); bKM = Buf("KM")
            VMR = Ring([(sb(L, "VM%d" % i, [128, 8, 4, 65], BF16), Buf("VM%d" % i)) for i in range(2)])
            QN = sb(L, "QN", [128, 4, 512], BF16); bQN = Buf("QN")
            KSR = Ring([(sb(L, "KS%d" % i, [128, 2, 512], BF16), Buf("KS%d" % i)) for i in range(2)])
            VSR = Ring([(sb(L, "VS%d" % i, [128, 4, 4, 65], BF16), Buf("VS%d" % i)) for i in range(2)])
            for (t_, b_) in VMR.items + VSR.items:
                P.op("pool", I("memset", t_[:], 1.0), w=[b_])
            xv = xT[b].rearrange("(c p) t -> p c t", p=128)

            def chain1(tb):
                t0 = tb * 512
                X, bX = XR[tb % 2]
                RSTD, bRSTD = RSR[tb % 2]
                ROPE, bROPE = RPR[tb % 2]
                P.op("sp", I("dma_start", out=X[:], in_=xv[:, :, t0:t0 + 512]), w=[bX], dma=True)
                P.op("sp", I("dma_start", out=ROPE[64:96, :, :], in_=rope_d[64:96, 0:2, t0:t0 + 512]), w=[bROPE], dma=True)
                P.op("act", I("activation", out=SQ[:], in_=X[:], func=AF.Square), r=[bX], w=[bSQ])
                stats_rstd(None, [(SQ[:, c, :], bSQ) for c in range(8)], 1024.0, None, RSTD, bRSTD)

            def chain2(tb):
                X, bX = XR[tb % 2]
                XN, bXN = XNR[tb % 2]
                RSTD, bRSTD = RSR[tb % 2]
                for c in range(8):
                    P.op("dve", I("scalar_tensor_tensor", out=XN[:, c, :], in0=X[:, c, :], scalar=GV[:, c:c + 1],
                                  in1=RSTD[:], op0=ALU.mult, op1=ALU.mult), r=[bX, bGV, bRSTD], w=[bXN])

            def body(tb, part):
                t0 = tb * 512
                XN, bXN = XNR[tb % 2]
                ROPE, bROPE = RPR[tb % 2]

                def proj(cols, M, wt=W_in, wb=bW_in):
                    pt, pb = PSR.next()
                    for c in range(8):
                        P.op("pe", I("matmul", pt[0:M, :], lhsT=wt[:, c, cols[0]:cols[1]], rhs=XN[:, c, :],
                                     start=(c == 0), stop=(c == 7)), r=[wb, bXN], w=[pb])
                    return pt, pb

                if part == 0:
                    for j in range(2):
                        pt, pb = proj((j * 128, (j + 1) * 128), 128)
                        P.op("act", I("activation", out=CQf[:, j, :], in_=pt[:, :], func=AF.Copy), r=[pb], w=[bCQf])
                        P.op("act", I("activation", out=CQs[:, j, :], in_=pt[:, :], func=AF.Square), r=[pb], w=[bCQs])
                    pt, pb = proj((256, 384), 128)
                    P.op("act", I("activation", out=CKf[:], in_=pt[:, :], func=AF.Copy), r=[pb], w=[bCKf])
                    P.op("act", I("activation", out=CKs[:], in_=pt[:, :], func=AF.Square), r=[pb], w=[bCKs])
                    pt, pb = proj((928, 1056), 128)
                    P.op("act", I("activation", out=KCR[:, t0:t0 + 512], in_=pt[:, :], func=AF.Copy), r=[pb], w=[bKCR])
                    pt, pb = proj((1056, 1184), 128)
                    P.op("dve", I("tensor_copy", out=VCR[:, t0:t0 + 512], in_=pt[:, :]), r=[pb], w=[bVCR])
                    stats_rstd(None, [(CQs[:, j, :], bCQs) for j in range(2)], 256.0, None, RQ, bRQ)
                    for j in range(2):
                        P.op("dve", I("scalar_tensor_tensor", out=CQN[:, j, :], in0=CQf[:, j, :], scalar=GV[:, 24 + j:25 + j],
                                      in1=RQ[:], op0=ALU.mult, op1=ALU.mult), r=[bCQf, bGV, bRQ], w=[bCQN])
                    stats_rstd(None, [(CKs[:], bCKs)], 128.0, None, RKV, bRKV)
                    P.op("dve", I("scalar_tensor_tensor", out=CKN[:], in0=CKf[:], scalar=GV[:, 26:27],
                                  in1=RKV[:], op0=ALU.mult, op1=ALU.mult), r=[bCKf, bGV, bRKV], w=[bCKN])
                    for j in range(4):
                        pt, pb = proj((416 + j * 128, 416 + (j + 1) * 128), 128)
                        if j % 2 == 0:
                            P.op("act", I("activation", out=QN[:, j, :], in_=pt[:, :], func=AF.Copy, scale=0.125),
                                 r=[pb], w=[bQN])
                        else:
                            P.op("dve", I("tensor_scalar", out=QN[:, j, :], in0=pt[:, :], scalar1=0.125, scalar2=None,
                                          op0=ALU.mult), r=[pb], w=[bQN])
                    P.op("pool", I("dma_start", out=QNs.rearrange("(j two) d t -> (two d) j t", two=2)[:, :, t0:t0 + 512],
                                   in_=QN[:]), r=[bQN], w=[bQNs], dma=True, disjoint=True)
                    KS, bKS = KSR.next()
                    for i, c0 in enumerate((1184, 1440)):
                        pt, pb = proj((c0, c0 + 128), 128)
                        if i % 2 == 0:
                            P.op("act", I("activation", out=KS[:, i, :], in_=pt[:, :], func=AF.Copy), r=[pb], w=[bKS])
                        else:
                            P.op("dve", I("tensor_copy", out=KS[:, i, :], in_=pt[:, :]), r=[pb], w=[bKS])
                    P.op("pool", I("dma_start", out=KSs.rearrange("k d t -> (k d) t")[:, t0:t0 + 512], in_=KS[:, 0, :]),
                         r=[bKS], w=[bKSs], dma=True, disjoint=True)
                    P.op("pool", I("dma_start", out=KWs.rearrange("k d t -> (k d) t")[:, t0:t0 + 512], in_=KS[:, 1, :]),
                         r=[bKS], w=[bKWs], dma=True, disjoint=True)
                    return
                pa, pab = proj((0, 96), 96, wt=WkrA, wb=bWkrA)
                pbb_, pbbb = proj((0, 96), 96, wt=WkrB, wb=bWkrB)
                P.op("dve", I("tensor_tensor", out=T1[64:96, :], in0=pa[64:96, :], in1=ROPE[64:96, 0, :], op=ALU.mult),
                     r=[pab, bROPE], w=[bT1])
                P.op("dve", I("tensor_tensor", out=T2[64:96, :], in0=pbb_[64:96, :], in1=ROPE[64:96, 1, :], op=ALU.mult),
                     r=[pbbb, bROPE], w=[bT2])
                P.op("dve", I("tensor_tensor", out=KPE[64:96, :], in0=T1[64:96, :], in1=T2[64:96, :], op=ALU.add),
                     r=[bT1, bT2], w=[bKPE])
                for h in range(8):
                    pa, pab = PSR.next()
                    for j in range(2):
                        P.op("pe", I("matmul", pa[0:96, :], lhsT=W_uq[:, j, h * 96:(h + 1) * 96], rhs=CQN[:, j, :],
                                     start=(j == 0), stop=(j == 1)), r=[bW_uq, bCQN], w=[pab])
                    pq, pqb = PSR.next()
                    for j in range(2):
                        P.op("pe", I("matmul", pq[0:96, :], lhsT=W_uqB[:, j, h, :], rhs=CQN[:, j, :],
                                     start=(j == 0), stop=(j == 1)), r=[bW_uqB, bCQN], w=[pqb])
                    P.op("act", I("activation", out=QM[0:64, h, :], in_=pa[0:64, :], func=AF.Copy, scale=SC_M),
                         r=[pab], w=[bQM])
                    P.op("dve", I("scalar_tensor_tensor", out=T1[64:96, :], in0=pa[64:96, :], scalar=SC_M,
                                  in1=ROPE[64:96, 0, :], op0=ALU.mult, op1=ALU.mult), r=[pab, bROPE], w=[bT1])
                    P.op("dve", I("scalar_tensor_tensor", out=T2[64:96, :], in0=pq[64:96, :], scalar=SC_M,
                                  in1=ROPE[64:96, 1, :], op0=ALU.mult, op1=ALU.mult), r=[pqb, bROPE], w=[bT2])
                    P.op("dve", I("tensor_tensor", out=QM[64:96, h, :], in0=T1[64:96, :], in1=T2[64:96, :], op=ALU.add),
                         r=[bT1, bT2], w=[bQM])
                    pk, pkb = PSR.next()
                    P.op("pe", I("matmul", pk[:, :], lhsT=W_ukv[:, h * 128:(h + 1) * 128], rhs=CKN[:, :],
                                 start=True, stop=True), r=[bW_ukv, bCKN], w=[pkb])
                    P.op("act", I("activation", out=KM[0:64, h, :], in_=pk[0:64, :], func=AF.Copy), r=[pkb], w=[bKM])
                    P.op("pool", I("tensor_copy", out=KM[64:96, h, :], in_=KPE[64:96, :]), r=[bKPE], w=[bKM])
                P.op("pool", I("dma_start", out=QMs.rearrange("h d t -> d h t")[:, :, t0:t0 + 512], in_=QM[:]),
                     r=[bQM], w=[bQMs], dma=True, disjoint=True)
                P.op("pool", I("dma_start", out=KMs.rearrange("h d t -> d h t")[:, :, t0:t0 + 512], in_=KM[:]),
                     r=[bKM], w=[bKMs], dma=True, disjoint=True)
                VM, bVM = VMR.next()
                wv4 = W_ukv[:, :].rearrange("p (h e) -> p h e", e=128)
                for ts in range(4):
                    pt, pb = PSR.next()
                    P.op("pe", I("matmul", pt[:, :], lhsT=CKN[:, ts * 128:(ts + 1) * 128], rhs=wv4[:, :, 64:128],
                                 start=True, stop=True), r=[bCKN, bW_ukv], w=[pb])
                    P.op("act", I("activation", out=VM[:, :, ts, 0:64], in_=pt[:, :].rearrange("p (h e) -> p h e", e=64),
                                  func=AF.Copy), r=[pb], w=[bVM])
                for h in range(8):
                    P.op("pool", I("dma_start", out=VMs[h, :, tb * 4:tb * 4 + 4, :], in_=VM[:, h, :, :]),
                         r=[bVM], w=[bVMs], dma=True, disjoint=True)
                VS, bVS = VSR.next()
                for ts in range(4):
                    pt, pb = PSR.next()
                    for c in range(8):
                        P.op("pe", I("matmul", pt[:, 0:128], lhsT=XN[:, c, ts * 128:(ts + 1) * 128], rhs=W_in[:, c, 1312:1440],
                                     start=(c == 0), stop=(c == 7)), r=[bXN, bW_in], w=[pb])
                    P.op("act", I("activation", out=VS[:, 0:2, ts, 0:64],
                                  in_=pt[:, 0:128].rearrange("p (k e) -> p k e", e=64), func=AF.Copy), r=[pb], w=[bVS])
                    pt, pb = PSR.next()
                    for c in range(8):
                        P.op("pe", I("matmul", pt[:, 0:152], lhsT=XN[:, c, ts * 128:(ts + 1) * 128], rhs=W_in[:, c, 1568:1720],
                                     start=(c == 0), stop=(c == 7)), r=[bXN, bW_in], w=[pb])
                    P.op("dve", I("tensor_copy", out=VS[:, 2:4, ts, 0:64],
                                  in_=pt[:, 0:128].rearrange("p (k e) -> p k e", e=64)), r=[pb], w=[bVS])
                    P.op("act", I("activation", out=GATES[:, tb * 4 + ts, :], in_=pt[:, 128:152], func=AF.Sigmoid),
                         r=[pb], w=[bGATES])
                for k_ in range(2):
                    P.op("pool", I("dma_start", out=VSs[k_, :, tb * 4:tb * 4 + 4, :], in_=VS[:, k_, :, :]),
                         r=[bVS], w=[bVSs], dma=True, disjoint=True)
                    P.op("pool", I("dma_start", out=VWs[k_, :, tb * 4:tb * 4 + 4, :], in_=VS[:, 2 + k_, :, :]),
                         r=[bVS], w=[bVWs], dma=True, disjoint=True)

            chain1(0)
            chain2(0)
            for tb in range(NTB):
                body(tb, 0)
                if tb + 1 < NTB:
                    chain1(tb + 1)
                body(tb, 1)
                if tb + 1 < NTB:
                    chain2(tb + 1)
            P.barrier()

    def phase_C():
        with ExitStack() as L:
            BIA = sb(L, "BIA", [128, 1]); bBIA = Buf("BIA")
            Hf = sb(L, "Hf", [128, 256]); bHf = Buf("Hf")
            H2 = sb(L, "H2", [128, 256]); bH2 = Buf("H2")
            SG = sb(L, "SG", [128, 256]); bSG = Buf("SG")
            GH = sb(L, "GH", [128, 256], BF16); bGH = Buf("GH")
            W1 = [sb(L, "W1k", [128, 32, 128], BF16), sb(L, "W1v", [128, 32, 128], BF16)]
            bW1 = [Buf("W1k"), Buf("W1v")]
            stg_ = sb(L, "cstg", [128, 4096]); sbuf_ = Buf("cstg")
            sv = stg_[:, :].rearrange("p (l h) -> p l h", h=128)
            for kv, wd in enumerate((w1k_d, w1v_d)):
                src = wd.rearrange("(l d) h -> d l h", d=64)
                for half in range(2):
                    P.op("sp", I("dma_start", out=sv[half * 64:(half + 1) * 64, :, :], in_=src), w=[sbuf_], dma=True)
                P.op("dve", I("tensor_copy", out=W1[kv][:, :, :], in_=sv[:, :, :]), r=[sbuf_], w=[bW1[kv]])
            P.op("pool", I("memset", VCA[:], 0.0), w=[bVCA])
            P.op("pool", I("memset", KCT[:], 0.0), w=[bKCT])
            for kv in range(2):
                RAW, bRAW = (KCR, bKCR) if kv == 0 else (VCR, bVCR)
                pbia, pbiab = PSR.next()
                for l in range(32):
                    P.op("pe", I("matmul", pbia[:, 0:1], lhsT=W1[kv][0:64, l, :], rhs=POS[kv][0:64, l:l + 1],
                                 start=(l == 0), stop=(l == 31)), r=[bW1[kv], bPOS[kv]], w=[pbiab])
                P.op("dve", I("tensor_copy", out=BIA[:], in_=pbia[:, 0:1]), r=[pbiab], w=[bBIA])
                for kh in range(2):
                    p0 = kh * 64
                    pt, pb = PSR.next()
                    for l in range(32):
                        P.op("pe", I("matmul", pt[:, 0:255], lhsT=W1[kv][p0:p0 + 64, l, :],
                                     rhs=RAW[p0:p0 + 64, l:l + 16 * 254 + 1:16], start=(l == 0), stop=(l == 31)),
                             r=[bW1[kv], bRAW], w=[pb])
                    P.op("act", I("activation", out=Hf[:, 0:255], in_=pt[:, 0:255], func=AF.Identity, bias=BIA[:, 0:1],
                                  scale=1.0), r=[pb, bBIA], w=[bHf])
                    P.op("dve", I("tensor_tensor", out=H2[:, 0:255], in0=Hf[:, 0:255], in1=Hf[:, 0:255], op=ALU.mult),
                         r=[bHf], w=[bH2])
                    P.op("dve", I("tensor_scalar", out=H2[:, 0:255], in0=H2[:, 0:255], scalar1=0.044715, scalar2=1.0,
                                  op0=ALU.mult, op1=ALU.add), r=[bH2], w=[bH2])
                    P.op("dve", I("tensor_tensor", out=H2[:, 0:255], in0=H2[:, 0:255], in1=Hf[:, 0:255], op=ALU.mult),
                         r=[bH2, bHf], w=[bH2])
                    P.op("act", I("activation", out=SG[:, 0:255], in_=H2[:, 0:255], func=AF.Sigmoid,
                                  scale=2.0 * math.sqrt(2.0 / math.pi)), r=[bH2], w=[bSG])
                    P.op("pool", I("memset", GH[:], 0.0), w=[bGH])
                    rev = bass.AP(tensor=GH, offset=GH[:, 255:256].offset, ap=[list(GH[:].ap[0]), [-1, 255]])
                    P.op("dve", I("tensor_tensor", out=rev, in0=SG[:, 0:255], in1=Hf[:, 0:255], op=ALU.mult),
                         r=[bSG, bHf], w=[bGH])
                    if kv == 0:
                        pt, pb = PSR.next()
                        P.op("pe", I("matmul", pt[0:64, 0:256], lhsT=W2[0][:, :], rhs=GH[:, :], start=True, stop=True),
                             r=[bW2[0], bGH], w=[pb])
                        P.op("act", I("activation", out=KCT[:, kh, :], in_=pt[0:64, 0:256], func=AF.Copy),
                             r=[pb], w=[bKCT])
                    else:
                        for nt in range(2):
                            pt, pb = PSR.next()
                            P.op("pe", I("matmul", pt[:, 0:64], lhsT=GH[:, nt * 128:(nt + 1) * 128], rhs=W2[1][:, :],
                                         start=True, stop=True), r=[bW2[1], bGH], w=[pb])
                            P.op("act", I("activation", out=VCA[:, kh, nt, 0:64], in_=pt[:, 0:64], func=AF.Copy),
                                 r=[pb], w=[bVCA])
            P.op("pool", I("memset", VCA[:, :, 1, 64:65], 1.0), w=[bVCA])
            P.op("pool", I("memset", VCA[:, :, 0, 64:65], 1.0), w=[bVCA])
            P.op("pool", I("memset", VCA[0:1, :, 0, :], 0.0), w=[bVCA])
            P.barrier()

    def run_attention(L, jobs):
        SR_ = Ring([(ps[i], psb[i]) for i in range(0, 4)])
        OR_ = Ring([(ps[i], psb[i]) for i in range(4, 6)])
        PTR = Ring([(sb(L, "PT%d" % i, [128, 512], BF16), Buf("PT%d" % i)) for i in range(5)])
        flat = []
        for ji, job in enumerate(jobs):
            nt_ = len(job["tiles"])
            job["ji"] = ji
            for ti in range(nt_):
                flat.append((job, ti, ti == 0, ti == nt_ - 1))
        state = {}
        loaded = set()
        LOOK = 24

        def stage1(i):
            job, ti, first, last = flat[i]
            for k2 in range(i, min(len(flat), i + LOOK)):
                j2 = flat[k2][0]
                if j2["ji"] > job["ji"] + 3:
                    break
                if id(j2) not in loaded:
                    loaded.add(id(j2))
                    if j2["load"] is not None:
                        j2["load"]()
            tl = job["tiles"][ti]
            kT, kb, vA, vb, lo, hi, masks = tl
            qap, qb_ = job["q"]
            st_, sbf = SR_.next()
            ptile, pbf = PTR.next()
            c0, c1 = lo * 128, hi * 128
            P.op("pe", I("matmul", st_[:, c0:c1], lhsT=kT, rhs=qap[:, c0:c1], start=True, stop=True),
                 r=[kb, qb_], w=[sbf])
            P.op("act", I("activation", out=ptile[:, c0:c1], in_=st_[:, c0:c1], func=AF.Exp, bias=job["bias"], scale=1.0),
                 r=[sbf] + job["rbias"], w=[pbf])
            for (qs, map_, mb) in masks:
                P.op("dve", I("tensor_tensor", out=ptile[:, qs * 128:(qs + 1) * 128], in0=ptile[:, qs * 128:(qs + 1) * 128],
                              in1=map_, op=ALU.mult), r=[pbf, mb], w=[pbf])
            state[i] = (ptile, pbf)

        OSR = Ring([(sb(L, "OSb%d" % i, [128, 512]), Buf("OSb%d" % i)) for i in range(2)])
        for (t_, b_) in OSR.items:
            P.op("pool", I("memset", t_[:], 0.0), w=[b_])
        TR_ = Ring([(ps[i], psb[i]) for i in range(6, 7)])
        DUMW = sb(L, "DUMW", [128, 512], BF16); bDUMW = Buf("DUMW")
        P.op("pool", I("memset", DUMW[:], 0.5), w=[bDUMW])
        bDUM = Buf("DUM")
        pending = []

        def stage2(i):
            job, ti, first, last = flat[i]
            kT, kb, vA, vb, lo, hi, masks = job["tiles"][ti]
            ptile, pbf = state.pop(i)
            if first:
                job["O"] = OR_.next()
                job["started"] = False
            O, Ob = job["O"]
            c0, c1 = lo * 128, hi * 128
            if WARM_N:
                P.op("pe", I("matmul", ps[7][:, 0:WARM_N], lhsT=ONES[:, :], rhs=DUMW[:, 0:WARM_N], start=True, stop=True),
                     r=[bONES, bDUMW], w=[bDUM])
            P.op("pe", I("matmul", O[0:65, c0:c1], lhsT=vA, rhs=ptile[:, c0:c1],
                         start=(not job["started"]), stop=last, skip_group_check=True), r=[pbf, vb], w=[Ob])
            job["started"] = True
            if last:
                OS_, bOS_ = OSR.next()
                P.op("dve", I("tensor_copy", out=OS_[0:65, :], in_=O[0:65, :]), r=[Ob], w=[bOS_])

                def fin(job=job, OS_=OS_, bOS_=bOS_):
                    Tt, Tb = TR_.next()
                    Tv = Tt[:, :].rearrange("p (s e) -> p s e", e=128)
                    for qs in range(4):
                        P.op("pe", I("transpose", out=Tv[:, qs, :], in_=OS_[:, qs * 128:(qs + 1) * 128],
                                     identity=IDN[:, :]), r=[bOS_, bIDN], w=[Tb])
                    job["evac"](Tv, Tb)
                pending.append((i + LAG + 2, fin))

        n = len(flat)
        for i in range(n + LAG):
            if i < n:
                stage1(i)
            if i >= LAG:
                stage2(i - LAG)
            while pending and pending[0][0] <= i:
                pending.pop(0)[1]()
        while pending:
            pending.pop(0)[1]()

    def std_evac(L, name):
        RR = Ring([(sb(L, name + "R%d" % i, [128, 4]), Buf(name + "R%d" % i)) for i in range(2)])
        OTR = Ring([(sb(L, name + "OT%d" % i, [128, 4, 64]), Buf(name + "OT%d" % i)) for i in range(2)])

        def mk(dst, dbuf, h, qb, gate_col):
            def evac(Ov, Ob):
                R, bR = RR.next()
                OT, bOT = OTR.next()
                P.op("dve", I("reciprocal", out=R[:, :], in_=Ov[:, :, 64]), r=[Ob], w=[bR])
                for qs in range(4):
                    if gate_col is None:
                        P.op("dve", I("tensor_scalar", out=OT[:, qs, :], in0=Ov[:, qs, 0:64], scalar1=R[:, qs:qs + 1],
                                      scalar2=None, op0=ALU.mult), r=[Ob, bR], w=[bOT])
                    else:
                        g = GATES[:, qb * 4 + qs, gate_col:gate_col + 1]
                        P.op("dve", I("tensor_scalar", out=OT[:, qs, :], in0=Ov[:, qs, 0:64], scalar1=R[:, qs:qs + 1],
                                      scalar2=g, op0=ALU.mult, op1=ALU.mult), r=[Ob, bR, bGATES], w=[bOT])
                dv = dst.rearrange("(t p) c -> p t c", p=128)[:, qb * 4:qb * 4 + 4, h * 64:(h + 1) * 64]
                P.op("pool", I("dma_start", out=dv, in_=OT[:]), r=[bOT], w=[dbuf], dma=True, disjoint=True)
            return evac
        return mk

    def phase_MLA():
        with ExitStack() as L:
            KR = Ring([(sb(L, "K%d" % i, [96, S], BF16), Buf("K%d" % i)) for i in range(2)])
            VR = Ring([(sb(L, "V%d" % i, [128, NT, 65], BF16), Buf("V%d" % i)) for i in range(2)])
            QR = Ring([(sb(L, "Q%d" % i, [96, 512], BF16), Buf("Q%d" % i)) for i in range(6)])
            mk = std_evac(L, "m")
            jobs = []
            for h in range(8):
                kv = {}
                for qb in range(NTB):
                    job = {}

                    def load(h=h, qb=qb, job=job, kv=kv):
                        if qb == 0:
                            kv["K"] = KR.next()
                            kv["V"] = VR.next()
                            P.op("sp", I("dma_start", out=kv["K"][0][:], in_=KMs[h]), r=[bKMs], w=[kv["K"][1]], dma=True)
                            P.op("sp", I("dma_start", out=kv["V"][0][:], in_=VMs[h]), r=[bVMs], w=[kv["V"][1]], dma=True)
                        Q, bQ = QR.next()
                        P.op("sp", I("dma_start", out=Q[:], in_=QMs[h, :, qb * 512:(qb + 1) * 512]), r=[bQMs], w=[bQ], dma=True)
                        job["q"] = (Q, bQ)
                        K, bK = kv["K"]
                        V, bV = kv["V"]
                        tiles = []
                        for kt in range(4 * qb + 4):
                            lo = max(0, kt - 4 * qb)
                            masks = [(lo, TRI[:, :], bTRI)] if kt >= 4 * qb else []
                            tiles.append((K[:, kt * 128:(kt + 1) * 128], bK, V[:, kt, :], bV, lo, 4, masks))
                        job["tiles"][:] = tiles
                    job["load"] = load
                    job["tiles"] = [None] * (4 * qb + 4)
                    job["bias"] = 0.0
                    job["rbias"] = []
                    job["evac"] = mk(OMs, bOMs, h, qb, None)
                    jobs.append(job)
            run_attention(L, jobs)
            P.barrier()

    def phase_NC():
        with ExitStack() as L:
            QR = Ring([(sb(L, "cQ%d" % i, [64, 512], BF16), Buf("cQ%d" % i)) for i in range(4)])
            BCR = Ring([(sb(L, "BC%d" % i, [128, 512], BF16), Buf("BC%d" % i)) for i in range(4)])
            SSR = Ring([(sb(L, "SS%d" % i, [128, 512]), Buf("SS%d" % i)) for i in range(3)])
            ER = Ring([(sb(L, "E%d" % i, [128, 512], BF16), Buf("E%d" % i)) for i in range(5)])
            RR = Ring([(sb(L, "cR%d" % i, [128, 4]), Buf("cR%d" % i)) for i in range(2)])
            OTR = Ring([(sb(L, "cOT%d" % i, [128, 4, 64]), Buf("cOT%d" % i)) for i in range(2)])
            IMP = sb(L, "IMP", [128, 4, 64]); bIMP = Buf("IMP")
            SELC = Ring([(sb(L, "SELC%d" % i, [128, 4, 128]), Buf("SELC%d" % i)) for i in range(2)])
            M8 = sb(L, "M8", [128, 4, 16]); bM8 = Buf("M8")
            TMPR = Ring([(sb(L, "TMP%d" % i, [128, 4, 64]), Buf("TMP%d" % i)) for i in range(2)])
            NGT = Ring([(sb(L, "NGT%d" % i, [64, 512], BF16), Buf("NGT%d" % i)) for i in range(2)])
            S_R = Ring([(ps[i], psb[i]) for i in range(0, 3)])
            O_R = Ring([(ps[i], psb[i]) for i in range(3, 5)])
            I_R = Ring([(ps[i], psb[i]) for i in range(5, 7)])
            T_R = Ring([(ps[7], psb[7])])
            units = []
            for kvh in range(2):
                for qb in range(NTB):
                    for g in range(4):
                        units.append((kvh, qb, g))
            ctx = {}

            def stageA(u):
                kvh, qb, g = units[u]
                h = kvh * 4 + g
                nts = [1] if qb < 4 else [0, 1]
                if g == 0:
                    SC_, bSC = SELC.next()
                    P.op("sp", I("dma_start", out=SC_[:], in_=selc_d[qb * 4:qb * 4 + 4].rearrange("t p c -> p t c")),
                         w=[bSC], dma=True)
                    ctx[(kvh, qb)] = (SC_, bSC)
                Q, bQ = QR.next()
                P.op("sp", I("dma_start", out=Q[:], in_=QNs[h, :, qb * 512:(qb + 1) * 512]), r=[bQNs], w=[bQ], dma=True)
                es_ = []
                for ni, nt in enumerate(nts):
                    BC, bBC = BCR.next()
                    src = bass.AP(tensor=HC_h, offset=h * HCL + 2048 * nt + 512 * qb, ap=[[16, 128], [1, 512]])
                    P.op("sp", I("dma_start", out=BC[:], in_=src), r=[bHCs], w=[bBC], dma=True)
                    St, Sb = S_R.next()
                    P.op("pe", I("matmul", St[:, :], lhsT=KCT[:, kvh, nt * 128:(nt + 1) * 128], rhs=Q[:, :],
                                 start=True, stop=False), r=[bKCT, bQ], w=[Sb])
                    P.op("pe", I("matmul", St[:, :], lhsT=IDNB[:, :], rhs=BC[:, :], start=False, stop=True),
                         r=[bIDNB, bBC], w=[Sb])
                    E, bE = ER.next()
                    P.op("act", I("activation", out=E[:], in_=St[:, :], func=AF.Exp), r=[Sb], w=[bE])
                    es_.append((nt, E, bE))
                ctx[u] = es_

            def stageB(u):
                kvh, qb, g = units[u]
                h = kvh * 4 + g
                es_ = ctx.pop(u)
                O, Ob = O_R.next()
                Im, Imb = I_R.next()
                Ov = O[:, 0:260].rearrange("p (s e) -> p s e", e=65)
                Iv = Im[:, 0:256].rearrange("p (s e) -> p s e", e=64)
                first = True
                for ni, (nt, E, bE) in enumerate(es_):
                    for qs in range(4):
                        P.op("pe", I("matmul", Ov[:, qs, :], lhsT=E[:, qs * 128:(qs + 1) * 128], rhs=VCA[:, kvh, nt, :],
                                     start=first, stop=(ni == len(es_) - 1), skip_group_check=True),
                             r=[bE, bVCA], w=[Ob])
                        P.op("pe", I("matmul", Iv[:, qs, :], lhsT=E[:, qs * 128:(qs + 1) * 128], rhs=OVL[:, nt, :],
                                     start=first, stop=(ni == len(es_) - 1), skip_group_check=True),
                             r=[bE, bOVL], w=[Imb])
                        first = False
                R, bR = RR.next()
                OT, bOT = OTR.next()
                P.op("dve", I("tensor_scalar", out=R[:, :], in0=Ov[:, :, 64], scalar1=1e-30, scalar2=None,
                              op0=ALU.add), r=[Ob], w=[bR])
                P.op("dve", I("reciprocal", out=R[:, :], in_=R[:, :]), r=[bR], w=[bR])
                for qs in range(4):
                    gcol = GATES[:, qb * 4 + qs, 3 * h:3 * h + 1]
                    P.op("dve", I("tensor_scalar", out=OT[:, qs, :], in0=Ov[:, qs, 0:64], scalar1=R[:, qs:qs + 1],
                                  scalar2=gcol, op0=ALU.mult, op1=ALU.mult), r=[Ob, bR, bGATES], w=[bOT])
                    if g == 0:
                        P.op("dve", I("tensor_scalar", out=IMP[:, qs, :], in0=Iv[:, qs, :], scalar1=R[:, qs:qs + 1],
                                      scalar2=None, op0=ALU.mult), r=[Imb, bR], w=[bIMP])
                    else:
                        P.op("dve", I("scalar_tensor_tensor", out=IMP[:, qs, :], in0=Iv[:, qs, :],
                                      scalar=R[:, qs:qs + 1], in1=IMP[:, qs, :], op0=ALU.mult, op1=ALU.add),
                             r=[Imb, bR, bIMP], w=[bIMP])
                dv = OCs.rearrange("(t p) c -> p t c", p=128)[:, qb * 4:qb * 4 + 4, h * 64:(h + 1) * 64]
                P.op("pool", I("dma_start", out=dv, in_=OT[:]), r=[bOT], w=[bOCs], dma=True, disjoint=True)
                if g == 3:
                    SC_, bSC = ctx.pop((kvh, qb))
                    P.op("dve", I("tensor_tensor", out=IMP[:], in0=IMP[:], in1=SC_[:, :, 0:64], op=ALU.mult),
                         r=[bIMP, bSC], w=[bIMP])
                    P.op("dve", I("tensor_tensor", out=IMP[:], in0=IMP[:], in1=SC_[:, :, 64:128], op=ALU.add),
                         r=[bIMP, bSC], w=[bIMP])
                    TMP, bTMP = TMPR.next()
                    for qs in range(4):
                        P.op("dve", I("max", out=M8[:, qs, 0:8], in_=IMP[:, qs, :]), r=[bIMP], w=[bM8])
                        P.op("dve", I("match_replace", out=TMP[:, qs, :], in_to_replace=M8[:, qs, 0:8], in_values=IMP[:, qs, :],
                                      imm_value=-3.0e38), r=[bM8, bIMP], w=[bTMP])
                        P.op("dve", I("max", out=M8[:, qs, 8:16], in_=TMP[:, qs, :]), r=[bTMP], w=[bM8])
                        P.op("dve", I("tensor_scalar", out=TMP[:, qs, :], in0=IMP[:, qs, :], scalar1=M8[:, qs, 15:16],
                                      scalar2=1.0, op0=ALU.is_ge, op1=ALU.subtract), r=[bIMP, bM8], w=[bTMP])

                    def topk_tail(kvh=kvh, qb=qb, TMP=TMP, bTMP=bTMP):
                        NG, bNG = NGT.next()
                        for qs in range(4):
                            Tt, Tb = T_R.next()
                            P.op("pe", I("transpose", out=Tt[0:64, 0:128], in_=TMP[:, qs, :], identity=IDN[:, :]),
                                 r=[bTMP, bIDN], w=[Tb])
                            P.op("act", I("activation", out=NG[:, qs * 128:(qs + 1) * 128], in_=Tt[0:64, 0:128], func=AF.Copy,
                                          scale=BIG), r=[Tb], w=[bNG])
                        P.op("pool", I("dma_start", out=NGs[kvh, :, qb * 512:(qb + 1) * 512], in_=NG[:]), r=[bNG], w=[bNGs], dma=True, disjoint=True)
                    return topk_tail
                return None

            nu = len(units)
            tail = None
            for u in range(nu + 1):
                if u < nu:
                    stageA(u)
                if tail is not None:
                    tail()
                    tail = None
                if u >= 1:
                    tail = stageB(u - 1)
            if tail is not None:
                tail()
            P.barrier()

    def phase_NSW():
        with ExitStack() as L:
            KSA = Ring([(sb(L, "KSA%d" % i, [128, S], BF16), Buf("KSA%d" % i)) for i in range(2)])
            KWR = Ring([(sb(L, "KW%d" % i, [128, S], BF16), Buf("KW%d" % i)) for i in range(2)])
            for (t_, b_) in KWR.items:
                P.op("pool", I("memset", t_[64:128, :], 0.0), w=[b_])
            VSR = Ring([(sb(L, "VSa%d" % i, [128, NT, 65], BF16), Buf("VSa%d" % i)) for i in range(2)])
            VWR = Ring([(sb(L, "VWa%d" % i, [128, NT, 65], BF16), Buf("VWa%d" % i)) for i in range(2)])
            QR = Ring([(sb(L, "QA%d" % i, [128, 512], BF16), Buf("QA%d" % i)) for i in range(6)])
            mks = std_evac(L, "s")
            mkw = std_evac(L, "w")
            jobs = []
            for kvh in range(2):
                kv = {}
                for g in range(4):
                    h = kvh * 4 + g
                    for qb in range(NTB):
                        js, jw = {}, {}

                        def load(h=h, g=g, kvh=kvh, qb=qb, js=js, jw=jw, kv=kv):
                            if g == 0 and qb == 0:
                                kv["KS"], kv["KW"], kv["VS"], kv["VW"] = KSA.next(), KWR.next(), VSR.next(), VWR.next()
                                P.op("sp", I("dma_start", out=kv["KS"][0][0:64, :], in_=KSs[kvh]), r=[bKSs], w=[kv["KS"][1]], dma=True)
                                P.op("sp", I("dma_start", out=kv["KS"][0][64:128, :], in_=ind_d), w=[kv["KS"][1]], dma=True)
                                P.op("sp", I("dma_start", out=kv["KW"][0][0:64, :], in_=KWs[kvh]), r=[bKWs], w=[kv["KW"][1]], dma=True)
                                P.op("sp", I("dma_start", out=kv["VS"][0][:], in_=VSs[kvh]), r=[bVSs], w=[kv["VS"][1]], dma=True)
                                P.op("sp", I("dma_start", out=kv["VW"][0][:], in_=VWs[kvh]), r=[bVWs], w=[kv["VW"][1]], dma=True)
                            Q, bQ = QR.next()
                            P.op("sp", I("dma_start", out=Q[0:64, :], in_=QNs[h, :, qb * 512:(qb + 1) * 512]), r=[bQNs], w=[bQ], dma=True)
                            P.op("sp", I("dma_start", out=Q[64:128, :], in_=NGs[kvh, :, qb * 512:(qb + 1) * 512]), r=[bNGs], w=[bQ], dma=True)
                            js["q"] = (Q, bQ)
                            jw["q"] = (Q[:, :], bQ)
                            K, bK = kv["KS"]
                            V, bV = kv["VS"]
                            tiles = []
                            for kt in range(4 * qb + 4):
                                lo = max(0, kt - 4 * qb)
                                masks = []
                                for qs in range(lo, 4):
                                    qt = 4 * qb + qs
                                    if kt == qt:
                                        masks.append((qs, EM[:, h, 0:128], bEM))
                                    elif kt == qt - 1:
                                        masks.append((qs, EM[:, h, 128:256], bEM))
                                tiles.append((K[:, kt * 128:(kt + 1) * 128], bK, V[:, kt, :], bV, lo, 4, masks))
                            js["tiles"][:] = tiles
                            K, bK = kv["KW"]
                            V, bV = kv["VW"]
                            tiles = []
                            for kt in range(max(0, 4 * qb - 4), 4 * qb + 4):
                                lo = max(0, kt - 4 * qb)
                                hi = min(4, kt - 4 * qb + 5)
                                masks = []
                                for qs in range(lo, hi):
                                    qt = 4 * qb + qs
                                    if kt == qt:
                                        masks.append((qs, EM[:, h, 0:128], bEM))
                                    elif kt == qt - 1:
                                        masks.append((qs, EM[:, h, 128:256], bEM))
                                    elif kt == qt - 4:
                                        masks.append((qs, FARM[:, :], bFARM))
                                tiles.append((K[:, kt * 128:(kt + 1) * 128], bK, V[:, kt, :], bV, lo, hi, masks))
                            jw["tiles"][:] = tiles
                        js["load"] = load
                        jw["load"] = None
                        js["tiles"] = [None] * (4 * qb + 4)
                        jw["tiles"] = [None] * (4 * qb + 4 - max(0, 4 * qb - 4))
                        for j_, mk_, dst, dbuf, col in ((js, mks, OSs, bOSs, 3 * h + 1), (jw, mkw, OWs, bOWs, 3 * h + 2)):
                            j_["bias"] = T31B[:, h:h + 1]
                            j_["rbias"] = [bT31B]
                            j_["evac"] = mk_(dst, dbuf, h, qb, col)
                            jobs.append(j_)
            run_attention(L, jobs)
            P.barrier()

    def phase_M(b):
        with ExitStack() as L:
            OMR = Ring([(sb(L, "OM%d" % i, [128, 512]), Buf("OM%d" % i)) for i in range(4)])
            ONR = Ring([(sb(L, "ON%d" % i, [128, 3, 512]), [Buf("ON%d_%d" % (i, j)) for j in range(3)]) for i in range(4)])
            MXR = Ring([(sb(L, "MX%d" % i, [128, 1024]), Buf("MX%d" % i)) for i in range(3)])
            JNKR = Ring([(sb(L, "JNK%d" % i, [128, 512]), Buf("JNK%d" % i)) for i in range(2)])
            SSQR = Ring([(sb(L, "SSQ%d" % i, [128, 2]), Buf("SSQ%d" % i)) for i in range(4)])
            MXT = Ring([(sb(L, "MXT%d" % i, [128, 8, 512], BF16), Buf("MXT%d" % i)) for i in range(2)])
            XR = Ring([(sb(L, "Xm%d" % i, [128, 8, 512]), Buf("Xm%d" % i)) for i in range(2)])
            W_out = sb(L, "W_out", [128, 8, 1024], BF16); bW_out = Buf("W_out")
            GOUT = sb(L, "GOUT", [128, 1024]); bGOUT = Buf("GOUT")
            MSR = Ring([(sb(L, "mstg%d" % i, [128, 1024]), Buf("mstg%d" % i)) for i in range(2)])
            wv = w_out_d.rearrange("(c p) n -> p c n", p=128)
            for c in range(8):
                load_cast(L, W_out[:, c, :], wv[:, c, :], bW_out, 128, 1024, MSR, c)
            P.op("sp", I("dma_start", out=GOUT[:], in_=gout_d), w=[bGOUT], dma=True)
            xv = xT[b].rearrange("(c p) t -> p c t", p=128)
            hv = HTs[b].rearrange("(c p) t -> p c t", p=128)
            for tb in range(NTB):
                MT, bMT = MXT.next()
                X, bX = XR.next()
                P.op("sp", I("dma_start", out=X[:], in_=xv[:, :, tb * 512:(tb + 1) * 512]), w=[bX], dma=True)
                for ts in range(4):
                    t = tb * 4 + ts
                    OM, bOM = OMR.next()
                    ON, bONs = ONR.next()
                    MX, bMX = MXR.next()
                    SSQ, bSSQ = SSQR.next()
                    P.op("sp", I("dma_start", out=OM[:], in_=OMs[t * 128:(t + 1) * 128, :]), r=[bOMs], w=[bOM], dma=True)
                    for i, (src, sbuf_) in enumerate(((OCs, bOCs), (OSs, bOSs), (OWs, bOWs))):
                        P.op(("act", "sp", "act")[i], I("dma_start", out=ON[:, i, :], in_=src[t * 128:(t + 1) * 128, :]),
                             r=[sbuf_], w=[bONs[i]], dma=True)
                    bON = bONs[0]
                    P.op("pool", I("tensor_tensor", out=ON[:, 0, :], in0=ON[:, 0, :], in1=ON[:, 1, :], op=ALU.add), r=[bONs[0], bONs[1]], w=[bONs[0]])
                    P.op("pool", I("tensor_tensor", out=ON[:, 0, :], in0=ON[:, 0, :], in1=ON[:, 2, :], op=ALU.add), r=[bONs[0], bONs[2]], w=[bONs[0]])
                    JNK, bJNK = JNKR.next()
                    P.op("act", I("activation", out=JNK[:], in_=OM[:], func=AF.Square, accum_out=SSQ[:, 0:1]), r=[bOM], w=[bJNK, bSSQ])
                    JNK, bJNK = JNKR.next()
                    P.op("act", I("activation", out=JNK[:], in_=ON[:, 0, :], func=AF.Square, accum_out=SSQ[:, 1:2]), r=[bON], w=[bJNK, bSSQ])
                    P.op("act", I("activation", out=SSQ[:], in_=SSQ[:], func=AF.Sqrt, bias=EPSB[:, 0:1], scale=1.0 / 512.0),
                         r=[bSSQ, bEPSB], w=[bSSQ])
                    P.op("dve", I("reciprocal", out=SSQ[:], in_=SSQ[:]), r=[bSSQ], w=[bSSQ])
                    P.op("dve", I("scalar_tensor_tensor", out=MX[:, 0:512], in0=OM[:], scalar=SSQ[:, 0:1], in1=GOUT[:, 0:512],
                                  op0=ALU.mult, op1=ALU.mult), r=[bOM, bSSQ, bGOUT], w=[bMX])
                    P.op("dve", I("scalar_tensor_tensor", out=MX[:, 512:1024], in0=ON[:, 0, :], scalar=SSQ[:, 1:2],
                                  in1=GOUT[:, 512:1024], op0=ALU.mult, op1=ALU.mult), r=[bON, bSSQ, bGOUT], w=[bMX])
                    for half in range(2):
                        pt, pb = PSR.next()
                        for c4 in range(4):
                            c = half * 4 + c4
                            P.op("pe", I("transpose", out=pt[:, c4 * 128:(c4 + 1) * 128], in_=MX[:, c * 128:(c + 1) * 128],
                                         identity=IDN[:, :]), r=[bMX, bIDN], w=[pb])
                        ov = MT[:, half * 4:half * 4 + 4, ts * 128:(ts + 1) * 128]
                        iv = pt[:, :].rearrange("p (c t) -> p c t", t=128)
                        if half == 0:
                            P.op("act", I("activation", out=ov, in_=iv, func=AF.Copy), r=[pb], w=[bMT])
                        else:
                            P.op("dve", I("tensor_copy", out=ov, in_=iv), r=[pb], w=[bMT])
                for m in range(8):
                    pt, pb = PSR.next()
                    for c in range(8):
                        P.op("pe", I("matmul", pt[:, :], lhsT=W_out[:, c, m * 128:(m + 1) * 128], rhs=MT[:, c, :],
                                     start=(c == 0), stop=(c == 7)), r=[bW_out, bMT], w=[pb])
                    P.op("dve", I("tensor_tensor", out=X[:, m, :], in0=X[:, m, :], in1=pt[:, :], op=ALU.add), r=[bX, pb], w=[bX])
                P.op("pool", I("dma_start", out=hv[:, :, tb * 512:(tb + 1) * 512], in_=X[:]), r=[bX], w=[bHTs[b]], dma=True, disjoint=True)
            P.barrier()

    stop = os.environ.get("MK_STOP", "")
    for b in range(NB):
        phase_P(b)
        if stop == "P":
            break
        phase_C()
        phase_MLA()
        if stop == "MLA":
            break
        phase_NC()
        if stop == "NC":
            break
        phase_NSW()
        phase_M(b)
        if stop == "M":
            break
    A.close()
    if stop:
        with ExitStack() as L:
            Z = sb(L, "Z", [128, 512]); bZ = Buf("Z")
            P.op("pool", I("memset", Z[:], 0.0), w=[bZ])
            P.op("sp", I("dma_start", out=outT[0, 0:128, 0:512], in_=Z[:]), r=[bZ], w=[bOUT], dma=True, disjoint=True)
            P.barrier()
        P.emit()
        es.close()
        return nc

    with ExitStack() as Fs:
        WG = sb(Fs, "WG", [128, 8, DFF], BF16); bWG = Buf("WG")
        WU = sb(Fs, "WU", [128, 8, DFF], BF16); bWU = Buf("WU")
        WD = sb(Fs, "WD", [128, 22, D], BF16); bWD = Buf("WD")
        with ExitStack() as SU:
            stg = [(sb(SU, "fstg%d" % i, [128, DFF]), Buf("fstg%d" % i)) for i in range(4)]
            SR = Ring(stg)
            ei = 0
            for (wd, wt, wb) in ((w_gate_d, WG, bWG), (w_up_d, WU, bWU)):
                wv = wd.rearrange("(c p) n -> p c n", p=128)
                for c in range(8):
                    load_cast(SU, wt[:, c, :], wv[:, c, :], wb, 128, DFF, SR, ei, indep=True); ei += 1
            wv = w_down_d.rearrange("(c p) n -> p c n", p=128)
            for c in range(22):
                load_cast(SU, WD[:, c, :], wv[:, c, :], bWD, 128, D, SR, ei, indep=True); ei += 1
            P.barrier()
        H = sb(Fs, "H", [128, 8, 512]); bH = Buf("H")
        OUT = sb(Fs, "OUT", [128, 8, 512]); bOUTt = Buf("OUTt")
        HN = sb(Fs, "HN", [128, 8, 512], BF16); bHN = Buf("HN")
        RSa = sb(Fs, "RSa", [128, 512]); bRSa = Buf("RSa")
        RSb = sb(Fs, "RSb", [128, 512]); bRSb = Buf("RSb")
        AT = sb(Fs, "AT", [128, 22, 512], BF16); bAT = [Buf("AT%d" % i) for i in range(22)]
        SGR = Ring([(sb(Fs, "SGf%d" % i, [128, 512]), Buf("SGf%d" % i)) for i in range(2)])
        SQR = Ring([(sb(Fs, "SQf%d" % i, [128, 512], BF16), Buf("SQf%d" % i)) for i in range(4)])
        blocks = [(b, tb) for b in range(NB) for tb in range(NTB)]

        def stats(src, bsrc, RS, bRS):
            pt, pb = PSR.next()
            for c in range(8):
                sq, bsq = SQR.next()
                P.op("act", I("activation", out=sq[:], in_=src[:, c, :], func=AF.Square), r=[bsrc], w=[bsq])
                P.op("pe", I("matmul", pt[:, :], lhsT=ONES[:, :], rhs=sq[:], start=(c == 0), stop=(c == 7)),
                     r=[bONES, bsq], w=[pb])
            P.op("act", I("activation", out=RS[:], in_=pt[:, :], func=AF.Sqrt, bias=EPSB[:, 0:1], scale=1.0 / 1024.0),
                 r=[pb, bEPSB], w=[bRS])
            P.op("dve", I("reciprocal", out=RS[:], in_=RS[:]), r=[bRS], w=[bRS])

        def chain1(k):
            b, tb = blocks[k]
            hv = HTs[b].rearrange("(c p) t -> p c t", p=128)
            P.op("sp", I("dma_start", out=H[:], in_=hv[:, :, tb * 512:(tb + 1) * 512]), r=[bHTs[b]], w=[bH], dma=True)
            stats(H, bH, RSa, bRSa)

        def chain2(k):
            for c in range(8):
                P.op("dve", I("scalar_tensor_tensor", out=HN[:, c, :], in0=H[:, c, :], scalar=GV[:, 8 + c:9 + c],
                              in1=RSa[:], op0=ALU.mult, op1=ALU.mult), r=[bH, bGV, bRSa], w=[bHN])

        def copies(k):
            for c in range(8):
                P.op("pool", I("tensor_copy", out=OUT[:, c, :], in_=H[:, c, :]), r=[bH], w=[bOUTt])

        def gateup(k, f):
            pg, pgb = PSR.next()
            for c in range(8):
                P.op("pe", I("matmul", pg[:, :], lhsT=WG[:, c, f * 128:(f + 1) * 128], rhs=HN[:, c, :],
                             start=(c == 0), stop=(c == 7)), r=[bWG, bHN], w=[pgb])
            pu, pub = PSR.next()
            for c in range(8):
                P.op("pe", I("matmul", pu[:, :], lhsT=WU[:, c, f * 128:(f + 1) * 128], rhs=HN[:, c, :],
                             start=(c == 0), stop=(c == 7)), r=[bWU, bHN], w=[pub])
            SG, bSG = SGR.next()
            P.op("act", I("activation", out=SG[:], in_=pg[:, :], func=AF.Silu), r=[pgb], w=[bSG])
            P.op("dve", I("tensor_tensor", out=AT[:, f, :], in0=SG[:], in1=pu[:, :], op=ALU.mult),
                 r=[bSG, pub], w=[bAT[f]])

        def down(k):
            for m in range(8):
                pt, pb = PSR.next()
                for f in range(22):
                    P.op("pe", I("matmul", pt[:, :], lhsT=WD[:, f, m * 128:(m + 1) * 128], rhs=AT[:, f, :],
                                 start=(f == 0), stop=(f == 21)), r=[bWD, bAT[f]], w=[pb])
                P.op("dve", I("tensor_tensor", out=OUT[:, m, :], in0=OUT[:, m, :], in1=pt[:, :], op=ALU.add),
                     r=[bOUTt, pb], w=[bOUTt])

        def final(k):
            b, tb = blocks[k]
            ov = outT[b].rearrange("(c p) t -> p c t", p=128)
            stats(OUT, bOUTt, RSb, bRSb)
            for c in range(8):
                P.op("dve", I("scalar_tensor_tensor", out=OUT[:, c, :], in0=OUT[:, c, :], scalar=GV[:, 16 + c:17 + c],
                              in1=RSb[:], op0=ALU.mult, op1=ALU.mult), r=[bOUTt, bGV, bRSb], w=[bOUTt])
            P.op("pool", I("dma_start", out=ov[:, :, tb * 512:(tb + 1) * 512], in_=OUT[:]), r=[bOUTt], w=[bOUT], dma=True, disjoint=True)

        nblk = len(blocks)
        chain1(0)
        for k in range(nblk):
            chain2(k)
            for f in range(22):
                gateup(k, f)
                if f == 1:
                    if k > 0:
                        final(k - 1)
                    copies(k)
                if f == 14 and k + 1 < nblk:
                    chain1(k + 1)
            down(k)
        final(nblk - 1)
        P.barrier()
    P.emit()
    es.close()
    return nc


def prep_inputs(inp):
    f = lambda a: np.ascontiguousarray(np.asarray(a, dtype=np.float32))
    c = host_consts()
    shared = {
        "w_in": f(inp["w_in"][0]), "w_uq": f(inp["mla_w_uq"][0]), "w_ukv": f(inp["mla_w_ukv"][0]),
        "w1k": f(inp["nsa_cmp_w1_k"][0]), "w1v": f(inp["nsa_cmp_w1_v"][0]),
        "w2k": f(inp["nsa_cmp_w2_k"][0]), "w2v": f(inp["nsa_cmp_w2_v"][0]),
        "poskT": f(np.asarray(inp["nsa_cmp_pos_k"][0]).T), "posvT": f(np.asarray(inp["nsa_cmp_pos_v"][0]).T),
        "t5": f(inp["t5_table"]), "w_out": f(inp["w_out"][0]),
        "w_gate": f(inp["w_gate"][0]), "w_up": f(inp["w_up"][0]), "w_down": f(inp["w_down"][0]),
    }
    gv = np.zeros((128, 32), np.float32)
    gv[:, 0:8] = np.asarray(inp["norm_mix_g"][0], np.float32).reshape(8, 128).T
    gv[:, 8:16] = np.asarray(inp["norm_ffn_g"][0], np.float32).reshape(8, 128).T
    gv[:, 16:24] = np.asarray(inp["final_norm_g"], np.float32).reshape(8, 128).T
    gv[:, 24:26] = np.asarray(inp["mla_q_norm_g"][0], np.float32).reshape(2, 128).T
    gv[:, 26] = np.asarray(inp["mla_kv_norm_g"][0], np.float32)
    shared["gvec"] = gv
    go = np.concatenate([np.asarray(inp["out_norm_mla_g"][0], np.float32), np.asarray(inp["out_norm_nsa_g"][0], np.float32)])
    shared["gout"] = np.ascontiguousarray(np.broadcast_to(go[None, :], (128, 1024)))
    for k in ("tri", "farm", "antiI", "ident", "ind", "ohd", "ohc", "selc", "ovl", "rope"):
        shared[k] = c[k]
    x = np.asarray(inp["x"], np.float32)
    maps = []
    for i in range(NCORES):
        m = dict(shared)
        m["xT"] = np.ascontiguousarray(x[i * NB:(i + 1) * NB].transpose(0, 2, 1))
        maps.append(m)
    return maps


def kernel(**inputs):
    nc = build()
    maps = prep_inputs(inputs)
    res = run_bass_kernel_spmd(nc, maps, core_ids=list(range(NCORES)))
    out = np.empty((NCORES * NB, S, D), np.float32)
    for i in range(NCORES):
        o = np.asarray(res.results[i]["outT"], np.float32)
        out[i * NB:(i + 1) * NB] = o.transpose(0, 2, 1)
    return out
```

```python
import math
import os
from contextlib import ExitStack

import ml_dtypes
import numpy as np

import concourse.bass as bass
import concourse.mybir as mybir
from concourse.bass_utils import run_bass_kernel_spmd

F32 = mybir.dt.float32
BF16 = mybir.dt.bfloat16
AF = mybir.ActivationFunctionType
ALU = mybir.AluOpType
NPBF = ml_dtypes.bfloat16

S = 4096
D = 1024
NB = 2
NCORES = 8
DFF = 2816
NTB = S // 512
NT = S // 128
EPS = 1e-6
BIG = 30000.0
SC_M = 96 ** -0.5
HCL = 8176
ENGS = ("pe", "act", "dve", "pool", "sp")
RDMA = 8
STRICT_SAME = True
WARM_N = int(os.environ.get('MK_WARM', '128'))
LAG = int(os.environ.get('MK_LAG', '2'))


class Buf:
    __slots__ = ("name", "w", "r", "ep")

    def __init__(self, name):
        self.name = name
        self.w = None
        self.r = []
        self.ep = -1


class Op:
    __slots__ = ("eng", "fn", "deps", "dma", "n", "sig", "need", "dk", "dval", "bar", "tag")


def I(meth, *a, **k):
    return lambda e: getattr(e, meth)(*a, **k)


class Prog:
    def __init__(self, nc):
        self.nc = nc
        self.ops = {e: [] for e in ENGS}
        self.dmas = {e: [] for e in ENGS}
        self.epoch = 0
        self.tag = ""

    def op(self, eng, fn, r=(), w=(), dma=False, bar=False, extra=()):
        o = Op()
        o.eng, o.fn, o.dma, o.bar = eng, fn, dma, bar
        o.tag = self.tag
        o.need = False
        o.sig = None
        o.n = len(self.ops[eng])
        deps = {}
        for b in list(r) + list(w):
            if b.ep != self.epoch:
                b.w, b.r, b.ep = None, [], self.epoch
        for b in r:
            if b.w is not None:
                deps[b.w] = "raw"
        for b in w:
            if b.w is not None:
                deps.setdefault(b.w, "waw")
            for x in b.r:
                deps.setdefault(x, "war")
        for x in extra:
            deps[x] = "raw"
        fin = []
        for d, kind in deps.items():
            if d is o:
                continue
            if d.eng == eng and not d.dma and not dma and not bar:
                if eng == "pe":
                    continue
                if not STRICT_SAME and (kind != "raw" or o.n - d.n > 3):
                    continue
            fin.append(d)
        if dma:
            k = len(self.dmas[eng])
            o.dk = (eng, k % RDMA)
            o.dval = 16 * (k // RDMA + 1)
            if k >= RDMA:
                fin.append(self.dmas[eng][k - RDMA])
            self.dmas[eng].append(o)
        for d in fin:
            d.need = True
        o.deps = fin
        self.ops[eng].append(o)
        ws = set(id(b) for b in w)
        for b in w:
            b.w = o
            b.r = []
        for b in r:
            if id(b) not in ws:
                b.r.append(o)
        return o

    def barrier(self):
        last = []
        for e in ENGS:
            if self.ops[e]:
                last.append(self.ops[e][-1])
            last.extend(self.dmas[e][-RDMA:])
        bsp = self.op("sp", None, bar=True, extra=last)
        for e in ENGS:
            if e != "sp":
                self.op(e, None, bar=True, extra=[bsp])
        self.epoch += 1

    def check(self):
        done = set()
        pc = {e: 0 for e in ENGS}
        prog = True
        while prog:
            prog = False
            for e in ENGS:
                while pc[e] < len(self.ops[e]):
                    o = self.ops[e][pc[e]]
                    if all(id(d) in done for d in o.deps):
                        done.add(id(o))
                        pc[e] += 1
                        prog = True
                    else:
                        break
        bad = {e: pc[e] for e in ENGS if pc[e] < len(self.ops[e])}
        if bad:
            msg = []
            for e, i in bad.items():
                o = self.ops[e][i]
                msg.append("%s blocked at op %d/%d (%s) waiting on %s" % (
                    e, i, len(self.ops[e]), getattr(o, "tag", ""),
                    [(d.eng, d.n, getattr(d, "tag", "")) for d in o.deps if id(d) not in done]))
            raise RuntimeError("DEADLOCK: " + " | ".join(msg))

    def emit(self):
        self.check()
        nc = self.nc
        for e in ENGS:
            c = 0
            for o in self.ops[e]:
                if o.need and not o.dma:
                    c += 1
                    o.sig = c
        with ExitStack() as st:
            sem = {e: st.enter_context(nc.semaphore("s_" + e)) for e in ENGS}
            dsem = {}
            for e in ENGS:
                if self.dmas[e]:
                    for i in range(RDMA):
                        dsem[(e, i)] = st.enter_context(nc.semaphore("d_%s%d" % (e, i)))
            block = st.enter_context(nc.Block())

            def run(ename, eng):
                waited = {}
                for o in self.ops[ename]:
                    for d in o.deps:
                        if d.dma:
                            key, s_, v = d.dk, dsem[d.dk], d.dval
                        else:
                            key, s_, v = d.eng, sem[d.eng], d.sig
                        if waited.get(key, 0) >= v:
                            continue
                        eng.wait_ge(s_, v)
                        waited[key] = v
                    if o.bar:
                        if o.sig is not None:
                            eng.sem_inc(sem[ename], 1)
                        continue
                    ins = o.fn(eng)
                    if o.dma:
                        ins.then_inc(dsem[o.dk], 16)
                    elif o.sig is not None:
                        ins.then_inc(sem[ename], 1)
                if ename == "sp":
                    for q in ENGS:
                        for d in self.dmas[q][-RDMA:]:
                            if waited.get(d.dk, 0) < d.dval:
                                eng.wait_ge(dsem[d.dk], d.dval)
                                waited[d.dk] = d.dval

            @block.sync
            def _(e):
                run("sp", e)

            @block.tensor
            def _(e):
                run("pe", e)

            @block.scalar
            def _(e):
                run("act", e)

            @block.vector
            def _(e):
                run("dve", e)

            @block.gpsimd
            def _(e):
                run("pool", e)


class Ring:
    def __init__(self, items):
        self.items = items
        self.i = 0

    def next(self):
        x = self.items[self.i % len(self.items)]
        self.i += 1
        return x


def _bucket(d):
    n = np.maximum(d, 0)
    nf = np.maximum(n, 1).astype(np.float32)
    large = 16 + (np.log(nf / np.float32(16)) / np.float32(math.log(8.0)) * np.float32(16)).astype(np.int32)
    large = np.minimum(large, 31)
    return np.where(n < 16, n, large)


_CONST = None


def host_consts():
    global _CONST
    if _CONST is not None:
        return _CONST
    c = {}
    k = np.arange(128)
    c["tri"] = (k[:, None] <= k[None, :]).astype(NPBF)
    c["farm"] = (k[:, None] > k[None, :]).astype(NPBF)
    c["antiI"] = (k[:, None] == 127 - k[None, :]).astype(NPBF)
    c["ident"] = np.eye(128, dtype=np.float32)
    t = np.arange(S)
    c["ind"] = (t[None, :] // 64 == np.arange(64)[:, None]).astype(NPBF)
    d = np.arange(384) - 127
    oh = np.zeros((33, 384), np.float32)
    b = _bucket(d)
    for i in range(384):
        oh[32 if d[i] < 0 else b[i], i] = 1.0
    c["ohd"] = oh
    m = np.arange(HCL) - 4111
    oh = np.zeros((33, HCL), np.float32)
    b = _bucket(m)
    oh[np.where(m < 0, 32, b), np.arange(HCL)] = 1.0
    c["ohc"] = oh
    sel = np.zeros((NT, 128, 128), np.float32)
    j = np.arange(64)
    for qt in range(NT):
        q = qt * 128 + k
        cur = q // 64
        forced = (j[None, :] == 0) | (j[None, :] == cur[:, None]) | (j[None, :] == cur[:, None] - 1)
        causal = j[None, :] <= cur[:, None]
        sel[qt, :, :64] = (causal & ~forced)
        sel[qt, :, 64:] = np.where(forced, 1e30, np.where(causal, 0.0, -1e30))
    c["selc"] = sel
    ov = np.zeros((256, 64), np.float32)
    for npr in range(1, 256):
        n = 255 - npr
        lo = np.maximum(16 * n, 64 * j)
        hi = np.minimum(16 * n + 32, 64 * j + 64)
        ov[npr] = np.maximum(0, hi - lo) / 16.0
    c["ovl"] = ov.astype(NPBF)
    inv = (10000.0 ** (-np.arange(0, 32, 2, dtype=np.float32) / 32)).astype(np.float32)
    ang = t.astype(np.float32)[None, :] * inv[:, None]
    cos = np.cos(ang).astype(np.float32)
    sin = np.sin(ang).astype(np.float32)
    cosT = np.concatenate([cos, cos], 0)
    sinT = np.concatenate([-sin, sin], 0)
    rope = np.zeros((96, 4, S), np.float32)
    rope[64:96, 0] = cosT
    rope[64:96, 1] = sinT
    rope[64:96, 2] = cosT * np.float32(SC_M)
    rope[64:96, 3] = sinT * np.float32(SC_M)
    c["rope"] = rope
    _CONST = c
    return c


def build(debug=None):
    nc = bass.Bass("TRN2", target_bir_lowering=False)
    P = Prog(nc)
    es = ExitStack()

    def din(name, shape, dt=F32):
        return nc.dram_tensor(name, list(shape), dt, kind="ExternalInput")

    def dscr(name, shape, dt=F32):
        kind = "ExternalOutput" if (debug and name in debug) else "Internal"
        return nc.dram_tensor(name, list(shape), dt, kind=kind)

    xT = din("xT", [NB, D, S]).ap()
    w_in_d = din("w_in", [D, 1720]).ap()
    w_uq_d = din("w_uq", [256, 768]).ap()
    w_ukv_d = din("w_ukv", [128, 1024]).ap()
    w1k_d = din("w1k", [2048, 128]).ap()
    w1v_d = din("w1v", [2048, 128]).ap()
    w2k_d = din("w2k", [128, 64]).ap()
    w2v_d = din("w2v", [128, 64]).ap()
    posk_d = din("poskT", [64, 32]).ap()
    posv_d = din("posvT", [64, 32]).ap()
    t5_d = din("t5", [32, 8]).ap()
    w_out_d = din("w_out", [D, D]).ap()
    w_gate_d = din("w_gate", [D, DFF]).ap()
    w_up_d = din("w_up", [D, DFF]).ap()
    w_down_d = din("w_down", [DFF, D]).ap()
    gvec_d = din("gvec", [128, 32]).ap()
    gout_d = din("gout", [128, 1024]).ap()
    tri_d = din("tri", [128, 128], BF16).ap()
    farm_d = din("farm", [128, 128], BF16).ap()
    anti_d = din("antiI", [128, 128], BF16).ap()
    ident_d = din("ident", [128, 128]).ap()
    ind_d = din("ind", [64, S], BF16).ap()
    ohd_d = din("ohd", [33, 384]).ap()
    ohc_d = din("ohc", [33, HCL]).ap()
    selc_d = din("selc", [NT, 128, 128]).ap()
    ovl_d = din("ovl", [256, 64], BF16).ap()
    rope_d = din("rope", [96, 4, S]).ap()

    outT_h = nc.dram_tensor("outT", [NB, D, S], F32, kind="ExternalOutput")
    outT = outT_h.ap()

    QMs = dscr("QMs", [8, 96, S], BF16).ap()
    KMs = dscr("KMs", [8, 96, S], BF16).ap()
    VMs = dscr("VMs", [8, 128, NT, 65], BF16).ap()
    QNs = dscr("QNs", [8, 64, S], BF16).ap()
    KSs = dscr("KSs", [2, 64, S], BF16).ap()
    KWs = dscr("KWs", [2, 64, S], BF16).ap()
    VSs = dscr("VSs", [2, 128, NT, 65], BF16).ap()
    VWs = dscr("VWs", [2, 128, NT, 65], BF16).ap()
    NGs = dscr("NGs", [2, 64, S], BF16).ap()
    OMs = dscr("OMs", [S, 512]).ap()
    OCs = dscr("OCs", [S, 512]).ap()
    OSs = dscr("OSs", [S, 512]).ap()
    OWs = dscr("OWs", [S, 512]).ap()
    HTs = dscr("HTs", [NB, D, S]).ap()
    GD_h = dscr("GDs", [8, 384])
    HC_h = dscr("HCs", [8, HCL], BF16)
    GDs, HCs = GD_h.ap(), HC_h.ap()
    bQMs, bKMs, bVMs, bQNs = Buf("QMs"), Buf("KMs"), Buf("VMs"), Buf("QNs")
    bKSs, bKWs, bVSs, bVWs, bNGs = Buf("KSs"), Buf("KWs"), Buf("VSs"), Buf("VWs"), Buf("NGs")
    bOMs, bOCs, bOSs, bOWs = Buf("OMs"), Buf("OCs"), Buf("OSs"), Buf("OWs")
    bHTs = [Buf("HT0"), Buf("HT1")]
    bGDs, bHCs = Buf("GDs"), Buf("HCs")
    bOUT = Buf("out")

    uid = [0]

    def sb(stack, name, shape, dt=F32):
        uid[0] += 1
        return stack.enter_context(nc.sbuf_tensor("%s_%d" % (name, uid[0]), list(shape), dt))

    ps = [es.enter_context(nc.psum_tensor("ps%d" % i, [128, 512], F32)) for i in range(8)]
    psb = [Buf("ps%d" % i) for i in range(8)]
    PSR = Ring(list(zip(ps, psb)))

    GV = sb(es, "GV", [128, 32]); bGV = Buf("GV")
    ONES = sb(es, "ONES", [128, 128], BF16); bONES = Buf("ONES")
    IDN = sb(es, "IDN", [128, 128]); bIDN = Buf("IDN")
    P.op("sp", I("dma_start", out=GV[:], in_=gvec_d), w=[bGV], dma=True)
    P.op("sp", I("dma_start", out=IDN[:], in_=ident_d), w=[bIDN], dma=True)
    P.op("pool", I("memset", ONES[:], 1.0), w=[bONES])
    IDNB = sb(es, "IDNB", [128, 128], BF16); bIDNB = Buf("IDNB")
    P.op("dve", I("tensor_copy", out=IDNB[:], in_=IDN[:]), r=[bIDN], w=[bIDNB])
    EPSB = sb(es, "EPSB", [128, 1]); bEPSB = Buf("EPSB")
    P.op("pool", I("memset", EPSB[:], EPS), w=[bEPSB])

    def load_cast(stack_stage, dst_ap, src_ap, dstbuf, rows, cols, ring, eng_i, indep=False):
        if indep:
            dstbuf = Buf("chunk")
        stg, sbuf_ = ring.next()
        P.op("sp", I("dma_start", out=stg[0:rows, 0:cols], in_=src_ap), w=[sbuf_], dma=True)
        eng = ("dve", "act", "pool", "dve", "act")[eng_i % 5]
        if eng == "act":
            P.op(eng, I("activation", out=dst_ap, in_=stg[0:rows, 0:cols], func=AF.Copy), r=[sbuf_], w=[dstbuf])
        else:
            P.op(eng, I("tensor_copy", out=dst_ap, in_=stg[0:rows, 0:cols]), r=[sbuf_], w=[dstbuf])

    A = ExitStack()
    W_in = sb(A, "W_in", [128, 8, 1720], BF16); bW_in = Buf("W_in")
    W_uq = sb(A, "W_uq", [128, 2, 768], BF16); bW_uq = Buf("W_uq")
    W_uqB = sb(A, "W_uqB", [128, 2, 8, 96], BF16); bW_uqB = Buf("W_uqB")
    WkrA = sb(A, "WkrA", [128, 8, 96], BF16); bWkrA = Buf("WkrA")
    WkrB = sb(A, "WkrB", [128, 8, 96], BF16); bWkrB = Buf("WkrB")
    W_ukv = sb(A, "W_ukv", [128, 1024], BF16); bW_ukv = Buf("W_ukv")
    W2 = [sb(A, "W2k", [128, 64], BF16), sb(A, "W2v", [128, 64], BF16)]
    bW2 = [Buf("W2k"), Buf("W2v")]
    POS = [sb(A, "POSk", [64, 32], BF16), sb(A, "POSv", [64, 32], BF16)]
    bPOS = [Buf("POSk"), Buf("POSv")]
    TRI = sb(A, "TRI", [128, 128], BF16); bTRI = Buf("TRI")
    FARM = sb(A, "FARM", [128, 128], BF16); bFARM = Buf("FARM")
    EM = sb(A, "EM", [128, 8, 256], BF16); bEM = Buf("EM")
    T31B = sb(A, "T31B", [128, 8]); bT31B = Buf("T31B")
    OVL = sb(A, "OVL", [128, 2, 64], BF16); bOVL = Buf("OVL")
    GATES = sb(A, "GATES", [128, NT, 24]); bGATES = Buf("GATES")
    KCR = sb(A, "KCR", [128, S], BF16); bKCR = Buf("KCR")
    VCR = sb(A, "VCR", [128, S], BF16); bVCR = Buf("VCR")
    KCT = sb(A, "KCT", [64, 2, 256], BF16); bKCT = Buf("KCT")
    VCA = sb(A, "VCA", [128, 2, 2, 65], BF16); bVCA = Buf("VCA")

    with ExitStack() as SU:
        stg = [(sb(SU, "stg%d" % i, [128, 4096]), Buf("stg%d" % i)) for i in range(2)]
        SR = Ring(stg)
        ei = 0
        wv = w_in_d.rearrange("(c p) n -> p c n", p=128)
        for c in range(8):
            load_cast(SU, W_in[:, c, :], wv[:, c, :], bW_in, 128, 1720, SR, ei); ei += 1
        wv = w_uq_d.rearrange("(c p) n -> p c n", p=128)
        for c in range(2):
            load_cast(SU, W_uq[:, c, :], wv[:, c, :], bW_uq, 128, 768, SR, ei); ei += 1
        load_cast(SU, W_ukv[:, :], w_ukv_d, bW_ukv, 128, 1024, SR, ei); ei += 1
        for kv, (wd, pd) in enumerate(((w2k_d, posk_d), (w2v_d, posv_d))):
            load_cast(SU, W2[kv][:, :], wd, bW2[kv], 128, 64, SR, ei); ei += 1
            load_cast(SU, POS[kv][:, :], pd, bPOS[kv], 64, 32, SR, ei); ei += 1
        P.op("pool", I("memset", W_uqB[:], 0.0), w=[bW_uqB])
        uq4 = W_uq[:, :, :].rearrange("p c (h e) -> p c h e", e=96)
        P.op("pool", I("tensor_copy", out=W_uqB[:, :, :, 64:80], in_=uq4[:, :, :, 80:96]), r=[bW_uq], w=[bW_uqB])
        P.op("pool", I("tensor_copy", out=W_uqB[:, :, :, 80:96], in_=uq4[:, :, :, 64:80]), r=[bW_uq], w=[bW_uqB])
        P.op("pool", I("memset", WkrA[:], 0.0), w=[bWkrA])
        P.op("pool", I("memset", WkrB[:], 0.0), w=[bWkrB])
        P.op("pool", I("tensor_copy", out=WkrA[:, :, 64:96], in_=W_in[:, :, 384:416]), r=[bW_in], w=[bWkrA])
        P.op("pool", I("tensor_copy", out=WkrB[:, :, 64:80], in_=W_in[:, :, 400:416]), r=[bW_in], w=[bWkrB])
        P.op("pool", I("tensor_copy", out=WkrB[:, :, 80:96], in_=W_in[:, :, 384:400]), r=[bW_in], w=[bWkrB])
        P.op("sp", I("dma_start", out=TRI[:], in_=tri_d), w=[bTRI], dma=True)
        P.op("sp", I("dma_start", out=FARM[:], in_=farm_d), w=[bFARM], dma=True)
        P.op("sp", I("dma_start", out=OVL[:], in_=ovl_d.rearrange("(t p) j -> p t j", p=128)), w=[bOVL], dma=True)
        P.op("sp", I("dma_start", out=T31B[:], in_=t5_d[31:32, :].to_broadcast([128, 8])), w=[bT31B], dma=True)
        TBLX = sb(SU, "TBLX", [33, 8]); bTBLX = Buf("TBLX")
        NT31 = sb(SU, "NT31", [8, 1]); bNT31 = Buf("NT31")
        OHD = sb(SU, "OHD", [33, 384]); bOHD = Buf("OHD")
        ANTI = sb(SU, "ANTI", [128, 128], BF16); bANTI = Buf("ANTI")
        P.op("pool", I("memset", TBLX[32:33, :], -BIG), w=[bTBLX])
        P.op("sp", I("dma_start", out=TBLX[0:32, :], in_=t5_d), w=[bTBLX], dma=True)
        P.op("sp", I("dma_start", out=NT31[:], in_=t5_d[31:32, :].rearrange("a h -> h a")), w=[bNT31], dma=True)
        P.op("dve", I("tensor_scalar", out=NT31[:], in0=NT31[:], scalar1=-1.0, scalar2=None, op0=ALU.mult),
             r=[bNT31], w=[bNT31])
        P.op("sp", I("dma_start", out=OHD[:], in_=ohd_d), w=[bOHD], dma=True)
        P.op("sp", I("dma_start", out=ANTI[:], in_=anti_d), w=[bANTI], dma=True)
        pt, pb = PSR.next()
        P.op("pe", I("matmul", pt[0:8, 0:384], lhsT=TBLX[:, :], rhs=OHD[:, :], start=True, stop=True),
             r=[bTBLX, bOHD], w=[pb])
        GT = sb(SU, "GT", [8, 384]); bGT = Buf("GT")
        P.op("act", I("activation", out=GT[:], in_=pt[0:8, 0:384], func=AF.Exp, bias=NT31[:, 0:1], scale=1.0),
             r=[pb, bNT31], w=[bGT])
        P.op("pool", I("dma_start", out=GDs, in_=GT[:]), r=[bGT], w=[bGDs], dma=True)
        EMF = sb(SU, "EMF", [128, 256]); bEMF = Buf("EMF")
        EMFb = sb(SU, "EMFb", [128, 256], BF16); bEMFb = Buf("EMFb")
        for h in range(8):
            hank = bass.AP(tensor=GD_h, offset=h * 384, ap=[[1, 128], [1, 256]])
            P.op("sp", I("dma_start", out=EMF[:], in_=hank), r=[bGDs], w=[bEMF], dma=True)
            P.op("dve", I("tensor_copy", out=EMFb[:], in_=EMF[:]), r=[bEMF], w=[bEMFb])
            pt, pb = PSR.next()
            P.op("pe", I("matmul", pt[:, 0:256], lhsT=ANTI[:, :], rhs=EMFb[:, :], start=True, stop=True),
                 r=[bANTI, bEMFb], w=[pb])
            P.op("act", I("activation", out=EM[:, h, :], in_=pt[:, 0:256], func=AF.Copy), r=[pb], w=[bEM])
        OHC = [(sb(SU, "OHC%d" % i, [33, 512]), Buf("OHC%d" % i)) for i in range(2)]
        HCT = [(sb(SU, "HCT%d" % i, [8, 512], BF16), Buf("HCT%d" % i)) for i in range(2)]
        OR_, HR_ = Ring(OHC), Ring(HCT)
        for ch in range(16):
            n = min(512, HCL - ch * 512)
            ot, ob = OR_.next()
            ht, hb = HR_.next()
            P.op("sp", I("dma_start", out=ot[:, 0:n], in_=ohc_d[:, ch * 512:ch * 512 + n]), w=[ob], dma=True)
            pt, pb = PSR.next()
            P.op("pe", I("matmul", pt[0:8, 0:n], lhsT=TBLX[:, :], rhs=ot[:, 0:n], start=True, stop=True),
                 r=[bTBLX, ob], w=[pb])
            P.op("dve", I("tensor_copy", out=ht[:, 0:n], in_=pt[0:8, 0:n]), r=[pb], w=[hb])
            P.op("pool", I("dma_start", out=HCs[:, ch * 512:ch * 512 + n], in_=ht[:, 0:n]), r=[hb], w=[bHCs], dma=True)
        P.barrier()

    def stats_rstd(stack_tiles, src_sq_aps, nfeat, rbuf_list, RSTD, bRSTD):
        pt, pb = PSR.next()
        n = len(src_sq_aps)
        for i, (ap_, b_) in enumerate(src_sq_aps):
            P.op("pe", I("matmul", pt[:, :], lhsT=ONES[:, :], rhs=ap_, start=(i == 0), stop=(i == n - 1)),
                 r=[bONES, b_], w=[pb])
        P.op("act", I("activation", out=RSTD[:], in_=pt[:, :], func=AF.Sqrt, bias=EPSB[:, 0:1], scale=1.0 / nfeat),
             r=[pb, bEPSB], w=[bRSTD])
        P.op("dve", I("reciprocal", out=RSTD[:], in_=RSTD[:]), r=[bRSTD], w=[bRSTD])

    def phase_P(b):
        with ExitStack() as L:
            XR = [(sb(L, "X%d" % i, [128, 8, 512]), Buf("X%d" % i)) for i in range(2)]
            XNR = [(sb(L, "XN%d" % i, [128, 8, 512], BF16), Buf("XN%d" % i)) for i in range(2)]
            RSR = [(sb(L, "RSTD%d" % i, [128, 512]), Buf("RSTD%d" % i)) for i in range(2)]
            RPR = [(sb(L, "ROPE%d" % i, [96, 2, 512]), Buf("ROPE%d" % i)) for i in range(2)]
            SQ = sb(L, "SQ", [128, 8, 512], BF16); bSQ = Buf("SQ")
            RQ = sb(L, "RQ", [128, 512]); bRQ = Buf("RQ")
            RKV = sb(L, "RKV", [128, 512]); bRKV = Buf("RKV")
            CQf = sb(L, "CQf", [128, 2, 512]); bCQf = Buf("CQf")
            CQs = sb(L, "CQs", [128, 2, 512], BF16); bCQs = Buf("CQs")
            CQN = sb(L, "CQN", [128, 2, 512], BF16); bCQN = Buf("CQN")
            CKf = sb(L, "CKf", [128, 512]); bCKf = Buf("CKf")
            CKs = sb(L, "CKs", [128, 512], BF16); bCKs = Buf("CKs")
            CKN = sb(L, "CKN", [128, 512], BF16); bCKN = Buf("CKN")
            T1 = sb(L, "T1", [96, 512]); bT1 = Buf("T1")
            T2 = sb(L, "T2", [96, 512]); bT2 = Buf("T2")
            KPE = sb(L, "KPE", [96, 512], BF16); bKPE = Buf("KPE")
            QM = sb(L, "QM", [96, 8, 512], BF16); bQM = Buf("QM")
            KM = sb(L, "KM", [96, 8, 512], BF16); bKM = Buf("KM")
            VMR = Ring([(sb(L, "VM%d" % i, [128, 8, 4, 65], BF16), Buf("VM%d" % i)) for i in range(2)])
            QN = sb(L, "QN", [128, 4, 512], BF16); bQN = Buf("QN")
            KSR = Ring([(sb(L, "KS%d" % i, [128, 2, 512], BF16), Buf("KS%d" % i)) for i in range(2)])
            VSR = Ring([(sb(L, "VS%d" % i, [128, 4, 4, 65], BF16), Buf("VS%d" % i)) for i in range(2)])
            for (t_, b_) in VMR.items + VSR.items:
                P.op("pool", I("memset", t_[:], 1.0), w=[b_])
            xv = xT[b].rearrange("(c p) t -> p c t", p=128)

            def chain1(tb):
                t0 = tb * 512
                X, bX = XR[tb % 2]
                RSTD, bRSTD = RSR[tb % 2]
                ROPE, bROPE = RPR[tb % 2]
                P.op("sp", I("dma_start", out=X[:], in_=xv[:, :, t0:t0 + 512]), w=[bX], dma=True)
                P.op("sp", I("dma_start", out=ROPE[64:96, :, :], in_=rope_d[64:96, 0:2, t0:t0 + 512]), w=[bROPE], dma=True)
                P.op("act", I("activation", out=SQ[:], in_=X[:], func=AF.Square), r=[bX], w=[bSQ])
                stats_rstd(None, [(SQ[:, c, :], bSQ) for c in range(8)], 1024.0, None, RSTD, bRSTD)

            def chain2(tb):
                X, bX = XR[tb % 2]
                XN, bXN = XNR[tb % 2]
                RSTD, bRSTD = RSR[tb % 2]
                for c in range(8):
                    P.op("dve", I("scalar_tensor_tensor", out=XN[:, c, :], in0=X[:, c, :], scalar=GV[:, c:c + 1],
                                  in1=RSTD[:], op0=ALU.mult, op1=ALU.mult), r=[bX, bGV, bRSTD], w=[bXN])

            def body(tb, part):
                t0 = tb * 512
                XN, bXN = XNR[tb % 2]
                ROPE, bROPE = RPR[tb % 2]

                def proj(cols, M, wt=W_in, wb=bW_in):
                    pt, pb = PSR.next()
                    for c in range(8):
                        P.op("pe", I("matmul", pt[0:M, :], lhsT=wt[:, c, cols[0]:cols[1]], rhs=XN[:, c, :],
                                     start=(c == 0), stop=(c == 7)), r=[wb, bXN], w=[pb])
                    return pt, pb

                if part == 0:
                    for j in range(2):
                        pt, pb = proj((j * 128, (j + 1) * 128), 128)
                        P.op("act", I("activation", out=CQf[:, j, :], in_=pt[:, :], func=AF.Copy), r=[pb], w=[bCQf])
                        P.op("act", I("activation", out=CQs[:, j, :], in_=pt[:, :], func=AF.Square), r=[pb], w=[bCQs])
                    pt, pb = proj((256, 384), 128)
                    P.op("act", I("activation", out=CKf[:], in_=pt[:, :], func=AF.Copy), r=[pb], w=[bCKf])
                    P.op("act", I("activation", out=CKs[:], in_=pt[:, :], func=AF.Square), r=[pb], w=[bCKs])
                    pt, pb = proj((928, 1056), 128)
                    P.op("act", I("activation", out=KCR[:, t0:t0 + 512], in_=pt[:, :], func=AF.Copy), r=[pb], w=[bKCR])
                    pt, pb = proj((1056, 1184), 128)
                    P.op("dve", I("tensor_copy", out=VCR[:, t0:t0 + 512], in_=pt[:, :]), r=[pb], w=[bVCR])
                    stats_rstd(None, [(CQs[:, j, :], bCQs) for j in range(2)], 256.0, None, RQ, bRQ)
                    for j in range(2):
                        P.op("dve", I("scalar_tensor_tensor", out=CQN[:, j, :], in0=CQf[:, j, :], scalar=GV[:, 24 + j:25 + j],
                                      in1=RQ[:], op0=ALU.mult, op1=ALU.mult), r=[bCQf, bGV, bRQ], w=[bCQN])
                    stats_rstd(None, [(CKs[:], bCKs)], 128.0, None, RKV, bRKV)
                    P.op("dve", I("scalar_tensor_tensor", out=CKN[:], in0=CKf[:], scalar=GV[:, 26:27],
                                  in1=RKV[:], op0=ALU.mult, op1=ALU.mult), r=[bCKf, bGV, bRKV], w=[bCKN])
                    for j in range(4):
                        pt, pb = proj((416 + j * 128, 416 + (j + 1) * 128), 128)
                        if j % 2 == 0:
                            P.op("act", I("activation", out=QN[:, j, :], in_=pt[:, :], func=AF.Copy, scale=0.125),
                                 r=[pb], w=[bQN])
                        else:
                            P.op("dve", I("tensor_scalar", out=QN[:, j, :], in0=pt[:, :], scalar1=0.125, scalar2=None,
                                          op0=ALU.mult), r=[pb], w=[bQN])
                    P.op("pool", I("dma_start", out=QNs.rearrange("(j two) d t -> (two d) j t", two=2)[:, :, t0:t0 + 512],
                                   in_=QN[:]), r=[bQN], w=[bQNs], dma=True)
                    KS, bKS = KSR.next()
                    for i, c0 in enumerate((1184, 1440)):
                        pt, pb = proj((c0, c0 + 128), 128)
                        if i % 2 == 0:
                            P.op("act", I("activation", out=KS[:, i, :], in_=pt[:, :], func=AF.Copy), r=[pb], w=[bKS])
                        else:
                            P.op("dve", I("tensor_copy", out=KS[:, i, :], in_=pt[:, :]), r=[pb], w=[bKS])
                    P.op("pool", I("dma_start", out=KSs.rearrange("k d t -> (k d) t")[:, t0:t0 + 512], in_=KS[:, 0, :]),
                         r=[bKS], w=[bKSs], dma=True)
                    P.op("pool", I("dma_start", out=KWs.rearrange("k d t -> (k d) t")[:, t0:t0 + 512], in_=KS[:, 1, :]),
                         r=[bKS], w=[bKWs], dma=True)
                    return
                pa, pab = proj((0, 96), 96, wt=WkrA, wb=bWkrA)
                pbb_, pbbb = proj((0, 96), 96, wt=WkrB, wb=bWkrB)
                P.op("dve", I("tensor_tensor", out=T1[64:96, :], in0=pa[64:96, :], in1=ROPE[64:96, 0, :], op=ALU.mult),
                     r=[pab, bROPE], w=[bT1])
                P.op("dve", I("tensor_tensor", out=T2[64:96, :], in0=pbb_[64:96, :], in1=ROPE[64:96, 1, :], op=ALU.mult),
                     r=[pbbb, bROPE], w=[bT2])
                P.op("dve", I("tensor_tensor", out=KPE[64:96, :], in0=T1[64:96, :], in1=T2[64:96, :], op=ALU.add),
                     r=[bT1, bT2], w=[bKPE])
                for h in range(8):
                    pa, pab = PSR.next()
                    for j in range(2):
                        P.op("pe", I("matmul", pa[0:96, :], lhsT=W_uq[:, j, h * 96:(h + 1) * 96], rhs=CQN[:, j, :],
                                     start=(j == 0), stop=(j == 1)), r=[bW_uq, bCQN], w=[pab])
                    pq, pqb = PSR.next()
                    for j in range(2):
                        P.op("pe", I("matmul", pq[0:96, :], lhsT=W_uqB[:, j, h, :], rhs=CQN[:, j, :],
                                     start=(j == 0), stop=(j == 1)), r=[bW_uqB, bCQN], w=[pqb])
                    P.op("act", I("activation", out=QM[0:64, h, :], in_=pa[0:64, :], func=AF.Copy, scale=SC_M),
                         r=[pab], w=[bQM])
                    P.op("dve", I("scalar_tensor_tensor", out=T1[64:96, :], in0=pa[64:96, :], scalar=SC_M,
                                  in1=ROPE[64:96, 0, :], op0=ALU.mult, op1=ALU.mult), r=[pab, bROPE], w=[bT1])
                    P.op("dve", I("scalar_tensor_tensor", out=T2[64:96, :], in0=pq[64:96, :], scalar=SC_M,
                                  in1=ROPE[64:96, 1, :], op0=ALU.mult, op1=ALU.mult), r=[pqb, bROPE], w=[bT2])
                    P.op("dve", I("tensor_tensor", out=QM[64:96, h, :], in0=T1[64:96, :], in1=T2[64:96, :], op=ALU.add),
                         r=[bT1, bT2], w=[bQM])
                    pk, pkb = PSR.next()
                    P.op("pe", I("matmul", pk[:, :], lhsT=W_ukv[:, h * 128:(h + 1) * 128], rhs=CKN[:, :],
                                 start=True, stop=True), r=[bW_ukv, bCKN], w=[pkb])
                    P.op("act", I("activation", out=KM[0:64, h, :], in_=pk[0:64, :], func=AF.Copy), r=[pkb], w=[bKM])
                    P.op("pool", I("tensor_copy", out=KM[64:96, h, :], in_=KPE[64:96, :]), r=[bKPE], w=[bKM])
                P.op("pool", I("dma_start", out=QMs.rearrange("h d t -> d h t")[:, :, t0:t0 + 512], in_=QM[:]),
                     r=[bQM], w=[bQMs], dma=True)
                P.op("pool", I("dma_start", out=KMs.rearrange("h d t -> d h t")[:, :, t0:t0 + 512], in_=KM[:]),
                     r=[bKM], w=[bKMs], dma=True)
                VM, bVM = VMR.next()
                wv4 = W_ukv[:, :].rearrange("p (h e) -> p h e", e=128)
                for ts in range(4):
                    pt, pb = PSR.next()
                    P.op("pe", I("matmul", pt[:, :], lhsT=CKN[:, ts * 128:(ts + 1) * 128], rhs=wv4[:, :, 64:128],
                                 start=True, stop=True), r=[bCKN, bW_ukv], w=[pb])
                    P.op("act", I("activation", out=VM[:, :, ts, 0:64], in_=pt[:, :].rearrange("p (h e) -> p h e", e=64),
                                  func=AF.Copy), r=[pb], w=[bVM])
                for h in range(8):
                    P.op("pool", I("dma_start", out=VMs[h, :, tb * 4:tb * 4 + 4, :], in_=VM[:, h, :, :]),
                         r=[bVM], w=[bVMs], dma=True)
                VS, bVS = VSR.next()
                for ts in range(4):
                    pt, pb = PSR.next()
                    for c in range(8):
                        P.op("pe", I("matmul", pt[:, 0:128], lhsT=XN[:, c, ts * 128:(ts + 1) * 128], rhs=W_in[:, c, 1312:1440],
                                     start=(c == 0), stop=(c == 7)), r=[bXN, bW_in], w=[pb])
                    P.op("act", I("activation", out=VS[:, 0:2, ts, 0:64],
                                  in_=pt[:, 0:128].rearrange("p (k e) -> p k e", e=64), func=AF.Copy), r=[pb], w=[bVS])
                    pt, pb = PSR.next()
                    for c in range(8):
                        P.op("pe", I("matmul", pt[:, 0:152], lhsT=XN[:, c, ts * 128:(ts + 1) * 128], rhs=W_in[:, c, 1568:1720],
                                     start=(c == 0), stop=(c == 7)), r=[bXN, bW_in], w=[pb])
                    P.op("dve", I("tensor_copy", out=VS[:, 2:4, ts, 0:64],
                                  in_=pt[:, 0:128].rearrange("p (k e) -> p k e", e=64)), r=[pb], w=[bVS])
                    P.op("act", I("activation", out=GATES[:, tb * 4 + ts, :], in_=pt[:, 128:152], func=AF.Sigmoid),
                         r=[pb], w=[bGATES])
                for k_ in range(2):
                    P.op("pool", I("dma_start", out=VSs[k_, :, tb * 4:tb * 4 + 4, :], in_=VS[:, k_, :, :]),
                         r=[bVS], w=[bVSs], dma=True)
                    P.op("pool", I("dma_start", out=VWs[k_, :, tb * 4:tb * 4 + 4, :], in_=VS[:, 2 + k_, :, :]),
                         r=[bVS], w=[bVWs], dma=True)

            chain1(0)
            chain2(0)
            for tb in range(NTB):
                body(tb, 0)
                if tb + 1 < NTB:
                    chain1(tb + 1)
                body(tb, 1)
                if tb + 1 < NTB:
                    chain2(tb + 1)
            P.barrier()

    def phase_C():
        with ExitStack() as L:
            BIA = sb(L, "BIA", [128, 1]); bBIA = Buf("BIA")
            Hf = sb(L, "Hf", [128, 256]); bHf = Buf("Hf")
            H2 = sb(L, "H2", [128, 256]); bH2 = Buf("H2")
            SG = sb(L, "SG", [128, 256]); bSG = Buf("SG")
            GH = sb(L, "GH", [128, 256], BF16); bGH = Buf("GH")
            W1 = [sb(L, "W1k", [128, 32, 128], BF16), sb(L, "W1v", [128, 32, 128], BF16)]
            bW1 = [Buf("W1k"), Buf("W1v")]
            stg_ = sb(L, "cstg", [128, 4096]); sbuf_ = Buf("cstg")
            sv = stg_[:, :].rearrange("p (l h) -> p l h", h=128)
            for kv, wd in enumerate((w1k_d, w1v_d)):
                src = wd.rearrange("(l d) h -> d l h", d=64)
                for half in range(2):
                    P.op("sp", I("dma_start", out=sv[half * 64:(half + 1) * 64, :, :], in_=src), w=[sbuf_], dma=True)
                P.op("dve", I("tensor_copy", out=W1[kv][:, :, :], in_=sv[:, :, :]), r=[sbuf_], w=[bW1[kv]])
            P.op("pool", I("memset", VCA[:], 0.0), w=[bVCA])
            P.op("pool", I("memset", KCT[:], 0.0), w=[bKCT])
            for kv in range(2):
                RAW, bRAW = (KCR, bKCR) if kv == 0 else (VCR, bVCR)
                pbia, pbiab = PSR.next()
                for l in range(32):
                    P.op("pe", I("matmul", pbia[:, 0:1], lhsT=W1[kv][0:64, l, :], rhs=POS[kv][0:64, l:l + 1],
                                 start=(l == 0), stop=(l == 31)), r=[bW1[kv], bPOS[kv]], w=[pbiab])
                P.op("dve", I("tensor_copy", out=BIA[:], in_=pbia[:, 0:1]), r=[pbiab], w=[bBIA])
                for kh in range(2):
                    p0 = kh * 64
                    pt, pb = PSR.next()
                    for l in range(32):
                        P.op("pe", I("matmul", pt[:, 0:255], lhsT=W1[kv][p0:p0 + 64, l, :],
                                     rhs=RAW[p0:p0 + 64, l:l + 16 * 254 + 1:16], start=(l == 0), stop=(l == 31)),
                             r=[bW1[kv], bRAW], w=[pb])
                    P.op("act", I("activation", out=Hf[:, 0:255], in_=pt[:, 0:255], func=AF.Identity, bias=BIA[:, 0:1],
                                  scale=1.0), r=[pb, bBIA], w=[bHf])
                    P.op("dve", I("tensor_tensor", out=H2[:, 0:255], in0=Hf[:, 0:255], in1=Hf[:, 0:255], op=ALU.mult),
                         r=[bHf], w=[bH2])
                    P.op("dve", I("tensor_scalar", out=H2[:, 0:255], in0=H2[:, 0:255], scalar1=0.044715, scalar2=1.0,
                                  op0=ALU.mult, op1=ALU.add), r=[bH2], w=[bH2])
                    P.op("dve", I("tensor_tensor", out=H2[:, 0:255], in0=H2[:, 0:255], in1=Hf[:, 0:255], op=ALU.mult),
                         r=[bH2, bHf], w=[bH2])
                    P.op("act", I("activation", out=SG[:, 0:255], in_=H2[:, 0:255], func=AF.Sigmoid,
                                  scale=2.0 * math.sqrt(2.0 / math.pi)), r=[bH2], w=[bSG])
                    P.op("pool", I("memset", GH[:], 0.0), w=[bGH])
                    rev = bass.AP(tensor=GH, offset=GH[:, 255:256].offset, ap=[list(GH[:].ap[0]), [-1, 255]])
                    P.op("dve", I("tensor_tensor", out=rev, in0=SG[:, 0:255], in1=Hf[:, 0:255], op=ALU.mult),
                         r=[bSG, bHf], w=[bGH])
                    if kv == 0:
                        pt, pb = PSR.next()
                        P.op("pe", I("matmul", pt[0:64, 0:256], lhsT=W2[0][:, :], rhs=GH[:, :], start=True, stop=True),
                             r=[bW2[0], bGH], w=[pb])
                        P.op("act", I("activation", out=KCT[:, kh, :], in_=pt[0:64, 0:256], func=AF.Copy),
                             r=[pb], w=[bKCT])
                    else:
                        for nt in range(2):
                            pt, pb = PSR.next()
                            P.op("pe", I("matmul", pt[:, 0:64], lhsT=GH[:, nt * 128:(nt + 1) * 128], rhs=W2[1][:, :],
                                         start=True, stop=True), r=[bW2[1], bGH], w=[pb])
                            P.op("act", I("activation", out=VCA[:, kh, nt, 0:64], in_=pt[:, 0:64], func=AF.Copy),
                                 r=[pb], w=[bVCA])
            P.op("pool", I("memset", VCA[:, :, 1, 64:65], 1.0), w=[bVCA])
            P.op("pool", I("memset", VCA[:, :, 0, 64:65], 1.0), w=[bVCA])
            P.op("pool", I("memset", VCA[0:1, :, 0, :], 0.0), w=[bVCA])
            P.barrier()

    def run_attention(L, jobs):
        SR_ = Ring([(ps[i], psb[i]) for i in range(0, 4)])
        OR_ = Ring([(ps[i], psb[i]) for i in range(4, 6)])
        PTR = Ring([(sb(L, "PT%d" % i, [128, 512], BF16), Buf("PT%d" % i)) for i in range(5)])
        flat = []
        for ji, job in enumerate(jobs):
            nt_ = len(job["tiles"])
            job["ji"] = ji
            for ti in range(nt_):
                flat.append((job, ti, ti == 0, ti == nt_ - 1))
        state = {}
        loaded = set()
        LOOK = 24

        def stage1(i):
            job, ti, first, last = flat[i]
            for k2 in range(i, min(len(flat), i + LOOK)):
                j2 = flat[k2][0]
                if j2["ji"] > job["ji"] + 3:
                    break
                if id(j2) not in loaded:
                    loaded.add(id(j2))
                    if j2["load"] is not None:
                        j2["load"]()
            tl = job["tiles"][ti]
            kT, kb, vA, vb, lo, hi, masks = tl
            qap, qb_ = job["q"]
            st_, sbf = SR_.next()
            ptile, pbf = PTR.next()
            c0, c1 = lo * 128, hi * 128
            P.op("pe", I("matmul", st_[:, c0:c1], lhsT=kT, rhs=qap[:, c0:c1], start=True, stop=True),
                 r=[kb, qb_], w=[sbf])
            P.op("act", I("activation", out=ptile[:, c0:c1], in_=st_[:, c0:c1], func=AF.Exp, bias=job["bias"], scale=1.0),
                 r=[sbf] + job["rbias"], w=[pbf])
            for (qs, map_, mb) in masks:
                P.op("dve", I("tensor_tensor", out=ptile[:, qs * 128:(qs + 1) * 128], in0=ptile[:, qs * 128:(qs + 1) * 128],
                              in1=map_, op=ALU.mult), r=[pbf, mb], w=[pbf])
            state[i] = (ptile, pbf)

        OSR = Ring([(sb(L, "OSb%d" % i, [128, 512]), Buf("OSb%d" % i)) for i in range(2)])
        for (t_, b_) in OSR.items:
            P.op("pool", I("memset", t_[:], 0.0), w=[b_])
        TR_ = Ring([(ps[i], psb[i]) for i in range(6, 7)])
        DUMW = sb(L, "DUMW", [128, 512], BF16); bDUMW = Buf("DUMW")
        P.op("pool", I("memset", DUMW[:], 0.5), w=[bDUMW])
        bDUM = Buf("DUM")
        pending = []

        def stage2(i):
            job, ti, first, last = flat[i]
            kT, kb, vA, vb, lo, hi, masks = job["tiles"][ti]
            ptile, pbf = state.pop(i)
            if first:
                job["O"] = OR_.next()
                job["started"] = False
            O, Ob = job["O"]
            c0, c1 = lo * 128, hi * 128
            if WARM_N:
                P.op("pe", I("matmul", ps[7][:, 0:WARM_N], lhsT=ONES[:, :], rhs=DUMW[:, 0:WARM_N], start=True, stop=True),
                     r=[bONES, bDUMW], w=[bDUM])
            P.op("pe", I("matmul", O[0:65, c0:c1], lhsT=vA, rhs=ptile[:, c0:c1],
                         start=(not job["started"]), stop=last, skip_group_check=True), r=[pbf, vb], w=[Ob])
            job["started"] = True
            if last:
                OS_, bOS_ = OSR.next()
                P.op("dve", I("tensor_copy", out=OS_[0:65, :], in_=O[0:65, :]), r=[Ob], w=[bOS_])

                def fin(job=job, OS_=OS_, bOS_=bOS_):
                    Tt, Tb = TR_.next()
                    Tv = Tt[:, :].rearrange("p (s e) -> p s e", e=128)
                    for qs in range(4):
                        P.op("pe", I("transpose", out=Tv[:, qs, :], in_=OS_[:, qs * 128:(qs + 1) * 128],
                                     identity=IDN[:, :]), r=[bOS_, bIDN], w=[Tb])
                    job["evac"](Tv, Tb)
                pending.append((i + LAG + 2, fin))

        n = len(flat)
        for i in range(n + LAG):
            if i < n:
                stage1(i)
            if i >= LAG:
                stage2(i - LAG)
            while pending and pending[0][0] <= i:
                pending.pop(0)[1]()
        while pending:
            pending.pop(0)[1]()

    def std_evac(L, name):
        RR = Ring([(sb(L, name + "R%d" % i, [128, 4]), Buf(name + "R%d" % i)) for i in range(2)])
        OTR = Ring([(sb(L, name + "OT%d" % i, [128, 4, 64]), Buf(name + "OT%d" % i)) for i in range(2)])

        def mk(dst, dbuf, h, qb, gate_col):
            def evac(Ov, Ob):
                R, bR = RR.next()
                OT, bOT = OTR.next()
                P.op("dve", I("reciprocal", out=R[:, :], in_=Ov[:, :, 64]), r=[Ob], w=[bR])
                for qs in range(4):
                    if gate_col is None:
                        P.op("dve", I("tensor_scalar", out=OT[:, qs, :], in0=Ov[:, qs, 0:64], scalar1=R[:, qs:qs + 1],
                                      scalar2=None, op0=ALU.mult), r=[Ob, bR], w=[bOT])
                    else:
                        g = GATES[:, qb * 4 + qs, gate_col:gate_col + 1]
                        P.op("dve", I("tensor_scalar", out=OT[:, qs, :], in0=Ov[:, qs, 0:64], scalar1=R[:, qs:qs + 1],
                                      scalar2=g, op0=ALU.mult, op1=ALU.mult), r=[Ob, bR, bGATES], w=[bOT])
                dv = dst.rearrange("(t p) c -> p t c", p=128)[:, qb * 4:qb * 4 + 4, h * 64:(h + 1) * 64]
                P.op("pool", I("dma_start", out=dv, in_=OT[:]), r=[bOT], w=[dbuf], dma=True)
            return evac
        return mk

    def phase_MLA():
        with ExitStack() as L:
            KR = Ring([(sb(L, "K%d" % i, [96, S], BF16), Buf("K%d" % i)) for i in range(2)])
            VR = Ring([(sb(L, "V%d" % i, [128, NT, 65], BF16), Buf("V%d" % i)) for i in range(2)])
            QR = Ring([(sb(L, "Q%d" % i, [96, 512], BF16), Buf("Q%d" % i)) for i in range(6)])
            mk = std_evac(L, "m")
            jobs = []
            for h in range(8):
                kv = {}
                for qb in range(NTB):
                    job = {}

                    def load(h=h, qb=qb, job=job, kv=kv):
                        if qb == 0:
                            kv["K"] = KR.next()
                            kv["V"] = VR.next()
                            P.op("sp", I("dma_start", out=kv["K"][0][:], in_=KMs[h]), r=[bKMs], w=[kv["K"][1]], dma=True)
                            P.op("sp", I("dma_start", out=kv["V"][0][:], in_=VMs[h]), r=[bVMs], w=[kv["V"][1]], dma=True)
                        Q, bQ = QR.next()
                        P.op("sp", I("dma_start", out=Q[:], in_=QMs[h, :, qb * 512:(qb + 1) * 512]), r=[bQMs], w=[bQ], dma=True)
                        job["q"] = (Q, bQ)
                        K, bK = kv["K"]
                        V, bV = kv["V"]
                        tiles = []
                        for kt in range(4 * qb + 4):
                            lo = max(0, kt - 4 * qb)
                            masks = [(lo, TRI[:, :], bTRI)] if kt >= 4 * qb else []
                            tiles.append((K[:, kt * 128:(kt + 1) * 128], bK, V[:, kt, :], bV, lo, 4, masks))
                        job["tiles"][:] = tiles
                    job["load"] = load
                    job["tiles"] = [None] * (4 * qb + 4)
                    job["bias"] = 0.0
                    job["rbias"] = []
                    job["evac"] = mk(OMs, bOMs, h, qb, None)
                    jobs.append(job)
            run_attention(L, jobs)
            P.barrier()

    def phase_NC():
        with ExitStack() as L:
            QR = Ring([(sb(L, "cQ%d" % i, [64, 512], BF16), Buf("cQ%d" % i)) for i in range(4)])
            BCR = Ring([(sb(L, "BC%d" % i, [128, 512], BF16), Buf("BC%d" % i)) for i in range(4)])
            SSR = Ring([(sb(L, "SS%d" % i, [128, 512]), Buf("SS%d" % i)) for i in range(3)])
            ER = Ring([(sb(L, "E%d" % i, [128, 512], BF16), Buf("E%d" % i)) for i in range(5)])
            RR = Ring([(sb(L, "cR%d" % i, [128, 4]), Buf("cR%d" % i)) for i in range(2)])
            OTR = Ring([(sb(L, "cOT%d" % i, [128, 4, 64]), Buf("cOT%d" % i)) for i in range(2)])
            IMP = sb(L, "IMP", [128, 4, 64]); bIMP = Buf("IMP")
            SELC = Ring([(sb(L, "SELC%d" % i, [128, 4, 128]), Buf("SELC%d" % i)) for i in range(2)])
            M8 = sb(L, "M8", [128, 4, 16]); bM8 = Buf("M8")
            TMPR = Ring([(sb(L, "TMP%d" % i, [128, 4, 64]), Buf("TMP%d" % i)) for i in range(2)])
            NGT = Ring([(sb(L, "NGT%d" % i, [64, 512], BF16), Buf("NGT%d" % i)) for i in range(2)])
            S_R = Ring([(ps[i], psb[i]) for i in range(0, 3)])
            O_R = Ring([(ps[i], psb[i]) for i in range(3, 5)])
            I_R = Ring([(ps[i], psb[i]) for i in range(5, 7)])
            T_R = Ring([(ps[7], psb[7])])
            units = []
            for kvh in range(2):
                for qb in range(NTB):
                    for g in range(4):
                        units.append((kvh, qb, g))
            ctx = {}

            def stageA(u):
                kvh, qb, g = units[u]
                h = kvh * 4 + g
                nts = [1] if qb < 4 else [0, 1]
                if g == 0:
                    SC_, bSC = SELC.next()
                    P.op("sp", I("dma_start", out=SC_[:], in_=selc_d[qb * 4:qb * 4 + 4].rearrange("t p c -> p t c")),
                         w=[bSC], dma=True)
                    ctx[(kvh, qb)] = (SC_, bSC)
                Q, bQ = QR.next()
                P.op("sp", I("dma_start", out=Q[:], in_=QNs[h, :, qb * 512:(qb + 1) * 512]), r=[bQNs], w=[bQ], dma=True)
                es_ = []
                for ni, nt in enumerate(nts):
                    BC, bBC = BCR.next()
                    src = bass.AP(tensor=HC_h, offset=h * HCL + 2048 * nt + 512 * qb, ap=[[16, 128], [1, 512]])
                    P.op("sp", I("dma_start", out=BC[:], in_=src), r=[bHCs], w=[bBC], dma=True)
                    St, Sb = S_R.next()
                    P.op("pe", I("matmul", St[:, :], lhsT=KCT[:, kvh, nt * 128:(nt + 1) * 128], rhs=Q[:, :],
                                 start=True, stop=False), r=[bKCT, bQ], w=[Sb])
                    P.op("pe", I("matmul", St[:, :], lhsT=IDNB[:, :], rhs=BC[:, :], start=False, stop=True),
                         r=[bIDNB, bBC], w=[Sb])
                    E, bE = ER.next()
                    P.op("act", I("activation", out=E[:], in_=St[:, :], func=AF.Exp), r=[Sb], w=[bE])
                    es_.append((nt, E, bE))
                ctx[u] = es_

            def stageB(u):
                kvh, qb, g = units[u]
                h = kvh * 4 + g
                es_ = ctx.pop(u)
                O, Ob = O_R.next()
                Im, Imb = I_R.next()
                Ov = O[:, 0:260].rearrange("p (s e) -> p s e", e=65)
                Iv = Im[:, 0:256].rearrange("p (s e) -> p s e", e=64)
                first = True
                for ni, (nt, E, bE) in enumerate(es_):
                    for qs in range(4):
                        P.op("pe", I("matmul", Ov[:, qs, :], lhsT=E[:, qs * 128:(qs + 1) * 128], rhs=VCA[:, kvh, nt, :],
                                     start=first, stop=(ni == len(es_) - 1), skip_group_check=True),
                             r=[bE, bVCA], w=[Ob])
                        P.op("pe", I("matmul", Iv[:, qs, :], lhsT=E[:, qs * 128:(qs + 1) * 128], rhs=OVL[:, nt, :],
                                     start=first, stop=(ni == len(es_) - 1), skip_group_check=True),
                             r=[bE, bOVL], w=[Imb])
                        first = False
                R, bR = RR.next()
                OT, bOT = OTR.next()
                P.op("dve", I("tensor_scalar", out=R[:, :], in0=Ov[:, :, 64], scalar1=1e-30, scalar2=None,
                              op0=ALU.add), r=[Ob], w=[bR])
                P.op("dve", I("reciprocal", out=R[:, :], in_=R[:, :]), r=[bR], w=[bR])
                for qs in range(4):
                    gcol = GATES[:, qb * 4 + qs, 3 * h:3 * h + 1]
                    P.op("dve", I("tensor_scalar", out=OT[:, qs, :], in0=Ov[:, qs, 0:64], scalar1=R[:, qs:qs + 1],
                                  scalar2=gcol, op0=ALU.mult, op1=ALU.mult), r=[Ob, bR, bGATES], w=[bOT])
                    if g == 0:
                        P.op("dve", I("tensor_scalar", out=IMP[:, qs, :], in0=Iv[:, qs, :], scalar1=R[:, qs:qs + 1],
                                      scalar2=None, op0=ALU.mult), r=[Imb, bR], w=[bIMP])
                    else:
                        P.op("dve", I("scalar_tensor_tensor", out=IMP[:, qs, :], in0=Iv[:, qs, :],
                                      scalar=R[:, qs:qs + 1], in1=IMP[:, qs, :], op0=ALU.mult, op1=ALU.add),
                             r=[Imb, bR, bIMP], w=[bIMP])
                dv = OCs.rearrange("(t p) c -> p t c", p=128)[:, qb * 4:qb * 4 + 4, h * 64:(h + 1) * 64]
                P.op("pool", I("dma_start", out=dv, in_=OT[:]), r=[bOT], w=[bOCs], dma=True)
                if g == 3:
                    SC_, bSC = ctx.pop((kvh, qb))
                    P.op("dve", I("tensor_tensor", out=IMP[:], in0=IMP[:], in1=SC_[:, :, 0:64], op=ALU.mult),
                         r=[bIMP, bSC], w=[bIMP])
                    P.op("dve", I("tensor_tensor", out=IMP[:], in0=IMP[:], in1=SC_[:, :, 64:128], op=ALU.add),
                         r=[bIMP, bSC], w=[bIMP])
                    TMP, bTMP = TMPR.next()
                    for qs in range(4):
                        P.op("dve", I("max", out=M8[:, qs, 0:8], in_=IMP[:, qs, :]), r=[bIMP], w=[bM8])
                        P.op("dve", I("match_replace", out=TMP[:, qs, :], in_to_replace=M8[:, qs, 0:8], in_values=IMP[:, qs, :],
                                      imm_value=-3.0e38), r=[bM8, bIMP], w=[bTMP])
                        P.op("dve", I("max", out=M8[:, qs, 8:16], in_=TMP[:, qs, :]), r=[bTMP], w=[bM8])
                        P.op("dve", I("tensor_scalar", out=TMP[:, qs, :], in0=IMP[:, qs, :], scalar1=M8[:, qs, 15:16],
                                      scalar2=1.0, op0=ALU.is_ge, op1=ALU.subtract), r=[bIMP, bM8], w=[bTMP])

                    def topk_tail(kvh=kvh, qb=qb, TMP=TMP, bTMP=bTMP):
                        NG, bNG = NGT.next()
                        for qs in range(4):
                            Tt, Tb = T_R.next()
                            P.op("pe", I("transpose", out=Tt[0:64, 0:128], in_=TMP[:, qs, :], identity=IDN[:, :]),
                                 r=[bTMP, bIDN], w=[Tb])
                            P.op("act", I("activation", out=NG[:, qs * 128:(qs + 1) * 128], in_=Tt[0:64, 0:128], func=AF.Copy,
                                          scale=BIG), r=[Tb], w=[bNG])
                        P.op("pool", I("dma_start", out=NGs[kvh, :, qb * 512:(qb + 1) * 512], in_=NG[:]), r=[bNG], w=[bNGs], dma=True)
                    return topk_tail
                return None

            nu = len(units)
            tail = None
            for u in range(nu + 1):
                if u < nu:
                    stageA(u)
                if tail is not None:
                    tail()
                    tail = None
                if u >= 1:
                    tail = stageB(u - 1)
            if tail is not None:
                tail()
            P.barrier()

    def phase_NSW():
        with ExitStack() as L:
            KSA = Ring([(sb(L, "KSA%d" % i, [128, S], BF16), Buf("KSA%d" % i)) for i in range(2)])
            KWR = Ring([(sb(L, "KW%d" % i, [128, S], BF16), Buf("KW%d" % i)) for i in range(2)])
            for (t_, b_) in KWR.items:
                P.op("pool", I("memset", t_[64:128, :], 0.0), w=[b_])
            VSR = Ring([(sb(L, "VSa%d" % i, [128, NT, 65], BF16), Buf("VSa%d" % i)) for i in range(2)])
            VWR = Ring([(sb(L, "VWa%d" % i, [128, NT, 65], BF16), Buf("VWa%d" % i)) for i in range(2)])
            QR = Ring([(sb(L, "QA%d" % i, [128, 512], BF16), Buf("QA%d" % i)) for i in range(6)])
            mks = std_evac(L, "s")
            mkw = std_evac(L, "w")
            jobs = []
            for kvh in range(2):
                kv = {}
                for g in range(4):
                    h = kvh * 4 + g
                    for qb in range(NTB):
                        js, jw = {}, {}

                        def load(h=h, g=g, kvh=kvh, qb=qb, js=js, jw=jw, kv=kv):
                            if g == 0 and qb == 0:
                                kv["KS"], kv["KW"], kv["VS"], kv["VW"] = KSA.next(), KWR.next(), VSR.next(), VWR.next()
                                P.op("sp", I("dma_start", out=kv["KS"][0][0:64, :], in_=KSs[kvh]), r=[bKSs], w=[kv["KS"][1]], dma=True)
                                P.op("sp", I("dma_start", out=kv["KS"][0][64:128, :], in_=ind_d), w=[kv["KS"][1]], dma=True)
                                P.op("sp", I("dma_start", out=kv["KW"][0][0:64, :], in_=KWs[kvh]), r=[bKWs], w=[kv["KW"][1]], dma=True)
                                P.op("sp", I("dma_start", out=kv["VS"][0][:], in_=VSs[kvh]), r=[bVSs], w=[kv["VS"][1]], dma=True)
                                P.op("sp", I("dma_start", out=kv["VW"][0][:], in_=VWs[kvh]), r=[bVWs], w=[kv["VW"][1]], dma=True)
                            Q, bQ = QR.next()
                            P.op("sp", I("dma_start", out=Q[0:64, :], in_=QNs[h, :, qb * 512:(qb + 1) * 512]), r=[bQNs], w=[bQ], dma=True)
                            P.op("sp", I("dma_start", out=Q[64:128, :], in_=NGs[kvh, :, qb * 512:(qb + 1) * 512]), r=[bNGs], w=[bQ], dma=True)
                            js["q"] = (Q, bQ)
                            jw["q"] = (Q[:, :], bQ)
                            K, bK = kv["KS"]
                            V, bV = kv["VS"]
                            tiles = []
                            for kt in range(4 * qb + 4):
                                lo = max(0, kt - 4 * qb)
                                masks = []
                                for qs in range(lo, 4):
                                    qt = 4 * qb + qs
                                    if kt == qt:
                                        masks.append((qs, EM[:, h, 0:128], bEM))
                                    elif kt == qt - 1:
                                        masks.append((qs, EM[:, h, 128:256], bEM))
                                tiles.append((K[:, kt * 128:(kt + 1) * 128], bK, V[:, kt, :], bV, lo, 4, masks))
                            js["tiles"][:] = tiles
                            K, bK = kv["KW"]
                            V, bV = kv["VW"]
                            tiles = []
                            for kt in range(max(0, 4 * qb - 4), 4 * qb + 4):
                                lo = max(0, kt - 4 * qb)
                                hi = min(4, kt - 4 * qb + 5)
                                masks = []
                                for qs in range(lo, hi):
                                    qt = 4 * qb + qs
                                    if kt == qt:
                                        masks.append((qs, EM[:, h, 0:128], bEM))
                                    elif kt == qt - 1:
                                        masks.append((qs, EM[:, h, 128:256], bEM))
                                    elif kt == qt - 4:
                                        masks.append((qs, FARM[:, :], bFARM))
                                tiles.append((K[:, kt * 128:(kt + 1) * 128], bK, V[:, kt, :], bV, lo, hi, masks))
                            jw["tiles"][:] = tiles
                        js["load"] = load
                        jw["load"] = None
                        js["tiles"] = [None] * (4 * qb + 4)
                        jw["tiles"] = [None] * (4 * qb + 4 - max(0, 4 * qb - 4))
                        for j_, mk_, dst, dbuf, col in ((js, mks, OSs, bOSs, 3 * h + 1), (jw, mkw, OWs, bOWs, 3 * h + 2)):
                            j_["bias"] = T31B[:, h:h + 1]
                            j_["rbias"] = [bT31B]
                            j_["evac"] = mk_(dst, dbuf, h, qb, col)
                            jobs.append(j_)
            run_attention(L, jobs)
            P.barrier()

    def phase_M(b):
        with ExitStack() as L:
            OMR = Ring([(sb(L, "OM%d" % i, [128, 512]), Buf("OM%d" % i)) for i in range(4)])
            ONR = Ring([(sb(L, "ON%d" % i, [128, 3, 512]), [Buf("ON%d_%d" % (i, j)) for j in range(3)]) for i in range(4)])
            MXR = Ring([(sb(L, "MX%d" % i, [128, 1024]), Buf("MX%d" % i)) for i in range(3)])
            JNKR = Ring([(sb(L, "JNK%d" % i, [128, 512]), Buf("JNK%d" % i)) for i in range(2)])
            SSQR = Ring([(sb(L, "SSQ%d" % i, [128, 2]), Buf("SSQ%d" % i)) for i in range(4)])
            MXT = Ring([(sb(L, "MXT%d" % i, [128, 8, 512], BF16), Buf("MXT%d" % i)) for i in range(2)])
            XR = Ring([(sb(L, "Xm%d" % i, [128, 8, 512]), Buf("Xm%d" % i)) for i in range(2)])
            W_out = sb(L, "W_out", [128, 8, 1024], BF16); bW_out = Buf("W_out")
            GOUT = sb(L, "GOUT", [128, 1024]); bGOUT = Buf("GOUT")
            MSR = Ring([(sb(L, "mstg%d" % i, [128, 1024]), Buf("mstg%d" % i)) for i in range(2)])
            wv = w_out_d.rearrange("(c p) n -> p c n", p=128)
            for c in range(8):
                load_cast(L, W_out[:, c, :], wv[:, c, :], bW_out, 128, 1024, MSR, c)
            P.op("sp", I("dma_start", out=GOUT[:], in_=gout_d), w=[bGOUT], dma=True)
            xv = xT[b].rearrange("(c p) t -> p c t", p=128)
            hv = HTs[b].rearrange("(c p) t -> p c t", p=128)
            for tb in range(NTB):
                MT, bMT = MXT.next()
                X, bX = XR.next()
                P.op("sp", I("dma_start", out=X[:], in_=xv[:, :, tb * 512:(tb + 1) * 512]), w=[bX], dma=True)
                for ts in range(4):
                    t = tb * 4 + ts
                    OM, bOM = OMR.next()
                    ON, bONs = ONR.next()
                    MX, bMX = MXR.next()
                    SSQ, bSSQ = SSQR.next()
                    P.op("sp", I("dma_start", out=OM[:], in_=OMs[t * 128:(t + 1) * 128, :]), r=[bOMs], w=[bOM], dma=True)
                    for i, (src, sbuf_) in enumerate(((OCs, bOCs), (OSs, bOSs), (OWs, bOWs))):
                        P.op("sp", I("dma_start", out=ON[:, i, :], in_=src[t * 128:(t + 1) * 128, :]), r=[sbuf_], w=[bONs[i]], dma=True)
                    bON = bONs[0]
                    P.op("pool", I("tensor_tensor", out=ON[:, 0, :], in0=ON[:, 0, :], in1=ON[:, 1, :], op=ALU.add), r=[bONs[0], bONs[1]], w=[bONs[0]])
                    P.op("pool", I("tensor_tensor", out=ON[:, 0, :], in0=ON[:, 0, :], in1=ON[:, 2, :], op=ALU.add), r=[bONs[0], bONs[2]], w=[bONs[0]])
                    JNK, bJNK = JNKR.next()
                    P.op("act", I("activation", out=JNK[:], in_=OM[:], func=AF.Square, accum_out=SSQ[:, 0:1]), r=[bOM], w=[bJNK, bSSQ])
                    JNK, bJNK = JNKR.next()
                    P.op("act", I("activation", out=JNK[:], in_=ON[:, 0, :], func=AF.Square, accum_out=SSQ[:, 1:2]), r=[bON], w=[bJNK, bSSQ])
                    P.op("act", I("activation", out=SSQ[:], in_=SSQ[:], func=AF.Sqrt, bias=EPSB[:, 0:1], scale=1.0 / 512.0),
                         r=[bSSQ, bEPSB], w=[bSSQ])
                    P.op("dve", I("reciprocal", out=SSQ[:], in_=SSQ[:]), r=[bSSQ], w=[bSSQ])
                    P.op("dve", I("scalar_tensor_tensor", out=MX[:, 0:512], in0=OM[:], scalar=SSQ[:, 0:1], in1=GOUT[:, 0:512],
                                  op0=ALU.mult, op1=ALU.mult), r=[bOM, bSSQ, bGOUT], w=[bMX])
                    P.op("dve", I("scalar_tensor_tensor", out=MX[:, 512:1024], in0=ON[:, 0, :], scalar=SSQ[:, 1:2],
                                  in1=GOUT[:, 512:1024], op0=ALU.mult, op1=ALU.mult), r=[bON, bSSQ, bGOUT], w=[bMX])
                    for half in range(2):
                        pt, pb = PSR.next()
                        for c4 in range(4):
                            c = half * 4 + c4
                            P.op("pe", I("transpose", out=pt[:, c4 * 128:(c4 + 1) * 128], in_=MX[:, c * 128:(c + 1) * 128],
                                         identity=IDN[:, :]), r=[bMX, bIDN], w=[pb])
                        ov = MT[:, half * 4:half * 4 + 4, ts * 128:(ts + 1) * 128]
                        iv = pt[:, :].rearrange("p (c t) -> p c t", t=128)
                        if half == 0:
                            P.op("act", I("activation", out=ov, in_=iv, func=AF.Copy), r=[pb], w=[bMT])
                        else:
                            P.op("dve", I("tensor_copy", out=ov, in_=iv), r=[pb], w=[bMT])
                for m in range(8):
                    pt, pb = PSR.next()
                    for c in range(8):
                        P.op("pe", I("matmul", pt[:, :], lhsT=W_out[:, c, m * 128:(m + 1) * 128], rhs=MT[:, c, :],
                                     start=(c == 0), stop=(c == 7)), r=[bW_out, bMT], w=[pb])
                    P.op("dve", I("tensor_tensor", out=X[:, m, :], in0=X[:, m, :], in1=pt[:, :], op=ALU.add), r=[bX, pb], w=[bX])
                P.op("pool", I("dma_start", out=hv[:, :, tb * 512:(tb + 1) * 512], in_=X[:]), r=[bX], w=[bHTs[b]], dma=True)
            P.barrier()

    stop = os.environ.get("MK_STOP", "")
    for b in range(NB):
        phase_P(b)
        if stop == "P":
            break
        phase_C()
        phase_MLA()
        if stop == "MLA":
            break
        phase_NC()
        if stop == "NC":
            break
        phase_NSW()
        phase_M(b)
        if stop == "M":
            break
    A.close()
    if stop:
        with ExitStack() as L:
            Z = sb(L, "Z", [128, 512]); bZ = Buf("Z")
            P.op("pool", I("memset", Z[:], 0.0), w=[bZ])
            P.op("sp", I("dma_start", out=outT[0, 0:128, 0:512], in_=Z[:]), r=[bZ], w=[bOUT], dma=True)
            P.barrier()
        P.emit()
        es.close()
        return nc

    with ExitStack() as Fs:
        WG = sb(Fs, "WG", [128, 8, DFF], BF16); bWG = Buf("WG")
        WU = sb(Fs, "WU", [128, 8, DFF], BF16); bWU = Buf("WU")
        WD = sb(Fs, "WD", [128, 22, D], BF16); bWD = Buf("WD")
        with ExitStack() as SU:
            stg = [(sb(SU, "fstg%d" % i, [128, DFF]), Buf("fstg%d" % i)) for i in range(4)]
            SR = Ring(stg)
            ei = 0
            for (wd, wt, wb) in ((w_gate_d, WG, bWG), (w_up_d, WU, bWU)):
                wv = wd.rearrange("(c p) n -> p c n", p=128)
                for c in range(8):
                    load_cast(SU, wt[:, c, :], wv[:, c, :], wb, 128, DFF, SR, ei, indep=True); ei += 1
            wv = w_down_d.rearrange("(c p) n -> p c n", p=128)
            for c in range(22):
                load_cast(SU, WD[:, c, :], wv[:, c, :], bWD, 128, D, SR, ei, indep=True); ei += 1
            P.barrier()
        H = sb(Fs, "H", [128, 8, 512]); bH = Buf("H")
        OUT = sb(Fs, "OUT", [128, 8, 512]); bOUTt = Buf("OUTt")
        HN = sb(Fs, "HN", [128, 8, 512], BF16); bHN = Buf("HN")
        RSa = sb(Fs, "RSa", [128, 512]); bRSa = Buf("RSa")
        RSb = sb(Fs, "RSb", [128, 512]); bRSb = Buf("RSb")
        AT = sb(Fs, "AT", [128, 22, 512], BF16); bAT = [Buf("AT%d" % i) for i in range(22)]
        SGR = Ring([(sb(Fs, "SGf%d" % i, [128, 512]), Buf("SGf%d" % i)) for i in range(2)])
        SQR = Ring([(sb(Fs, "SQf%d" % i, [128, 512], BF16), Buf("SQf%d" % i)) for i in range(4)])
        blocks = [(b, tb) for b in range(NB) for tb in range(NTB)]

        def stats(src, bsrc, RS, bRS):
            pt, pb = PSR.next()
            for c in range(8):
                sq, bsq = SQR.next()
                P.op("act", I("activation", out=sq[:], in_=src[:, c, :], func=AF.Square), r=[bsrc], w=[bsq])
                P.op("pe", I("matmul", pt[:, :], lhsT=ONES[:, :], rhs=sq[:], start=(c == 0), stop=(c == 7)),
                     r=[bONES, bsq], w=[pb])
            P.op("act", I("activation", out=RS[:], in_=pt[:, :], func=AF.Sqrt, bias=EPSB[:, 0:1], scale=1.0 / 1024.0),
                 r=[pb, bEPSB], w=[bRS])
            P.op("dve", I("reciprocal", out=RS[:], in_=RS[:]), r=[bRS], w=[bRS])

        def chain1(k):
            b, tb = blocks[k]
            hv = HTs[b].rearrange("(c p) t -> p c t", p=128)
            P.op("sp", I("dma_start", out=H[:], in_=hv[:, :, tb * 512:(tb + 1) * 512]), r=[bHTs[b]], w=[bH], dma=True)
            stats(H, bH, RSa, bRSa)

        def chain2(k):
            for c in range(8):
                P.op("dve", I("scalar_tensor_tensor", out=HN[:, c, :], in0=H[:, c, :], scalar=GV[:, 8 + c:9 + c],
                              in1=RSa[:], op0=ALU.mult, op1=ALU.mult), r=[bH, bGV, bRSa], w=[bHN])

        def copies(k):
            for c in range(8):
                P.op("pool", I("tensor_copy", out=OUT[:, c, :], in_=H[:, c, :]), r=[bH], w=[bOUTt])

        def gateup(k, f):
            pg, pgb = PSR.next()
            for c in range(8):
                P.op("pe", I("matmul", pg[:, :], lhsT=WG[:, c, f * 128:(f + 1) * 128], rhs=HN[:, c, :],
                             start=(c == 0), stop=(c == 7)), r=[bWG, bHN], w=[pgb])
            pu, pub = PSR.next()
            for c in range(8):
                P.op("pe", I("matmul", pu[:, :], lhsT=WU[:, c, f * 128:(f + 1) * 128], rhs=HN[:, c, :],
                             start=(c == 0), stop=(c == 7)), r=[bWU, bHN], w=[pub])
            SG, bSG = SGR.next()
            P.op("act", I("activation", out=SG[:], in_=pg[:, :], func=AF.Silu), r=[pgb], w=[bSG])
            P.op("dve", I("tensor_tensor", out=AT[:, f, :], in0=SG[:], in1=pu[:, :], op=ALU.mult),
                 r=[bSG, pub], w=[bAT[f]])

        def down(k):
            for m in range(8):
                pt, pb = PSR.next()
                for f in range(22):
                    P.op("pe", I("matmul", pt[:, :], lhsT=WD[:, f, m * 128:(m + 1) * 128], rhs=AT[:, f, :],
                                 start=(f == 0), stop=(f == 21)), r=[bWD, bAT[f]], w=[pb])
                P.op("dve", I("tensor_tensor", out=OUT[:, m, :], in0=OUT[:, m, :], in1=pt[:, :], op=ALU.add),
                     r=[bOUTt, pb], w=[bOUTt])

        def final(k):
            b, tb = blocks[k]
            ov = outT[b].rearrange("(c p) t -> p c t", p=128)
            stats(OUT, bOUTt, RSb, bRSb)
            for c in range(8):
                P.op("dve", I("scalar_tensor_tensor", out=OUT[:, c, :], in0=OUT[:, c, :], scalar=GV[:, 16 + c:17 + c],
                              in1=RSb[:], op0=ALU.mult, op1=ALU.mult), r=[bOUTt, bGV, bRSb], w=[bOUTt])
            P.op("pool", I("dma_start", out=ov[:, :, tb * 512:(tb + 1) * 512], in_=OUT[:]), r=[bOUTt], w=[bOUT], dma=True)

        nblk = len(blocks)
        chain1(0)
        for k in range(nblk):
            chain2(k)
            for f in range(22):
                gateup(k, f)
                if f == 1:
                    if k > 0:
                        final(k - 1)
                    copies(k)
                if f == 14 and k + 1 < nblk:
                    chain1(k + 1)
            down(k)
        final(nblk - 1)
        P.barrier()
    P.emit()
    es.close()
    return nc


def prep_inputs(inp):
    f = lambda a: np.ascontiguousarray(np.asarray(a, dtype=np.float32))
    c = host_consts()
    shared = {
        "w_in": f(inp["w_in"][0]), "w_uq": f(inp["mla_w_uq"][0]), "w_ukv": f(inp["mla_w_ukv"][0]),
        "w1k": f(inp["nsa_cmp_w1_k"][0]), "w1v": f(inp["nsa_cmp_w1_v"][0]),
        "w2k": f(inp["nsa_cmp_w2_k"][0]), "w2v": f(inp["nsa_cmp_w2_v"][0]),
        "poskT": f(np.asarray(inp["nsa_cmp_pos_k"][0]).T), "posvT": f(np.asarray(inp["nsa_cmp_pos_v"][0]).T),
        "t5": f(inp["t5_table"]), "w_out": f(inp["w_out"][0]),
        "w_gate": f(inp["w_gate"][0]), "w_up": f(inp["w_up"][0]), "w_down": f(inp["w_down"][0]),
    }
    gv = np.zeros((128, 32), np.float32)
    gv[:, 0:8] = np.asarray(inp["norm_mix_g"][0], np.float32).reshape(8, 128).T
    gv[:, 8:16] = np.asarray(inp["norm_ffn_g"][0], np.float32).reshape(8, 128).T
    gv[:, 16:24] = np.asarray(inp["final_norm_g"], np.float32).reshape(8, 128).T
    gv[:, 24:26] = np.asarray(inp["mla_q_norm_g"][0], np.float32).reshape(2, 128).T
    gv[:, 26] = np.asarray(inp["mla_kv_norm_g"][0], np.float32)
    shared["gvec"] = gv
    go = np.concatenate([np.asarray(inp["out_norm_mla_g"][0], np.float32), np.asarray(inp["out_norm_nsa_g"][0], np.float32)])
    shared["gout"] = np.ascontiguousarray(np.broadcast_to(go[None, :], (128, 1024)))
    for k in ("tri", "farm", "antiI", "ident", "ind", "ohd", "ohc", "selc", "ovl", "rope"):
        shared[k] = c[k]
    x = np.asarray(inp["x"], np.float32)
    maps = []
    for i in range(NCORES):
        m = dict(shared)
        m["xT"] = np.ascontiguousarray(x[i * NB:(i + 1) * NB].transpose(0, 2, 1))
        maps.append(m)
    return maps


def kernel(**inputs):
    nc = build()
    maps = prep_inputs(inputs)
    res = run_bass_kernel_spmd(nc, maps, core_ids=list(range(NCORES)))
    out = np.empty((NCORES * NB, S, D), np.float32)
    for i in range(NCORES):
        o = np.asarray(res.results[i]["outT"], np.float32)
        out[i * NB:(i + 1) * NB] = o.transpose(0, 2, 1)
    return out
```
